# Optimizing a Trainium2 kernel written in Bass

```python
import math
import jax
import jax.numpy as jnp
from jax import lax
import numpy as np

D_MODEL = 1024
BATCH = 8
SEQ = 4096
DEPTH = 1

D_MIX = D_MODEL
HGRN_WIDTH = D_MIX // 2
RET_WIDTH = D_MIX - HGRN_WIDTH
HGRN_HEADS = 4
HGRN_DK = HGRN_WIDTH // HGRN_HEADS
HGRN_DV = HGRN_WIDTH // HGRN_HEADS
RET_HEADS = 4
RET_DK = RET_WIDTH // RET_HEADS
RET_DV = RET_WIDTH // RET_HEADS
CHUNK = 64
ROPE_BASE = 10000.0
PROJ_SIZES = (HGRN_HEADS * HGRN_DK, HGRN_HEADS * HGRN_DK, HGRN_HEADS * HGRN_DV, HGRN_HEADS * HGRN_DV,
              RET_HEADS * RET_DK, RET_HEADS * RET_DK, RET_HEADS * RET_DV, RET_HEADS * RET_DV)
D_PROJ = sum(PROJ_SIZES)
N_GROUPS = 4
EXPERTS_PER_GROUP = 8
N_EXPERTS = N_GROUPS * EXPERTS_PER_GROUP
TOP_K = 2
D_EXPERT = D_MODEL // 2
MOE_BLOCK = 128
DEEPNORM_ALPHA = (2.0 * DEPTH) ** 0.25
DEEPNORM_BETA = (8.0 * DEPTH) ** -0.25
LN_EPS = 1e-5

kernel_name = "hymba_style_hgrn2_retention_hmoe_deepnorm_adaln"


def _layer_norm(x, w=None, b=None):
    xf = x.astype(jnp.float32)
    mu = jnp.mean(xf, axis=-1, keepdims=True)
    var = jnp.mean(jnp.square(xf - mu), axis=-1, keepdims=True)
    y = (xf - mu) * lax.rsqrt(var + LN_EPS)
    if w is not None:
        y = y * w.astype(jnp.float32) + b.astype(jnp.float32)
    return y.astype(x.dtype)


def _head_rms_norm(o, w):
    H, d = o.shape[2], o.shape[3]
    o = o * lax.rsqrt(jnp.mean(jnp.square(o), axis=-1, keepdims=True) + LN_EPS)
    return o * w.astype(jnp.float32).reshape(H, d)


def _head_group_norm(o, w):
    H, d = o.shape[2], o.shape[3]
    mu = jnp.mean(o, axis=-1, keepdims=True)
    var = jnp.mean(jnp.square(o - mu), axis=-1, keepdims=True)
    return (o - mu) * lax.rsqrt(var + LN_EPS) * w.astype(jnp.float32).reshape(H, d)


def _to_chunks(t):
    B, S, H, d = t.shape
    return t.reshape(B, S // CHUNK, CHUNK, H, d).transpose(0, 3, 1, 2, 4)


def _from_chunks(t):
    B, H, N, C, d = t.shape
    return t.transpose(0, 2, 3, 1, 4).reshape(B, N * C, H, d)


def _chunk_state_scan(decay, kv):
    B, H, N, dk, dv = kv.shape

    def step(state, inp):
        d_n, kv_n = inp
        return d_n[..., None] * state + kv_n, state

    s0 = jnp.zeros((B, H, dk, dv), kv.dtype)
    _, s_before = lax.scan(step, s0, (jnp.moveaxis(decay, 2, 0), jnp.moveaxis(kv, 2, 0)))
    return jnp.moveaxis(s_before, 0, 2)


def _hgrn2_chunkwise(q, log_f, k, v):
    C = q.shape[3]
    causal = jnp.tril(jnp.ones((C, C), dtype=bool))
    b = jnp.cumsum(log_f, axis=3)
    q_in = q * jnp.exp(b)
    k_in = k * jnp.exp(-b)
    a = jnp.where(causal, jnp.einsum('bhntd,bhnsd->bhnts', q_in, k_in), 0.0)
    o_intra = jnp.einsum('bhnts,bhnsv->bhntv', a, v)
    b_last = b[:, :, :, -1:, :]
    kv = jnp.einsum('bhnsd,bhnsv->bhndv', k * jnp.exp(b_last - b), v)
    s_before = _chunk_state_scan(jnp.exp(b_last[:, :, :, 0, :]), kv)
    return o_intra + jnp.einsum('bhntd,bhndv->bhntv', q_in, s_before)


def _retention_chunkwise(q, k, v, log_gamma):
    C = q.shape[3]
    idx = jnp.arange(C, dtype=jnp.float32)
    rel = idx[:, None] - idx[None, :]
    causal = rel >= 0
    dmat = jnp.where(causal, jnp.exp(jnp.where(causal, rel, 0.0)[None] * log_gamma[:, None, None]), 0.0)
    scores = jnp.einsum('bhntd,bhnsd->bhnts', q, k) * dmat[None, :, None]
    o_intra = jnp.einsum('bhnts,bhnsv->bhntv', scores, v)
    k_decay = jnp.exp((C - 1.0 - idx)[None, :] * log_gamma[:, None])
    kv = jnp.einsum('bhnsd,bhnsv->bhndv', k * k_decay[None, :, None, :, None], v)
    B, H, N = q.shape[0], q.shape[1], q.shape[2]
    chunk_decay = jnp.broadcast_to(jnp.exp(C * log_gamma)[None, :, None, None], (B, H, N, 1))
    s_before = _chunk_state_scan(chunk_decay, kv)
    q_decay = jnp.exp((idx + 1.0)[None, :] * log_gamma[:, None])
    o_inter = jnp.einsum('bhntd,bhndv->bhntv', q, s_before) * q_decay[None, :, None, :, None]
    return o_intra + o_inter


def _rotary(t, cos, sin):
    half = t.shape[-1] // 2
    t1, t2 = t[..., :half], t[..., half:]
    return jnp.concatenate([t1 * cos - t2 * sin, t1 * sin + t2 * cos], axis=-1)


def _hybrid_mixer(h, positions, w_in, w_out, lb, hgrn_norm_w, ret_norm_w):
    B, S, _ = h.shape
    proj = jnp.einsum('bsd,de->bse', h, w_in).astype(jnp.float32)
    offs = np.cumsum(PROJ_SIZES)[:-1].tolist()
    hq, hf, hi, hg, rq, rk, rv, rg = jnp.split(proj, offs, axis=-1)

    lbh = lb.astype(jnp.float32).reshape(HGRN_HEADS, HGRN_DK)
    z = hf.reshape(B, S, HGRN_HEADS, HGRN_DK)
    log_f = jnp.log(lbh + (1.0 - lbh) * jax.nn.sigmoid(z))
    k_a = (1.0 - lbh) * jax.nn.sigmoid(-z)
    q_a = jax.nn.silu(hq.reshape(B, S, HGRN_HEADS, HGRN_DK))
    v_a = hi.reshape(B, S, HGRN_HEADS, HGRN_DV)
    o_a = _from_chunks(_hgrn2_chunkwise(_to_chunks(q_a), _to_chunks(log_f), _to_chunks(k_a), _to_chunks(v_a)))
    o_a = _head_rms_norm(o_a, hgrn_norm_w) * jax.nn.silu(hg.reshape(B, S, HGRN_HEADS, HGRN_DV))

    inv_freq = jnp.power(ROPE_BASE, -jnp.arange(0, RET_DK, 2, dtype=jnp.float32) / RET_DK)
    ang = positions.astype(jnp.float32)[..., None] * inv_freq
    cos = jnp.cos(ang)[:, :, None, :]
    sin = jnp.sin(ang)[:, :, None, :]
    q_b = _rotary(rq.reshape(B, S, RET_HEADS, RET_DK), cos, sin) * (RET_DK ** -0.5)
    k_b = _rotary(rk.reshape(B, S, RET_HEADS, RET_DK), cos, sin)
    v_b = rv.reshape(B, S, RET_HEADS, RET_DV)
    log_gamma = jnp.log(1.0 - jnp.exp2(-5.0 - jnp.arange(RET_HEADS, dtype=jnp.float32)))
    o_b = _from_chunks(_retention_chunkwise(_to_chunks(q_b), _to_chunks(k_b), _to_chunks(v_b), log_gamma))
    o_b = _head_group_norm(o_b, ret_norm_w) * jax.nn.silu(rg.reshape(B, S, RET_HEADS, RET_DV))

    o = jnp.concatenate([o_a.reshape(B, S, -1), o_b.reshape(B, S, -1)], axis=-1).astype(h.dtype)
    return jnp.einsum('bse,ed->bsd', o, w_out)


def _grouped_experts(x, expert_idx, gates, w_gate, w_up, w_down):
    N, D = x.shape
    K = expert_idx.shape[1]
    E = w_gate.shape[0]
    flat_e = expert_idx.reshape(-1)
    flat_tok = jnp.repeat(jnp.arange(N, dtype=jnp.int32), K)
    flat_g = gates.reshape(-1)
    order = jnp.argsort(flat_e)
    se, stok, sg = flat_e[order], flat_tok[order], flat_g[order]
    counts = jnp.bincount(flat_e, length=E).astype(jnp.int32)
    starts = jnp.cumsum(counts) - counts
    padded = ((counts + MOE_BLOCK - 1) // MOE_BLOCK) * MOE_BLOCK
    pends = jnp.cumsum(padded)
    pstarts = pends - padded
    dest = pstarts[se] + (jnp.arange(N * K, dtype=jnp.int32) - starts[se])
    n_slots = ((N * K + MOE_BLOCK - 1) // MOE_BLOCK) * MOE_BLOCK + E * MOE_BLOCK
    slot_tok = jnp.full((n_slots,), N, dtype=jnp.int32).at[dest].set(stok)
    slot_g = jnp.zeros((n_slots,), x.dtype).at[dest].set(sg)
    n_blocks = n_slots // MOE_BLOCK
    block_start = jnp.arange(n_blocks, dtype=jnp.int32) * MOE_BLOCK
    block_e = jnp.minimum(jnp.searchsorted(pends, block_start, side='right'), E - 1).astype(jnp.int32)
    x_pad = jnp.concatenate([x, jnp.zeros((1, D), x.dtype)], axis=0)
    xb = x_pad[slot_tok].reshape(n_blocks, MOE_BLOCK, D)

    def expert_block(args):
        xblk, e = args
        a = xblk @ w_gate[e]
        u = xblk @ w_up[e]
        return (jax.nn.silu(a) * u) @ w_down[e]

    yb = lax.map(expert_block, (xb, block_e)).reshape(n_slots, D)
    out = jnp.zeros((N + 1, D), x.dtype).at[slot_tok].add(yb * slot_g[:, None])
    return out[:N]


def _hierarchical_moe(h, w_rg, b_rg, w_re, b_re, w_gate, w_up, w_down):
    B, S, D = h.shape
    x = h.reshape(B * S, D)
    g_logits = (x @ w_rg).astype(jnp.float32) + b_rg.astype(jnp.float32)
    g_prob = jax.nn.softmax(g_logits, axis=-1)
    g_star = jnp.argmax(g_logits, axis=-1).astype(jnp.int32)
    p_star = jnp.take_along_axis(g_prob, g_star[:, None], axis=1)
    e_logits = ((x @ w_re).astype(jnp.float32) + b_re.astype(jnp.float32)).reshape(-1, N_GROUPS, EXPERTS_PER_GROUP)
    e_sel = jnp.take_along_axis(e_logits, g_star[:, None, None], axis=1)[:, 0]
    top_v, top_i = lax.top_k(e_sel, TOP_K)
    gates = (p_star * jax.nn.softmax(top_v, axis=-1)).astype(h.dtype)
    expert_idx = (g_star[:, None] * EXPERTS_PER_GROUP + top_i).astype(jnp.int32)
    y = _grouped_experts(x, expert_idx, gates, w_gate, w_up, w_down)
    return y.reshape(B, S, D)


def setup_inputs(seed: int = 0) -> dict:
    key = jax.random.key(seed)
    ks = jax.random.split(key, 24)
    f32 = jnp.float32

    def nrm(k, shape, s):
        return s * jax.random.normal(k, shape, f32)

    beta = DEEPNORM_BETA
    col_scale = jnp.concatenate([
        jnp.full((PROJ_SIZES[0],), 1.0, f32), jnp.full((PROJ_SIZES[1],), 1.0, f32),
        jnp.full((PROJ_SIZES[2],), beta, f32), jnp.full((PROJ_SIZES[3],), 1.0, f32),
        jnp.full((PROJ_SIZES[4],), 1.0, f32), jnp.full((PROJ_SIZES[5],), 1.0, f32),
        jnp.full((PROJ_SIZES[6],), beta, f32), jnp.full((PROJ_SIZES[7],), 1.0, f32)])
    return {
        "x": jax.random.normal(ks[0], (BATCH, SEQ, D_MODEL), f32),
        "c": jax.random.normal(ks[1], (BATCH, D_MODEL), f32),
        "positions": jnp.tile(jnp.arange(SEQ, dtype=jnp.int32)[None, :], (BATCH, 1)),
        "w_ada": nrm(ks[2], (DEPTH, D_MODEL, 6 * D_MODEL), 0.5 * D_MODEL ** -0.5),
        "b_ada": nrm(ks[3], (DEPTH, 6 * D_MODEL), 0.01),
        "w_in": nrm(ks[4], (DEPTH, D_MODEL, D_PROJ), D_MODEL ** -0.5) * col_scale,
        "w_out": nrm(ks[5], (DEPTH, D_MIX, D_MODEL), D_MIX ** -0.5) * beta,
        "hgrn_lb": nrm(ks[6], (DEPTH + 1, HGRN_HEADS * HGRN_DK), 0.1),
        "hgrn_norm_w": 1.0 + nrm(ks[7], (DEPTH, HGRN_HEADS * HGRN_DV), 0.02),
        "ret_norm_w": 1.0 + nrm(ks[8], (DEPTH, RET_HEADS * RET_DV), 0.02),
        "post_ln1_w": 1.0 + nrm(ks[9], (DEPTH, D_MODEL), 0.02),
        "post_ln1_b": nrm(ks[10], (DEPTH, D_MODEL), 0.02),
        "w_rg": nrm(ks[11], (DEPTH, D_MODEL, N_GROUPS), D_MODEL ** -0.5),
        "b_rg": nrm(ks[12], (DEPTH, N_GROUPS), 0.01),
        "w_re": nrm(ks[13], (DEPTH, D_MODEL, N_EXPERTS), D_MODEL ** -0.5),
        "b_re": nrm(ks[14], (DEPTH, N_EXPERTS), 0.01),
        "w_gate": nrm(ks[15], (DEPTH, N_EXPERTS, D_MODEL, D_EXPERT), D_MODEL ** -0.5),
        "w_up": nrm(ks[16], (DEPTH, N_EXPERTS, D_MODEL, D_EXPERT), D_MODEL ** -0.5) * beta,
        "w_down": nrm(ks[17], (DEPTH, N_EXPERTS, D_EXPERT, D_MODEL), D_EXPERT ** -0.5) * beta,
        "post_ln2_w": 1.0 + nrm(ks[18], (DEPTH, D_MODEL), 0.02),
        "post_ln2_b": nrm(ks[19], (DEPTH, D_MODEL), 0.02),
    }


def reference(x, c, positions, w_ada, b_ada, w_in, w_out, hgrn_lb, hgrn_norm_w, ret_norm_w,
              post_ln1_w, post_ln1_b, w_rg, b_rg, w_re, b_re, w_gate, w_up, w_down,
              post_ln2_w, post_ln2_b):
    lb_all = jnp.cumsum(jax.nn.softmax(hgrn_lb.astype(jnp.float32), axis=0), axis=0)
    c_act = jax.nn.silu(c)
    for l in range(DEPTH):
        mod = c_act @ w_ada[l] + b_ada[l]
        shift1, scale1, gate1, shift2, scale2, gate2 = jnp.split(mod, 6, axis=-1)
        h = _layer_norm(x) * (1.0 + scale1[:, None, :]) + shift1[:, None, :]
        y = _hybrid_mixer(h, positions, w_in[l], w_out[l], lb_all[l], hgrn_norm_w[l], ret_norm_w[l])
        x = _layer_norm(DEEPNORM_ALPHA * x + gate1[:, None, :] * y, post_ln1_w[l], post_ln1_b[l])
        h = _layer_norm(x) * (1.0 + scale2[:, None, :]) + shift2[:, None, :]
        y = _hierarchical_moe(h, w_rg[l], b_rg[l], w_re[l], b_re[l], w_gate[l], w_up[l], w_down[l])
        x = _layer_norm(DEEPNORM_ALPHA * x + gate2[:, None, :] * y, post_ln2_w[l], post_ln2_b[l])
    return x
```

```python
import math
import numpy as np
import concourse.bass as bass
import concourse.mybir as mybir
from concourse.bass_utils import run_bass_kernel_spmd
from contextlib import ExitStack

F32 = mybir.dt.float32; BF16 = mybir.dt.bfloat16; I32 = mybir.dt.int32
ALU = mybir.AluOpType; AF = mybir.ActivationFunctionType; AX = mybir.AxisListType

NT = 32
ALPHA = 2.0 ** 0.25
EPS = 1e-5
BIG = 1.0e30
TWO_PI = 2.0 * math.pi


class _Op:
    __slots__ = ("eng", "fn", "deps", "sig", "val", "dsem", "dval")

    def __init__(self, eng, fn):
        self.eng = eng; self.fn = fn; self.deps = []; self.sig = False
        self.val = 0; self.dsem = None; self.dval = 0


class Sched:
    ENGS = ("pe", "act", "dve", "pool", "sp")

    def __init__(self, nc, es):
        self.nc = nc; self.es = es
        self.ops = {e: [] for e in self.ENGS}
        self.lastw = {}; self.readers = {}
        self.dsems = {}; self.dcount = {}; self.dlast = {}
        self.barrier_ops = []

    def _deps(self, op, reads, writes):
        deps = list(self.barrier_ops)
        for r in reads:
            w = self.lastw.get(r)
            if w is not None: deps.append(w)
        for w_ in writes:
            w = self.lastw.get(w_)
            if w is not None: deps.append(w)
            deps.extend(self.readers.get(w_, ()))
        for r in reads:
            self.readers.setdefault(r, []).append(op)
        for w_ in writes:
            self.lastw[w_] = op; self.readers[w_] = []
        seen = set()
        for d in deps:
            if d is op or id(d) in seen: continue
            seen.add(id(d)); op.deps.append(d)
            if d.dsem is None and not (d.eng == "pe" and op.eng == "pe"): d.sig = True

    def op(self, eng, fn, reads=(), writes=()):
        o = _Op(eng, fn); self._deps(o, reads, writes); self.ops[eng].append(o); return o

    def dma(self, eng, fn, slot, reads=(), writes=()):
        o = _Op(eng, fn)
        if slot not in self.dsems:
            self.dsems[slot] = self.es.enter_context(self.nc.semaphore("d_" + slot)); self.dcount[slot] = 0
        self.dcount[slot] += 16
        o.dsem = self.dsems[slot]; o.dval = self.dcount[slot]
        self._deps(o, reads, writes); self.ops[eng].append(o); self.dlast[slot] = o; return o

    def barrier(self):
        b = []
        for e in self.ENGS:
            if self.ops[e]:
                o = self.ops[e][-1]
                if o.dsem is None: o.sig = True
                b.append(o)
        b.extend(self.dlast.values())
        self.barrier_ops = b

    def emit(self, block, final_waits=()):
        nc = self.nc
        cengs = ("pe", "act", "dve", "pool")
        sems = {e: self.es.enter_context(nc.semaphore("s_" + e)) for e in cengs}
        total = {}
        for e in self.ENGS:
            comp = [o for o in self.ops[e] if o.dsem is None]
            if comp: comp[-1].sig = True
            c = 0
            for o in self.ops[e]:
                if o.dsem is None and o.sig:
                    assert e in cengs
                    c += 1; o.val = c
            total[e] = c

        def run(engname, engobj):
            waited = {}
            init = getattr(self, "init_" + engname, None)
            if init is not None: init(engobj)
            for o in self.ops[engname]:
                need = {}
                for d in o.deps:
                    if d.dsem is not None:
                        key = ("d", id(d.dsem)); sem = d.dsem; v = d.dval
                    else:
                        if d.eng == "pe" and engname == "pe": continue
                        key = ("e", d.eng); sem = sems[d.eng]; v = d.val
                    if v > need.get(key, (None, 0))[1]: need[key] = (sem, v)
                for key, (sem, v) in need.items():
                    if waited.get(key, 0) >= v: continue
                    engobj.wait_ge(sem, v); waited[key] = v
                ins = o.fn(engobj)
                if o.dsem is not None: ins.then_inc(o.dsem, 16)
                elif o.sig: ins.then_inc(sems[engname], 1)
            for e2 in cengs:
                if e2 != engname and total[e2] > 0: engobj.wait_ge(sems[e2], total[e2])
            for slot, sem in self.dsems.items():
                engobj.wait_ge(sem, self.dcount[slot])

        final = list(final_waits)

        @block.tensor
        def _(t): run("pe", t)

        @block.scalar
        def _(a): run("act", a)

        @block.vector
        def _(v): run("dve", v)

        @block.gpsimd
        def _(g): run("pool", g)

        @block.sync
        def _(s):
            run("sp", s)
            last = {}
            for d in final:
                last[id(d.dsem)] = (d.dsem, max(last.get(id(d.dsem), (None, 0))[1], d.dval))
            for sem, v in last.values(): s.wait_ge(sem, v)


def build_program(stage="full", stop=99, ntiles=NT):
    nc = bass.Bass("TRN2", target_bir_lowering=False)

    def din(name, shape, dt=F32):
        return nc.dram_tensor(name, shape, dt, kind="ExternalInput").ap()

    x_d = din("x", [4096, 1024]); ccol_d = din("ccol", [128, 8]); pos_d = din("pos", [32, 128], I32)
    wada_d = din("w_ada", [1024, 6144]); bada_d = din("b_ada", [1, 6144])
    win_d = din("w_in", [1024, 4096]); wout_d = din("w_out", [1024, 1024])
    lb_d = din("hgrn_lb", [2, 512]); hnw_d = din("hgrn_norm_w", [1, 512]); rnw_d = din("ret_norm_w", [1, 512])
    l1w_d = din("post_ln1_w", [1, 1024]); l1b_d = din("post_ln1_b", [1, 1024])
    l2w_d = din("post_ln2_w", [1, 1024]); l2b_d = din("post_ln2_b", [1, 1024])
    wr_d = din("w_r", [1024, 36]); br_d = din("b_r", [1, 36])
    if stage == "full":
        wg_d = din("w_gate", [8192, 2048]); wu_d = din("w_up", [8192, 2048]); wd_d = din("w_down", [8192, 2048])
    blkiota_d = din("blkiota", [128, 96]); piota_d = din("piota", [128, 1])
    zeros_d = din("zeros_bf", [2048, 1024], BF16)
    ident_d = din("ident", [128, 128]); tri_d = din("tri", [128, 128]); invf_d = din("invf", [1, 64])
    qd_d = din("qd", [128, 4]); kd_d = din("kd", [128, 4]); ebr_d = din("ebr", [128, 4])
    out_d = nc.dram_tensor("out", [4096, 1024], F32, kind="ExternalOutput").ap()
    x1_d = nc.dram_tensor("x1_scr", [4096, 1024], F32).ap()
    h2_d = nc.dram_tensor("h2_scr", [4096, 1024], BF16).ap()
    xs_d = nc.dram_tensor("xs_scr", [16384, 1024], BF16).ap()
    ys_d = nc.dram_tensor("ys_scr", [16384, 1024], F32).ap()
    g2row_d = nc.dram_tensor("g2row_scr", [1, 1024], F32).ap()
    cs_d = nc.dram_tensor("cs_scr", [128, 2, 2048], F32).ap()
    wgb_d = nc.dram_tensor("wgb_scr", [8192, 2048], BF16).ap()
    wub_d = nc.dram_tensor("wub_scr", [8192, 2048], BF16).ap()
    wdb_d = nc.dram_tensor("wdb_scr", [8192, 2048], BF16).ap()

    with ExitStack() as es:
        S = Sched(nc, es)

        def T(name, shape, dt=F32):
            return es.enter_context(nc.sbuf_tensor("sb_" + name, shape, dt))

        def PS(name, shape, dt=F32):
            return es.enter_context(nc.psum_tensor("pm_" + name, shape, dt))

        arena = T("arena", [128, 40960], BF16)
        arena2 = T("arena2", [128, 8192], F32)
        arena3 = T("arena3", [128, 8192], F32)
        w_in_sb = arena[:, 0:32768].rearrange("p (k n) -> p k n", k=8)
        w_out_sb = arena[:, 32768:40960].rearrange("p (k n) -> p k n", k=8)
        cos_t = arena2[:, 0:2048].rearrange("p (j f) -> p j f", j=32)
        sin_t = arena2[:, 2048:4096].rearrange("p (j f) -> p j f", j=32)
        x_sb = [arena2[:, 4096:5120], arena2[:, 5120:6144], arena2[:, 7168:8192]]
        g1bc = arena2[:, 6144:7168]

        def a3(s, n=1):
            return arena3[:, s * 512:(s + n) * 512]

        def a3n(s, n=1):
            return ["t%d" % j for j in range(s, s + n)]

        ccol = T("ccol", [128, 8]); cact = T("cact", [128, 8])
        ident_f = T("ident_f", [128, 128]); ident_b = T("ident_b", [128, 128], BF16); tri = T("tri", [128, 128])
        ones_f = T("ones_f", [128, 128])
        invf_bc = T("invf_bc", [128, 64]); qd = T("qd", [128, 4]); kd = T("kd", [128, 4]); ebl = T("ebl", [128, 8])
        lb = T("lb", [128, 512]); oml = T("oml", [128, 512]); normw = T("normw", [128, 1024])
        l1w = T("l1w", [128, 1024]); l1b = T("l1b", [128, 1024])
        br_bc = T("br_bc", [128, 36]); wr_sb = T("wr_sb", [128, 8, 36], BF16)
        modcol = T("modcol", [128, 32])
        posi = T("posi", [32, 128], I32); posf = T("posf", [32, 128]); posT = T("posT", [128, 32])
        bch = [arena2[:, 4096:4608], arena2[:, 4608:5120]]
        mrow = [arena2[:, 7168:7680], arena2[:, 7680:8192]]
        sc2_bc = T("sc2_bc", [128, 1024]); sh2_bc = T("sh2_bc", [128, 1024])
        blkiota = T("blkiota", [128, 96]); piota = T("piota", [128, 1])
        st = T("st", [128, 12]); mv = T("mv", [128, 2]); rs = T("rs", [128, 1])
        ost = T("ost", [128, 24]); omv = T("omv", [128, 4, 2]); r4 = T("r4", [128, 4])
        xn_bf = T("xn_bf", [128, 1024], BF16); hT = T("hT", [128, 8, 128], BF16)
        q_in = T("q_in", [128, 1024], BF16)
        cs_t = [T("cs_t0", [128, 2, 64]), T("cs_t1", [128, 2, 64])]
        eblA = [T("eblA0", [128, 4]), T("eblA1", [128, 4])]
        q_inT = T("q_inT", [128, 8, 128], BF16); k_inT = T("k_inT", [128, 8, 128], BF16)
        qin = [arena2[:, p * 512:(p + 1) * 512].bitcast(BF16) for p in range(2)]
        kin = [arena2[:, 1024 + p * 512:1024 + (p + 1) * 512].bitcast(BF16) for p in range(2)]
        vbf = [arena2[:, 2048 + p * 512:2048 + (p + 1) * 512].bitcast(BF16) for p in range(2)]
        ga = [arena2[:, 3072 + p * 512:3072 + (p + 1) * 512] for p in range(2)]
        gb = [arena2[:, 6144 + p * 512:6144 + (p + 1) * 512] for p in range(2)]
        AT = [T("AT0", [128, 4, 128], BF16), T("AT1", [128, 4, 128], BF16)]
        S32 = T("S32", [128, 8, 128]); S_bf = T("S_bf", [128, 8, 128], BF16)
        o_fin = T("o_fin", [128, 1024], BF16); oT_sb = T("oT_sb", [128, 8, 128], BF16)
        h2T_t = T("h2T_t", [128, 8, 128], BF16)
        h2tok = T("h2tok", [128, 1024], BF16)
        logits_all = T("logits_all", [128, 32, 36])
        G_all = S32[:, :, :].rearrange("p a (b c) -> p (a b) c", b=4)
        qf = q_in[:, :].bitcast(F32)
        gsm = qf[:, 0:256].rearrange("p (a b) -> p a b", a=8)

        ptr = PS("ptr", [128, 8, 128], BF16); ptq = PS("ptq", [128, 8, 128], BF16)
        pp = [PS("pp0", [128, 512]), PS("pp1", [128, 512])]
        pb = PS("pb", [128, 512]); ps_ = PS("ps", [128, 512]); po = PS("po", [128, 512]); pkv = PS("pkv", [128, 512])
        ps3 = ps_[:, :].rearrange("p (h d) -> p h d", h=4)
        po3 = po[:, :].rearrange("p (h d) -> p h d", h=4)
        pkv3 = pkv[:, :].rearrange("p (h d) -> p h d", h=4)

        block = es.enter_context(nc.Block())

        def r3(ap, h=4):
            return ap.rearrange("p (h d) -> p h d", h=h)

        def ld(eng, dst, src, slot, w):
            return S.dma(eng, lambda e: e.dma_start(out=dst, in_=src), slot, writes=w)

        ld("sp", ccol[:], ccol_d[:, :], "c", ["ccol"])
        ld("sp", ident_f[:], ident_d[:, :], "identf", ["ident_f"])
        ld("sp", tri[:], tri_d[:, :], "tri", ["tri"])
        ld("sp", invf_bc[:], invf_d.partition_broadcast(128), "invf", ["invf"])
        ld("sp", qd[:], qd_d[:, :], "qd", ["qd"])
        ld("sp", kd[:], kd_d[:, :], "kd", ["kd"])
        ld("sp", ebl[:, 4:8], ebr_d[:, :], "ebr", ["ebl1"])
        ld("sp", lb[:], lb_d[0:1, :].partition_broadcast(128), "lbA", ["lb"])
        ld("sp", oml[:], lb_d[1:2, :].partition_broadcast(128), "lbB", ["oml"])
        ld("sp", normw[:, 0:512], hnw_d.partition_broadcast(128), "hnw", ["normw0"])
        ld("sp", normw[:, 512:1024], rnw_d.partition_broadcast(128), "rnw", ["normw1"])
        ld("sp", l1w[:], l1w_d.partition_broadcast(128), "l1w", ["l1w"])
        ld("sp", l1b[:], l1b_d.partition_broadcast(128), "l1b", ["l1b"])
        ld("sp", br_bc[:], br_d.partition_broadcast(128), "br", ["br_bc"])
        ld("sp", posi[:], pos_d[:, :], "pos", ["posi"])
        ld("sp", blkiota[:], blkiota_d[:, :], "blkiota", ["blkiota"])
        ld("sp", piota[:], piota_d[:, :], "piota", ["piota"])
        ld("pool", ident_b[:], ident_d[:, :], "identb", ["ident_b"])
        ld("pool", wr_sb[:], wr_d.rearrange("(k p) n -> p k n", p=128), "wr", ["wr_sb"])
        for j in range(4):
            ld("pool", w_in_sb[:, :, j * 1024:(j + 1) * 1024],
               win_d[:, j * 1024:(j + 1) * 1024].rearrange("(k p) n -> p k n", p=128), "win", ["w_in"])
        ld("pool", w_out_sb, wout_d.rearrange("(k p) n -> p k n", p=128), "wout", ["w_out"])

        S.op("dve", lambda e: e.memset(ones_f[:], 1.0), writes=["ones_f"])
        S.op("dve", lambda e: e.memset(S32[:], 0.0), writes=["S32_0", "S32_1"])
        S.op("pool", lambda e: e.memset(S_bf[:], 0.0), writes=["Sbf0", "Sbf1"])

        S.op("act", lambda e: e.activation(out=cact[:], in_=ccol[:], func=AF.Exp, scale=-1.0), ["ccol"], ["cact"])
        S.op("act", lambda e: e.activation(out=cact[:], in_=cact[:], func=AF.Ln, bias=1.0), ["cact"], ["cact"])
        S.op("act", lambda e: e.activation(out=cact[:], in_=cact[:], func=AF.Exp, scale=-1.0), ["cact"], ["cact"])
        S.op("dve", lambda e: e.tensor_tensor(out=cact[:], in0=cact[:], in1=ccol[:], op=ALU.mult), ["cact", "ccol"], ["cact"])

        S.op("dve", lambda e: e.tensor_tensor(out=lb[:], in0=lb[:], in1=oml[:], op=ALU.subtract), ["lb", "oml"], ["lb"])
        S.op("act", lambda e: e.activation(out=lb[:], in_=lb[:], func=AF.Exp, scale=-1.0), ["lb"], ["lb"])
        S.op("act", lambda e: e.activation(out=lb[:], in_=lb[:], func=AF.Ln, bias=1.0), ["lb"], ["lb"])
        S.op("act", lambda e: e.activation(out=lb[:], in_=lb[:], func=AF.Exp, scale=-1.0), ["lb"], ["lb"])
        S.op("dve", lambda e: e.tensor_scalar(out=oml[:], in0=lb[:], scalar1=-1.0, scalar2=1.0, op0=ALU.mult, op1=ALU.add),
             ["lb"], ["oml"])

        tmp4 = arena2[:, 1024:1536]
        for cg in range(12):
            par = cg % 2; eng = "dve"
            wst = arena3[:, par * 4096:(par + 1) * 4096].rearrange("p (k n) -> p k n", k=8)
            wreg = a3n(par * 8, 8)
            S.dma("sp", lambda e, wst=wst, cg=cg: e.dma_start(
                out=wst, in_=wada_d[:, cg * 512:(cg + 1) * 512].rearrange("(k p) n -> p k n", p=128)),
                "wada%d" % par, writes=wreg)
            S.dma("sp", lambda e, par=par, cg=cg: e.dma_start(out=bch[par], in_=bada_d[0:1, cg * 512:(cg + 1) * 512].partition_broadcast(128)),
                  "bada%d" % par, writes=["bch%d" % par])
            tmpw = arena2[:, par * 512:(par + 1) * 512]; tn = "tmpw%d" % par
            S.op(eng, lambda e, wst=wst, tmpw=tmpw: e.tensor_scalar_mul(out=tmpw, in0=wst[:, 0, :], scalar1=cact[:, 0:1]),
                 wreg + ["cact"], [tn])
            for k in range(1, 8):
                S.op(eng, lambda e, wst=wst, tmpw=tmpw, k=k: e.scalar_tensor_tensor(out=tmpw, in0=wst[:, k, :], scalar=cact[:, k:k + 1],
                                                                                  in1=tmpw, op0=ALU.mult, op1=ALU.add),
                     wreg + ["cact", tn], [tn])
            S.op("pe", lambda e, tmpw=tmpw: e.matmul(pb[:, :], lhsT=ones_f[:], rhs=tmpw, start=True, stop=True), [tn, "ones_f"], ["pb"])
            seg = cg // 2; half = cg % 2
            if seg in (2, 3, 4):
                dst = {2: g1bc, 3: sh2_bc, 4: sc2_bc}[seg][:, half * 512:(half + 1) * 512]
                dname = "%s%d" % ({2: "g1bc", 3: "sh2bc", 4: "sc2bc"}[seg], half)
            else:
                dst = mrow[par]; dname = "mrow%d" % par
            S.op("dve", lambda e, dst=dst, par=par: e.tensor_tensor(out=dst, in0=pb[:, :], in1=bch[par], op=ALU.add),
                 ["pb", "bch%d" % par], [dname])
            if seg in (1, 4):
                S.op("dve", lambda e, dst=dst: e.tensor_scalar_add(out=dst, in0=dst, scalar1=1.0), [dname], [dname])
            if seg in (0, 1):
                base = {0: 0, 1: 8}[seg] + half * 4
                S.op("dve", lambda e, dst=dst: e.tensor_tensor(out=r3(tmp4), in0=r3(dst), in1=ident_f[:].unsqueeze(1).broadcast_to([128, 4, 128]),
                                                               op=ALU.mult), [dname, "ident_f"], ["tmp4"])
                S.op("dve", lambda e, base=base: e.tensor_reduce(out=modcol[:, base:base + 4], in_=r3(tmp4), axis=AX.X, op=ALU.add),
                     ["tmp4"], ["modcol"])
            elif seg == 5:
                S.dma("sp", lambda e, par=par, half=half: e.dma_start(out=g2row_d[0:1, half * 512:(half + 1) * 512],
                                                                      in_=mrow[par][0:1, :]),
                      "g2st", reads=["mrow%d" % par], writes=["g2row_d"])
        for k in range(8):
            S.op("dve", lambda e, k=k: e.tensor_tensor(out=w_out_sb[:, k, :], in0=w_out_sb[:, k, :], in1=g1bc, op=ALU.mult),
                 ["w_out", "g1bc0", "g1bc1"], ["w_out"])

        S.op("dve", lambda e: e.tensor_copy(out=posf[:], in_=posi[:]), ["posi"], ["posf"])
        S.op("pe", lambda e: e.transpose(out=pb[:, 0:32], in_=posf[:], identity=ident_f[0:32, 0:32]), ["posf", "ident_f"], ["pb"])
        S.op("dve", lambda e: e.tensor_copy(out=posT[:], in_=pb[:, 0:32]), ["pb"], ["posT"])
        ang = a3(0, 4); yv = a3(4, 4); kf = a3(8, 4); kfm = a3(12, 4); ki = a3(12, 4).bitcast(I32)
        angn = a3n(0, 4); yvn = a3n(4, 4); kfn = a3n(8, 4); kin_ = a3n(12, 4)
        S.op("dve", lambda e: e.tensor_tensor(out=ang.rearrange("p (j f) -> p j f", j=32),
                                              in0=posT[:].unsqueeze(2).broadcast_to([128, 32, 64]),
                                              in1=invf_bc[:].unsqueeze(1).broadcast_to([128, 32, 64]), op=ALU.mult),
             ["posT", "invf"], angn)

        def sin_table(src, srcn, dst, dstn):
            S.op("dve", lambda e: e.tensor_single_scalar(out=kf, in_=src, scalar=1.0 / TWO_PI, op=ALU.mult), srcn, kfn)
            S.op("dve", lambda e: e.tensor_copy(out=ki, in_=kf), kfn, kin_)
            S.op("dve", lambda e: e.tensor_copy(out=kf, in_=ki), kin_, kfn)
            S.op("dve", lambda e: e.scalar_tensor_tensor(out=kf, in0=kf, scalar=-TWO_PI, in1=src, op0=ALU.mult, op1=ALU.add),
                 kfn + srcn, kfn)
            S.op("dve", lambda e: e.tensor_single_scalar(out=kfm, in_=kf, scalar=math.pi, op=ALU.is_gt), kfn, kin_)
            S.op("dve", lambda e: e.scalar_tensor_tensor(out=kf, in0=kfm, scalar=-TWO_PI, in1=kf, op0=ALU.mult, op1=ALU.add),
                 kfn + kin_, kfn)
            S.op("dve", lambda e: e.tensor_single_scalar(out=kfm, in_=kf, scalar=-math.pi, op=ALU.is_lt), kfn, kin_)
            S.op("dve", lambda e: e.scalar_tensor_tensor(out=kf, in0=kfm, scalar=TWO_PI, in1=kf, op0=ALU.mult, op1=ALU.add),
                 kfn + kin_, kfn)
            S.op("act", lambda e: e.activation(out=dst, in_=kf, func=AF.Sin), kfn, dstn)

        S.op("dve", lambda e: e.tensor_scalar_add(out=yv, in0=ang, scalar1=math.pi / 2), angn, yvn)
        sin_table(yv, yvn, arena2[:, 0:2048], ["cos"])
        sin_table(ang, angn, arena2[:, 2048:4096], ["sin"])
        S.dma("sp", lambda e: e.dma_start(out=cs_d[:, 0, :], in_=arena2[:, 0:2048]), "csst", reads=["cos"], writes=["cs_d0"])
        S.dma("sp", lambda e: e.dma_start(out=cs_d[:, 1, :], in_=arena2[:, 2048:4096]), "csst", reads=["sin"], writes=["cs_d1"])

        if stage == "setup":
            fin = []
            fin.append(S.dma("sp", lambda e: e.dma_start(out=out_d[0:128, 0:32], in_=modcol[:]), "dbg", reads=["modcol"]))
            fin.append(S.dma("sp", lambda e: e.dma_start(out=out_d[0:128, 32:96], in_=cos_t[:, 3, :]), "dbg", reads=["cos"]))
            fin.append(S.dma("sp", lambda e: e.dma_start(out=out_d[0:128, 96:160], in_=sin_t[:, 3, :]), "dbg", reads=["sin"]))
            fin.append(S.dma("sp", lambda e: e.dma_start(out=out_d[0:128, 160:672], in_=lb[:]), "dbg", reads=["lb"]))
            fin.append(S.dma("sp", lambda e: e.dma_start(out=out_d[128:256, 0:1024], in_=g1bc), "dbg", reads=["g1bc0", "g1bc1"]))
            S.emit(block, final_waits=fin)
            return nc

        S.barrier()

        def ln_stats(src, srcn):
            S.op("dve", lambda e: e.bn_stats(out=st[:, 0:6], in_=src[:, 0:512]), srcn, ["st"])
            S.op("dve", lambda e: e.bn_stats(out=st[:, 6:12], in_=src[:, 512:1024]), srcn, ["st"])
            S.op("dve", lambda e: e.bn_aggr(out=mv[:], in_=st[:]), ["st"], ["mv"])
            S.op("act", lambda e: e.activation(out=rs[:], in_=mv[:, 1:2], func=AF.Ln, bias=EPS), ["mv"], ["rs"])
            S.op("act", lambda e: e.activation(out=rs[:], in_=rs[:], func=AF.Exp, scale=-0.5), ["rs"], ["rs"])

        def transposes8(dst_ps, dstn, src, srcn):
            for c in range(8):
                S.op("pe", lambda e, c=c: e.transpose(out=dst_ps[:, c, :], in_=src[:, c * 128:(c + 1) * 128], identity=ident_b[:]),
                     srcn + ["ident_b"], dstn)

        def modulate(dst, dstn, sc0, sh0):
            mm = "act"
            for c in range(8):
                if (mm == "mix" and c % 2 == 0) or mm == "act":
                    S.op("act", lambda e, c=c: e.activation(out=dst[:, c, :], in_=ptr[:, c, :], func=AF.Identity,
                                                            scale=modcol[:, sc0 + c:sc0 + c + 1], bias=modcol[:, sh0 + c:sh0 + c + 1]),
                         ["ptr", "modcol"], [dstn + str(c)])
                else:
                    S.op("dve", lambda e, c=c: e.tensor_scalar(out=dst[:, c, :], in0=ptr[:, c, :],
                                                               scalar1=modcol[:, sc0 + c:sc0 + c + 1],
                                                               scalar2=modcol[:, sh0 + c:sh0 + c + 1], op0=ALU.mult, op1=ALU.add),
                         ["ptr", "modcol"], [dstn + str(c)])

        def proj(g, bank):
            for k in range(8):
                S.op("pe", lambda e, k=k: e.matmul(pp[bank][:, :], lhsT=hT[:, k, :], rhs=w_in_sb[:, k, g * 512:(g + 1) * 512],
                                                   start=(k == 0), stop=(k == 7)),
                     ["hT%d" % c for c in range(8)] + ["w_in"], ["pp%d" % bank])

        def sigmoid_from_psum(bank, dst, dstn):
            S.op("act", lambda e: e.activation(out=dst, in_=pp[bank][:, :], func=AF.Exp, scale=-1.0), ["pp%d" % bank], dstn)
            S.op("act", lambda e: e.activation(out=dst, in_=dst, func=AF.Ln, bias=1.0), dstn, dstn)
            S.op("act", lambda e: e.activation(out=dst, in_=dst, func=AF.Exp, scale=-1.0), dstn, dstn)

        def rotary(i, src, srcn, dst, dstn, dec, decn):
            s3 = r3(src); x1 = s3[:, :, 0:64]; x2 = s3[:, :, 64:128]
            cst = cs_t[i % 2]
            cosb = cst[:, 0, :].unsqueeze(1).broadcast_to([128, 4, 64])
            sinb = cst[:, 1, :].unsqueeze(1).broadcast_to([128, 4, 64])
            A = r3(a3(10)[:, 0:256]); B = r3(a3(10)[:, 256:512]); C = r3(a3(11)[:, 0:256]); Dd = r3(a3(11)[:, 256:512])
            decb = dec[:, 0:4].unsqueeze(2).broadcast_to([128, 4, 64])
            d3 = r3(dst)
            S.op("pool", lambda e: e.tensor_tensor(out=A, in0=x1, in1=cosb, op=ALU.mult), srcn + ["cs%d" % (i % 2)], ["t10a"])
            S.op("pool", lambda e: e.tensor_tensor(out=B, in0=x2, in1=sinb, op=ALU.mult), srcn + ["cs%d" % (i % 2)], ["t10b"])
            S.op("pool", lambda e: e.tensor_tensor(out=A, in0=A, in1=B, op=ALU.subtract), ["t10a", "t10b"], ["t10a"])
            S.op("dve", lambda e: e.tensor_tensor(out=C, in0=x1, in1=sinb, op=ALU.mult), srcn + ["cs%d" % (i % 2)], ["t11a"])
            S.op("dve", lambda e: e.tensor_tensor(out=Dd, in0=x2, in1=cosb, op=ALU.mult), srcn + ["cs%d" % (i % 2)], ["t11b"])
            S.op("dve", lambda e: e.tensor_tensor(out=C, in0=C, in1=Dd, op=ALU.add), ["t11a", "t11b"], ["t11a"])
            S.op("pool", lambda e: e.tensor_tensor(out=d3[:, :, 0:64], in0=A, in1=decb, op=ALU.mult), ["t10a", decn], [dstn + "lo"])
            S.op("dve", lambda e: e.tensor_tensor(out=d3[:, :, 64:128], in0=C, in1=decb, op=ALU.mult), ["t11a", decn], [dstn + "hi"])

        x1_stores = []
        t0 = a3(0); t1 = a3(1); t2 = a3(2); t3 = a3(3); t4 = a3(4); t5 = a3(5); t8 = a3(8); t9 = a3(9)

        def x_load(i):
            p3 = i % 3; xs = x_sb[p3]
            S.dma("sp", lambda e: e.dma_start(out=xs, in_=x_d[i * 128:(i + 1) * 128, :]), "x%d" % p3, writes=["x%d" % p3])

        def f_p1(i):
            p = i % 2; xs = x_sb[i % 3]; xn_ = ["x%d" % (i % 3)]
            ln_stats(xs, xn_)
            S.op("dve", lambda e: e.tensor_scalar(out=xn_bf[:], in0=xs, scalar1=mv[:, 0:1], scalar2=rs[:, 0:1],
                                                  op0=ALU.subtract, op1=ALU.mult), xn_ + ["mv", "rs"], ["xn_lo", "xn_hi"])

        def f_p2(i):
            p = i % 2
            S.dma("sp", lambda e: e.dma_start(out=cs_t[p][:], in_=cs_d[:, :, i * 64:(i + 1) * 64]), "cs%d" % p,
                  reads=["cs_d0", "cs_d1"], writes=["cs%d" % p])
            transposes8(ptr, ["ptr"], xn_bf, ["xn_lo", "xn_hi"])
            modulate(hT, "hT", 8, 0)

        def f_forget(i):
            p = i % 2
            proj(1, 0)
            proj(0, 1)
            sigmoid_from_psum(0, t0, ["t0"])
            S.op("dve", lambda e: e.tensor_tensor(out=t0, in0=t0, in1=oml[:], op=ALU.mult), ["t0", "oml"], ["t0"])
            S.op("pool", lambda e: e.tensor_tensor(out=t0, in0=t0, in1=lb[:], op=ALU.add), ["t0", "lb"], ["t0"])
            S.op("act", lambda e: e.activation(out=t1, in_=t0, func=AF.Ln), ["t0"], ["t1"])
            S.op("pool", lambda e: e.tensor_scalar(out=t2, in0=t0, scalar1=-1.0, scalar2=1.0, op0=ALU.mult, op1=ALU.add),
                 ["t0"], ["t2"])
            S.op("pe", lambda e: e.matmul(pb[:, :], lhsT=tri[:], rhs=t1, start=True, stop=True), ["tri", "t1"], ["pb"])
            S.op("act", lambda e: e.activation(out=t3, in_=pb[:, :], func=AF.Exp), ["pb"], ["t3"])
            S.op("act", lambda e: e.activation(out=t4, in_=pb[:, :], func=AF.Exp, scale=-1.0), ["pb"], ["t4"])
            for h in range(4):
                S.op("pe", lambda e, h=h: e.matmul(pb[:, h:h + 1], lhsT=t1[:, h * 128:(h + 1) * 128], rhs=ones_f[:, 0:1],
                                                   start=True, stop=True), ["t1", "ones_f"], ["pb"])
            S.op("act", lambda e: e.activation(out=eblA[p][:], in_=pb[:, 0:4], func=AF.Exp), ["pb"], ["ebl0_%d" % p])
            S.op("pool", lambda e: e.tensor_tensor(out=kin[p][:, 0:512], in0=t2, in1=t4, op=ALU.mult), ["t2", "t4"], ["kin_a%d" % p])

        def f_query(i):
            p = i % 2
            sigmoid_from_psum(1, t5, ["t5"])
            S.op("dve", lambda e: e.tensor_tensor(out=t5, in0=pp[1][:, :], in1=t5, op=ALU.mult), ["pp1", "t5"], ["t5"])
            S.op("dve", lambda e: e.tensor_tensor(out=qin[p][:, 0:512], in0=t5, in1=t3, op=ALU.mult), ["t5", "t3"], ["qin_a%d" % p])

        def f_retq(i):
            proj(4, 0)
            S.op("act", lambda e: e.activation(out=t8, in_=pp[0][:, :], func=AF.Copy), ["pp0"], ["t8"])

        def f_retk(i):
            proj(5, 1)
            S.op("act", lambda e: e.activation(out=t9, in_=pp[1][:, :], func=AF.Copy), ["pp1"], ["t9"])

        def f_rot(i):
            p = i % 2
            rotary(i, t8, ["t8"], qin[p][:, 512:1024], "qin_b%d" % p, qd, "qd")
            rotary(i, t9, ["t9"], kin[p][:, 512:1024], "kin_b%d" % p, kd, "kd")

        def f_values(i):
            p = i % 2
            proj(2, 0)
            S.op("act", lambda e: e.activation(out=vbf[p][:, 0:512], in_=pp[0][:, :], func=AF.Copy), ["pp0"], ["v_a%d" % p])
            proj(6, 1)
            S.op("act", lambda e: e.activation(out=vbf[p][:, 512:1024], in_=pp[1][:, :], func=AF.Copy), ["pp1"], ["v_b%d" % p])

        def f_gates(i):
            p = i % 2
            proj(3, 0)
            sigmoid_from_psum(0, ga[p], ["ga%d" % p])
            S.op("dve", lambda e: e.tensor_tensor(out=ga[p], in0=pp[0][:, :], in1=ga[p], op=ALU.mult), ["pp0", "ga%d" % p], ["ga%d" % p])
            proj(7, 1)
            sigmoid_from_psum(1, gb[p], ["gb%d" % p])
            S.op("dve", lambda e: e.tensor_tensor(out=gb[p], in0=pp[1][:, :], in1=gb[p], op=ALU.mult), ["pp1", "gb%d" % p], ["gb%d" % p])

        def b_qk(i):
            p = i % 2
            qn = ["qin_a%d" % p, "qin_b%dlo" % p, "qin_b%dhi" % p]; kn = ["kin_a%d" % p, "kin_b%dlo" % p, "kin_b%dhi" % p]
            transposes8(ptq, ["ptq"], qin[p], qn)
            S.op("act", lambda e: e.activation(out=q_inT[:], in_=ptq[:], func=AF.Copy), ["ptq"], ["qinT"])
            transposes8(ptq, ["ptq"], kin[p], kn)
            S.op("dve", lambda e: e.tensor_copy(out=k_inT[:], in_=ptq[:]), ["ptq"], ["kinT"])

        def b_sc(i, m):
            for hh in range(4):
                h = 4 * m + hh
                S.op("pe", lambda e, h=h, hh=hh: e.matmul(ps3[:, hh, :], lhsT=k_inT[:, h, :], rhs=q_inT[:, h, :],
                                                         start=True, stop=True), ["kinT", "qinT"], ["ps"])
            S.op("dve", lambda e: e.tensor_tensor(out=AT[m][:], in0=ps3, in1=tri[:].unsqueeze(1).broadcast_to([128, 4, 128]),
                                                  op=ALU.mult), ["ps", "tri"], ["AT%d" % m])

        def b_mix(i, m):
            p = i % 2
            kn = ["kin_a%d" % p, "kin_b%dlo" % p, "kin_b%dhi" % p]
            vn = ["v_a%d" % p, "v_b%d" % p][m]
            k_in = kin[p]; v_bf = vbf[p]
            for hh in range(4):
                h = 4 * m + hh
                S.op("pe", lambda e, h=h, hh=hh: e.matmul(po3[:, hh, :], lhsT=AT[m][:, hh, :], rhs=v_bf[:, h * 128:(h + 1) * 128],
                                                         start=True, stop=False), ["AT%d" % m, vn], ["po"])
                S.op("pe", lambda e, h=h, hh=hh: e.matmul(po3[:, hh, :], lhsT=q_inT[:, h, :], rhs=S_bf[:, h, :],
                                                         start=False, stop=True), ["qinT", "Sbf%d" % m], ["po"])
            for hh in range(4):
                h = 4 * m + hh
                S.op("pe", lambda e, h=h, hh=hh: e.matmul(pkv3[:, hh, :], lhsT=k_in[:, h * 128:(h + 1) * 128],
                                                         rhs=v_bf[:, h * 128:(h + 1) * 128], start=True, stop=True),
                     kn + [vn], ["pkv"])
            Sm = S32[:, 4 * m:4 * m + 4, :]
            eb4 = eblA[p][:, 0:4] if m == 0 else ebl[:, 4:8]
            ebn = "ebl0_%d" % p if m == 0 else "ebl1"
            S.op("dve", lambda e: e.tensor_tensor(out=Sm, in0=Sm, in1=pkv3, op=ALU.add), ["S32_%d" % m, "pkv"], ["S32_%d" % m])
            S.op("dve", lambda e: e.tensor_tensor(out=Sm, in0=Sm, in1=eb4.unsqueeze(2).broadcast_to([128, 4, 128]), op=ALU.mult),
                 ["S32_%d" % m, ebn], ["S32_%d" % m])
            S.op("pool", lambda e: e.tensor_copy(out=S_bf[:, 4 * m:4 * m + 4, :], in_=Sm), ["S32_%d" % m], ["Sbf%d" % m])
            for hh in range(4):
                S.op("dve", lambda e, hh=hh: e.bn_stats(out=ost[:, hh * 6:(hh + 1) * 6], in_=po3[:, hh, :]), ["po"], ["ost"])
            for hh in range(4):
                S.op("dve", lambda e, hh=hh: e.bn_aggr(out=omv[:, hh, :], in_=ost[:, hh * 6:(hh + 1) * 6]), ["ost"], ["omv"])
            on = a3(12 + m); onn = ["t%d" % (12 + m)]
            if m == 0:
                S.op("dve", lambda e: e.tensor_tensor(out=r4[:], in0=omv[:, :, 0], in1=omv[:, :, 0], op=ALU.mult), ["omv"], ["r4"])
                S.op("dve", lambda e: e.tensor_tensor(out=r4[:], in0=r4[:], in1=omv[:, :, 1], op=ALU.add), ["omv", "r4"], ["r4"])
                S.op("act", lambda e: e.activation(out=r4[:], in_=r4[:], func=AF.Ln, bias=EPS), ["r4"], ["r4"])
            else:
                S.op("act", lambda e: e.activation(out=r4[:], in_=omv[:, :, 1], func=AF.Ln, bias=EPS), ["omv"], ["r4"])
            S.op("act", lambda e: e.activation(out=r4[:], in_=r4[:], func=AF.Exp, scale=-0.5), ["r4"], ["r4"])
            r4b = r4[:].unsqueeze(2).broadcast_to([128, 4, 128])
            if m == 0:
                S.op("dve", lambda e: e.tensor_tensor(out=r3(on), in0=po3, in1=r4b, op=ALU.mult), ["po", "r4"], onn)
            else:
                S.op("dve", lambda e: e.tensor_tensor(out=r3(on), in0=po3, in1=omv[:, :, 0].unsqueeze(2).broadcast_to([128, 4, 128]),
                                                      op=ALU.subtract), ["po", "omv"], onn)
                S.op("dve", lambda e: e.tensor_tensor(out=r3(on), in0=r3(on), in1=r4b, op=ALU.mult), onn + ["r4"], onn)
            gt = [ga[p], gb[p]][m]; gtn = ["ga%d" % p, "gb%d" % p][m]
            S.op("pool", lambda e: e.tensor_tensor(out=on, in0=on, in1=normw[:, m * 512:(m + 1) * 512], op=ALU.mult),
                 onn + ["normw%d" % m], onn)
            S.op("pool", lambda e: e.tensor_tensor(out=o_fin[:, m * 512:(m + 1) * 512], in0=on, in1=gt, op=ALU.mult),
                 onn + [gtn], ["ofin%d" % m])

        x1p = a3(14, 2); x1n = a3n(14, 2)

        def b_out(i):
            p = i % 2; xs = x_sb[i % 3]; xn_ = ["x%d" % (i % 3)]
            transposes8(ptr, ["ptr"], o_fin, ["ofin0", "ofin1"])
            S.op("act", lambda e: e.activation(out=oT_sb[:], in_=ptr[:], func=AF.Copy), ["ptr"], ["oT"])
            for dc in range(2):
                for k in range(8):
                    S.op("pe", lambda e, k=k, dc=dc: e.matmul(pp[dc][:, :], lhsT=oT_sb[:, k, :], rhs=w_out_sb[:, k, dc * 512:(dc + 1) * 512],
                                                             start=(k == 0), stop=(k == 7)), ["oT", "w_out"], ["pp%d" % dc])
                S.op("dve", lambda e, dc=dc: e.scalar_tensor_tensor(out=x1p[:, dc * 512:(dc + 1) * 512], in0=xs[:, dc * 512:(dc + 1) * 512],
                                                                    scalar=ALPHA, in1=pp[dc][:, :], op0=ALU.mult, op1=ALU.add),
                     xn_ + ["pp%d" % dc], ["t%d" % (14 + dc)])
            ln_stats(x1p, x1n)
            S.op("dve", lambda e: e.tensor_scalar(out=x1p, in0=x1p, scalar1=mv[:, 0:1], scalar2=rs[:, 0:1],
                                                  op0=ALU.subtract, op1=ALU.mult), x1n + ["mv", "rs"], x1n)
            for (eng_, lo_, nm_) in (("dve", 0, "t14"), ("pool", 512, "t15")):
                S.op(eng_, lambda e, lo_=lo_: e.tensor_tensor(out=x1p[:, lo_:lo_ + 512], in0=x1p[:, lo_:lo_ + 512],
                                                               in1=l1w[:, lo_:lo_ + 512], op=ALU.mult), [nm_, "l1w"], [nm_])
                S.op(eng_, lambda e, lo_=lo_: e.tensor_tensor(out=x1p[:, lo_:lo_ + 512], in0=x1p[:, lo_:lo_ + 512],
                                                               in1=l1b[:, lo_:lo_ + 512], op=ALU.add), [nm_, "l1b"], [nm_])
            dst = out_d if stage == "mixer" else x1_d
            x1_stores.append(S.dma("pool", lambda e: e.dma_start(out=dst[i * 128:(i + 1) * 128, :], in_=x1p), "x1st",
                                   reads=x1n, writes=["x1_d%d" % i]))

        def b_h2a(i):
            if stage == "mixer":
                return
            ln_stats(x1p, x1n)
            xq = a3(12, 2)
            S.op("dve", lambda e: e.tensor_scalar(out=xq, in0=x1p, scalar1=mv[:, 0:1], scalar2=rs[:, 0:1],
                                                  op0=ALU.subtract, op1=ALU.mult), x1n + ["mv", "rs"], a3n(12, 2))
            for (eng_, lo_, nm_, hn_) in (("dve", 0, "t12", "h2_lo"), ("pool", 512, "t13", "h2_hi")):
                S.op(eng_, lambda e, lo_=lo_: e.tensor_tensor(out=xq[:, lo_:lo_ + 512], in0=xq[:, lo_:lo_ + 512],
                                                               in1=sc2_bc[:, lo_:lo_ + 512], op=ALU.mult), [nm_, "sc2bc0", "sc2bc1"], [nm_])
                S.op(eng_, lambda e, lo_=lo_: e.tensor_tensor(out=h2tok[:, lo_:lo_ + 512], in0=xq[:, lo_:lo_ + 512],
                                                               in1=sh2_bc[:, lo_:lo_ + 512], op=ALU.add),
                     [nm_, "sh2bc0", "sh2bc1"], [hn_])
            S.dma("pool", lambda e: e.dma_start(out=h2_d[i * 128:(i + 1) * 128, :], in_=h2tok[:]), "h2st",
                  reads=["h2_lo", "h2_hi"], writes=["h2_d%d" % i])

        def b_h2b(i):
            if stage == "mixer":
                return
            transposes8(ptr, ["ptr"], h2tok, ["h2_lo", "h2_hi"])
            S.op("act", lambda e: e.activation(out=h2T_t[:], in_=ptr[:], func=AF.Copy), ["ptr"], ["h2T"])
            for k in range(8):
                S.op("pe", lambda e, k=k: e.matmul(pb[:, 0:36], lhsT=h2T_t[:, k, :], rhs=wr_sb[:, k, :], start=(k == 0), stop=(k == 7)),
                     ["h2T", "wr_sb"], ["pb"])
            S.op("dve", lambda e: e.tensor_tensor(out=logits_all[:, i, :], in0=pb[:, 0:36], in1=br_bc[:], op=ALU.add),
                 ["pb", "br_bc"], ["logits"])

        x_load(0)
        if ntiles > 1: x_load(1)
        f_p1(0)
        for i in range(-1, ntiles + 1):
            if 2 <= i + 2 < ntiles: x_load(i + 2)
            nf = i + 1 if i + 1 < ntiles else None
            nn = i + 2 if 1 <= i + 2 < ntiles else None
            bk = i if 0 <= i < ntiles else None
            bh = i - 1 if 0 <= i - 1 < ntiles else None
            if bh is not None: b_h2a(bh)
            if nf is not None: f_p2(nf)
            if bk is not None: b_qk(bk)
            if bk is not None: b_sc(bk, 0); b_sc(bk, 1)
            if bh is not None: b_h2b(bh)
            if nf is not None: f_forget(nf); f_query(nf)
            if bk is not None: b_mix(bk, 0)
            if nf is not None: f_retq(nf); f_retk(nf)
            if bk is not None: b_mix(bk, 1)
            if nf is not None: f_values(nf)
            if bk is not None and stage == "full":
                for (src_, dst_, nm_) in ((wg_d, wgb_d, "cg"), (wu_d, wub_d, "cu"), (wd_d, wdb_d, "cd")):
                    S.dma("pool", lambda e, src_=src_, dst_=dst_, bk=bk: e.dma_start(out=dst_[bk * 256:(bk + 1) * 256, :],
                                                                                    in_=src_[bk * 256:(bk + 1) * 256, :]),
                          "wcast_" + nm_, writes=["%s_%d" % (nm_, bk)])
            if nn is not None: f_p1(nn)
            if bk is not None: b_out(bk)
            if stage == "full" and bk is not None and 4 <= bk < 12:
                zz = bk - 4
                S.dma("sp", lambda e, zz=zz: e.dma_start(out=xs_d[zz * 2048:(zz + 1) * 2048, :], in_=zeros_d[:, :]),
                      "xsz", writes=["xs_z%d" % zz])
            if nf is not None: f_rot(nf)
            if nf is not None: f_gates(nf)

        if stage == "mixer":
            if stop < 99:
                S.barrier()
                x1_stores.append(S.dma("sp", lambda e: e.dma_start(out=out_d[0:128, 0:128], in_=ident_f[:]), "dbg", reads=["ident_f"]))
            S.emit(block, final_waits=x1_stores)
            return nc

        S.barrier()

        l2w = a3(0, 2); l2b = a3(2, 2); g2bc = a3(4, 2)
        S.dma("sp", lambda e: e.dma_start(out=l2w, in_=l2w_d.partition_broadcast(128)), "l2w", writes=a3n(0, 2))
        S.dma("sp", lambda e: e.dma_start(out=l2b, in_=l2b_d.partition_broadcast(128)), "l2b", writes=a3n(2, 2))
        S.dma("sp", lambda e: e.dma_start(out=g2bc, in_=g2row_d.partition_broadcast(128)), "g2ld", reads=["g2row_d"], writes=a3n(4, 2))

        REG = {}

        def _pool_init(g):
            REG["xs"] = g.to_reg(16383)
            REG["w"] = g.to_reg(4095)
        S.init_pool = _pool_init

        def av(lo, n):
            return arena[:, lo:lo + n]
        NW = 3
        Wg = [av(w * 12288, 4096).rearrange("p (k n) -> p k n", k=8) for w in range(NW)]
        Wu = [av(w * 12288 + 4096, 4096).rearrange("p (k n) -> p k n", k=8) for w in range(NW)]
        Wd = [av(w * 12288 + 8192, 4096).rearrange("p (k n) -> p k n", k=4) for w in range(NW)]
        ztile = av(24576, 1024)
        h2ld = [av(25600 + w * 1024, 1024) for w in range(2)]
        Mb = av(27648, 1024)
        Ls_b = av(28672, 128); ones_b = av(28800, 128)
        a2b = arena2[:, 2048:8192].bitcast(BF16)
        xs_sb = [a2b[:, w * 1024:(w + 1) * 1024] for w in range(3)]
        xsT = [a2b[:, 3072 + w * 1024:3072 + (w + 1) * 1024].rearrange("p (k n) -> p k n", k=8) for w in range(2)]
        act_sb = [a2b[:, 5120 + w * 512:5120 + (w + 1) * 512] for w in range(2)]
        actT = [a2b[:, 6144 + w * 512:6144 + (w + 1) * 512].rearrange("p (k n) -> p k n", k=4) for w in range(2)]

        def b2(s_):
            return arena2[:, s_ * 1024:(s_ + 1) * 1024]

        def v3(ap):
            return ap.rearrange("p (j x) -> p j x", j=32)

        gl = logits_all[:, :, 0:4]
        el4 = logits_all[:, :, 4:36].rearrange("p j (g e) -> p j g e", g=4)
        gmax = gsm[:, 0, :]; gsum = gsm[:, 1, :]; pstar = gsm[:, 2, :]; v1 = gsm[:, 3, :]; v2 = gsm[:, 4, :]
        w1 = gsm[:, 5, :]; w2 = gsm[:, 6, :]
        gtmp = qf[:, 256:384].rearrange("p (a b) -> p a b", a=32); gm = qf[:, 384:512].rearrange("p (a b) -> p a b", a=32)
        elm = v3(a3(6, 2)); elmn = a3n(6, 2)
        mk1 = v3(a3(8, 2)); mk1n = a3n(8, 2)
        mk2 = v3(a3(10, 2)); mk2n = a3n(10, 2)
        elm4 = a3(6, 2).rearrange("p (j g e) -> p j g e", j=32, g=4)

        def bc32(ap, n):
            return ap.unsqueeze(2).broadcast_to([128, 32, n])

        S.op("dve", lambda e: e.tensor_reduce(out=gmax, in_=gl, axis=AX.X, op=ALU.max), ["logits"], ["gmax"])
        S.op("dve", lambda e: e.tensor_tensor(out=gtmp, in0=gl, in1=bc32(gmax, 4), op=ALU.subtract), ["logits", "gmax"], ["gtmp"])
        S.op("act", lambda e: e.activation(out=gtmp, in_=gtmp, func=AF.Exp), ["gtmp"], ["gtmp"])
        S.op("dve", lambda e: e.tensor_reduce(out=gsum, in_=gtmp, axis=AX.X, op=ALU.add), ["gtmp"], ["gsum"])
        S.op("dve", lambda e: e.reciprocal(out=pstar, in_=gsum), ["gsum"], ["pstar"])
        S.op("dve", lambda e: e.tensor_tensor(out=gm, in0=gl, in1=bc32(gmax, 4), op=ALU.is_equal), ["logits", "gmax"], ["gm"])
        S.op("dve", lambda e: e.tensor_tensor(out=elm4, in0=el4, in1=gm.unsqueeze(3).broadcast_to([128, 32, 4, 8]), op=ALU.mult),
             ["logits", "gm"], elmn)
        S.op("dve", lambda e: e.tensor_scalar(out=gtmp, in0=gm, scalar1=-1.0, scalar2=BIG, op0=ALU.add, op1=ALU.mult),
             ["gm"], ["gtmp"])
        S.op("dve", lambda e: e.tensor_tensor(out=elm4, in0=elm4, in1=gtmp.unsqueeze(3).broadcast_to([128, 32, 4, 8]), op=ALU.add),
             elmn + ["gtmp"], elmn)
        S.op("dve", lambda e: e.tensor_reduce(out=v1, in_=elm, axis=AX.X, op=ALU.max), elmn, ["v1"])
        S.op("dve", lambda e: e.tensor_tensor(out=mk1, in0=elm, in1=bc32(v1, 32), op=ALU.is_equal), elmn + ["v1"], mk1n)
        S.op("dve", lambda e: e.scalar_tensor_tensor(out=elm, in0=mk1, scalar=-BIG, in1=elm, op0=ALU.mult, op1=ALU.add),
             elmn + mk1n, elmn)
        S.op("dve", lambda e: e.tensor_reduce(out=v2, in_=elm, axis=AX.X, op=ALU.max), elmn, ["v2"])
        S.op("dve", lambda e: e.tensor_tensor(out=mk2, in0=elm, in1=bc32(v2, 32), op=ALU.is_equal), elmn + ["v2"], mk2n)
        S.op("dve", lambda e: e.tensor_tensor(out=w1, in0=v2, in1=v1, op=ALU.subtract), ["v1", "v2"], ["w1"])
        S.op("act", lambda e: e.activation(out=w1, in_=w1, func=AF.Exp), ["w1"], ["w1"])
        S.op("dve", lambda e: e.tensor_scalar_add(out=w1, in0=w1, scalar1=1.0), ["w1"], ["w1"])
        S.op("dve", lambda e: e.reciprocal(out=w1, in_=w1), ["w1"], ["w1"])
        S.op("dve", lambda e: e.tensor_scalar(out=w2, in0=w1, scalar1=-1.0, scalar2=1.0, op0=ALU.mult, op1=ALU.add), ["w1"], ["w2"])
        S.op("dve", lambda e: e.tensor_tensor(out=w1, in0=w1, in1=pstar, op=ALU.mult), ["w1", "pstar"], ["w1"])
        S.op("dve", lambda e: e.tensor_tensor(out=w2, in0=w2, in1=pstar, op=ALU.mult), ["w2", "pstar"], ["w2"])

        S.op("dve", lambda e: e.tensor_tensor(out=Ls_b, in0=tri[:], in1=ident_f[:], op=ALU.subtract), ["tri", "ident_f"], ["Ls_b"])
        S.op("pool", lambda e: e.memset(ones_b, 1.0), (), ["ones_b"])
        S.op("pool", lambda e: e.memset(ztile, 0.0), (), ["ztile"])
        S.op("dve", lambda e: e.tensor_tensor(out=v3(Mb), in0=mk1, in1=mk2, op=ALU.add), mk1n + mk2n, ["Mb"])
        for hf in range(2):
            S.op("pe", lambda e, hf=hf: e.matmul(pp[hf][:, :], lhsT=Ls_b, rhs=Mb[:, hf * 512:(hf + 1) * 512], start=True, stop=True),
                 ["Ls_b", "Mb"], ["pp%d" % hf])
        S.op("pe", lambda e: e.matmul(pb[:, :], lhsT=ones_b, rhs=Mb[:, 0:512], start=True, stop=True), ["ones_b", "Mb"], ["pb"])
        S.op("pe", lambda e: e.matmul(ps_[:, :], lhsT=ones_b, rhs=Mb[:, 512:1024], start=True, stop=True), ["ones_b", "Mb"], ["ps"])
        R1s = b2(2); CSs = b2(3); A = [b2(0), b2(1)]
        for hf in range(2):
            S.op("act", lambda e, hf=hf: e.activation(out=R1s[:, hf * 512:(hf + 1) * 512], in_=pp[hf][:, :], func=AF.Copy),
                 ["pp%d" % hf], ["b2_2"])
        def bn(x_):
            return ["b2_%d" % x_, "b2_%dlo" % x_]
        S.op("dve", lambda e: e.tensor_copy(out=A[0][:, 0:512], in_=pb[:, :]), ["pb"], ["b2_0lo"])
        S.op("dve", lambda e: e.tensor_copy(out=A[0][:, 512:1024], in_=ps_[:, :]), ["ps"], ["b2_0"])
        S.op("pool", lambda e: e.tensor_copy(out=CSs, in_=A[0]), bn(0), ["b2_3"])
        cur = 0
        for sft in (1, 2, 4, 8, 16):
            src = v3(A[cur]); dst = v3(A[1 - cur]); sn = bn(cur); dn = bn(1 - cur)
            S.op("pool", lambda e, src=src, dst=dst, sft=sft: e.tensor_copy(out=dst[:, 0:sft, :], in_=src[:, 0:sft, :]), sn, [dn[1]])
            S.op("dve", lambda e, src=src, dst=dst, sft=sft: e.tensor_tensor(out=dst[:, sft:32, :], in0=src[:, sft:32, :],
                                                                             in1=src[:, 0:32 - sft, :], op=ALU.add), sn, [dn[0]])
            cur = 1 - cur
        Iinc = A[cur]; In = bn(cur); oth = A[1 - cur]; on_ = bn(1 - cur)
        cnt = gsm[:, 7, :]
        nbk = gsm[:, 0, :]; pend = gsm[:, 1, :]; pst = gsm[:, 3, :]; tmp32 = gsm[:, 4, :]
        nbi = gm.rearrange("p a b -> p (a b)")[:, 0:32].bitcast(I32)
        S.op("dve", lambda e: e.tensor_scalar(out=cnt, in0=v3(Iinc)[:, 31, :], scalar1=255.0, scalar2=1.0 / 256.0,
                                              op0=ALU.add, op1=ALU.mult), In, ["cnt"])
        S.op("dve", lambda e: e.tensor_scalar_add(out=cnt, in0=cnt, scalar1=-0.498046875), ["cnt"], ["cnt"])
        S.op("dve", lambda e: e.tensor_copy(out=nbi, in_=cnt), ["cnt", "gm", "gmax", "gsum"], ["nbi"])
        S.op("dve", lambda e: e.tensor_copy(out=nbk, in_=nbi), ["nbi", "gmax"], ["nbk"])
        S.op("dve", lambda e: e.tensor_copy(out=pend, in_=nbk), ["nbk", "gsum"], ["pend"])
        pcur = pend; poth = tmp32; pcn = "pend"; pon = "tmp32"
        for sft in (1, 2, 4, 8, 16):
            S.op("dve", lambda e, pcur=pcur, poth=poth, sft=sft: e.tensor_copy(out=poth[:, 0:sft], in_=pcur[:, 0:sft]),
                 [pcn, "v2"], [pon])
            S.op("dve", lambda e, pcur=pcur, poth=poth, sft=sft: e.tensor_tensor(out=poth[:, sft:32], in0=pcur[:, sft:32],
                                                                                in1=pcur[:, 0:32 - sft], op=ALU.add), [pcn], [pon])
            pcur, poth = poth, pcur; pcn, pon = pon, pcn
        pendf = pcur; pendn = pcn
        S.op("dve", lambda e: e.tensor_tensor(out=pst, in0=pendf, in1=nbk, op=ALU.subtract), [pendn, "nbk", "v1"], ["pst"])
        S.op("dve", lambda e: e.tensor_single_scalar(out=pst, in_=pst, scalar=256.0, op=ALU.mult), ["pst"], ["pst"])
        dfull = v3(oth)
        S.op("dve", lambda e: e.tensor_tensor(out=oth, in0=Iinc, in1=CSs, op=ALU.subtract), In + ["b2_3"], on_)
        S.op("dve", lambda e: e.tensor_tensor(out=oth, in0=oth, in1=R1s, op=ALU.add), on_ + ["b2_2"], on_)
        S.op("dve", lambda e: e.tensor_tensor(out=dfull, in0=dfull, in1=pst.unsqueeze(1).broadcast_to([128, 32, 32]), op=ALU.add),
             on_ + ["pst"], on_)
        d12f = gm.rearrange("p a b -> p (a b)")[:, 32:96]
        dest_i = T("dest_i", [128, 64], I32)
        tmpm = v3(b2(4))
        for kk, (mk, mkn) in enumerate(((mk1, mk1n), (mk2, mk2n))):
            S.op("dve", lambda e, mk=mk: e.tensor_tensor(out=tmpm, in0=mk, in1=dfull, op=ALU.mult), mkn + on_, ["b2_4"])
            S.op("dve", lambda e, kk=kk: e.tensor_reduce(out=d12f[:, kk * 32:(kk + 1) * 32], in_=tmpm, axis=AX.X, op=ALU.add),
                 ["b2_4", "nbi"], ["d12f%d" % kk])
        S.op("dve", lambda e: e.tensor_copy(out=dest_i[:], in_=d12f), ["d12f0", "d12f1"], ["dest_i"])
        NBLK = 64
        cmp3 = arena2[:, 5 * 1024:7 * 1024].rearrange("p (b x) -> p b x", b=NBLK)
        bef = T("bef", [128, NBLK]); widx = T("widx", [128, 2, NBLK], I32)
        S.op("dve", lambda e: e.tensor_tensor(out=cmp3, in0=pendf.unsqueeze(1).broadcast_to([128, NBLK, 32]),
                                              in1=blkiota[:, 0:NBLK].unsqueeze(2).broadcast_to([128, NBLK, 32]), op=ALU.is_le),
             [pendn, "blkiota"], ["b2_5", "b2_6", "b2_7"])
        S.op("dve", lambda e: e.tensor_reduce(out=bef[:], in_=cmp3, axis=AX.X, op=ALU.add), ["b2_5", "b2_6", "b2_7"], ["bef"])
        S.op("dve", lambda e: e.tensor_scalar(out=bef[:], in0=bef[:], scalar1=128.0, scalar2=piota[:, 0:1], op0=ALU.mult, op1=ALU.add),
             ["bef", "piota"], ["bef"])
        S.op("dve", lambda e: e.tensor_copy(out=widx[:, 0, :], in_=bef[:]), ["bef"], ["widx0"])

        zn = ["xs_z%d" % zz for zz in range(8)]
        scat = []
        for j in range(NT):
            hb = h2ld[j % 2]; hbn = "h2ld%d" % (j % 2)
            S.dma("sp", lambda e, j=j, hb=hb: e.dma_start(out=hb, in_=h2_d[j * 128:(j + 1) * 128, :]), hbn,
                  reads=["h2_d%d" % j], writes=[hbn])
            for kk in range(2):
                S.dma("pool", lambda e, j=j, kk=kk, hb=hb: e.indirect_dma_start(
                    out=xs_d[:, :], out_offset=bass.IndirectOffsetOnAxis(ap=dest_i[:, kk * 32 + j:kk * 32 + j + 1], axis=0),
                    in_=hb, in_offset=None, bounds_check=REG["xs"], oob_is_err=False),
                    "scat%d_%d" % (j % 2, kk), reads=[hbn, "dest_i"] + zn, writes=["xs_s%d_%d" % (j, kk)])
        xsn = ["xs_s%d_%d" % (j, kk) for j in range(NT) for kk in range(2)]
        S.barrier()

        slt = [a3(12), a3(13)]
        ysb = [b2(0), b2(1)]
        pau = [(pp[0], "pp0", pp[1], "pp1"), (pb, "pb", ps_, "ps")]
        def gather_weights(blk):
            wb = blk % NW
            for (Wt, wsrc, nm) in ((Wg, wgb_d, "wg"), (Wu, wub_d, "wu"), (Wd, wdb_d, "wd")):
                Wflat = Wt[wb].rearrange("p k n -> p (k n)")
                S.dma("pool", lambda e, Wflat=Wflat, wsrc=wsrc, blk=blk: e.indirect_dma_start(
                    out=Wflat, out_offset=None, in_=wsrc.rearrange("(r two) n -> r (two n)", two=2),
                    in_offset=bass.IndirectOffsetOnAxis(ap=widx[:, 0, blk:blk + 1], axis=0),
                    bounds_check=REG["w"], oob_is_err=False),
                    "%s%d" % (nm, wb), reads=["widx0"], writes=["%s%d_0" % (nm, wb), "%s%d_1" % (nm, wb)])

        def xs_load(sl):
            xb = sl % 3
            S.dma("sp", lambda e: e.dma_start(out=xs_sb[xb], in_=xs_d[sl * 128:(sl + 1) * 128, :]),
                  "xsld%d" % xb, writes=["xs_sb%d" % xb])

        def stage_a1(sl):
            xb = sl % 3; sb = sl % 2
            transposes8(ptr, ["ptr"], xs_sb[xb], ["xs_sb%d" % xb])
            S.op("dve", lambda e: e.tensor_copy(out=xsT[sb], in_=ptr[:]), ["ptr"], ["xsT%d" % sb])

        def stage_a2(sl):
            wb = (sl // 2) % NW; sb = sl % 2
            pa_, pan_, pu_, pun_ = pau[sb]
            wgn = ["wg%d_0" % wb, "wg%d_1" % wb]; wun = ["wu%d_0" % wb, "wu%d_1" % wb]
            for k in range(8):
                S.op("pe", lambda e, k=k: e.matmul(pa_[:, :], lhsT=xsT[sb][:, k, :], rhs=Wg[wb][:, k, :],
                                                   start=(k == 0), stop=(k == 7)), ["xsT%d" % sb] + wgn, [pan_])
            for k in range(8):
                S.op("pe", lambda e, k=k: e.matmul(pu_[:, :], lhsT=xsT[sb][:, k, :], rhs=Wu[wb][:, k, :],
                                                   start=(k == 0), stop=(k == 7)), ["xsT%d" % sb] + wun, [pun_])

        def stage_b1(sl):
            sb = sl % 2
            pa_, pan_, pu_, pun_ = pau[sb]
            S.op("act", lambda e: e.activation(out=slt[sb], in_=pa_[:, :], func=AF.Silu), [pan_], ["t%d" % (12 + sb)])
            S.op("dve", lambda e: e.tensor_tensor(out=act_sb[sb], in0=slt[sb], in1=pu_[:, :], op=ALU.mult),
                 ["t%d" % (12 + sb), pun_], ["act_sb%d" % sb])
            for fc in range(4):
                S.op("pe", lambda e, fc=fc: e.transpose(out=ptq[:, fc, :], in_=act_sb[sb][:, fc * 128:(fc + 1) * 128],
                                                        identity=ident_b[:]), ["act_sb%d" % sb, "ident_b"], ["ptq"])
            S.op("act", lambda e: e.activation(out=actT[sb], in_=ptq[:, 0:4, :], func=AF.Copy), ["ptq"], ["actT%d" % sb])

        def stage_b2(sl):
            wb = (sl // 2) % NW; sb = sl % 2
            wdn = ["wd%d_0" % wb, "wd%d_1" % wb]
            for dc in range(2):
                pyb = [po, pkv][dc]; pyn_ = ["po", "pkv"][dc]
                for fc in range(4):
                    S.op("pe", lambda e, fc=fc, dc=dc, pyb=pyb: e.matmul(
                        pyb[:, :], lhsT=actT[sb][:, fc, :], rhs=Wd[wb][:, fc, dc * 512:(dc + 1) * 512],
                        start=(fc == 0), stop=(fc == 3)), ["actT%d" % sb] + wdn, [pyn_])
            S.op("act", lambda e: e.activation(out=ysb[sb][:, 0:512], in_=po[:, :], func=AF.Copy), ["po"], ["ysb%d_0" % sb])
            S.op("dve", lambda e: e.tensor_copy(out=ysb[sb][:, 512:1024], in_=pkv[:, :]), ["pkv"], ["ysb%d_1" % sb])
            S.dma("sp", lambda e: e.dma_start(out=ys_d[sl * 128:(sl + 1) * 128, :], in_=ysb[sb]),
                  "ysst%d" % sb, reads=["ysb%d_0" % sb, "ysb%d_1" % sb], writes=["ys_%d" % sl])

        NSUB = 2 * NBLK
        for bb in range(NW): gather_weights(bb)
        xs_load(0); xs_load(1)
        for it in range(NSUB + 3):
            if it + 2 < NSUB: xs_load(it + 2)
            if it < NSUB: stage_a1(it)
            if 0 <= it - 1 < NSUB: stage_a2(it - 1)
            if 0 <= it - 2 < NSUB: stage_b1(it - 2)
            if 0 <= it - 3 < NSUB: stage_b2(it - 3)
            if it >= 4 and (it - 4) % 2 == 0:
                nb_ = (it - 4) // 2 + NW
                if nb_ < NBLK: gather_weights(nb_)
        S.barrier()

        out_stores = []
        NB = 4
        arf = arena[:, 0:16384].bitcast(F32)

        def cbuf(j):
            pj = j % NB
            return (arf[:, (2 * pj) * 1024:(2 * pj + 1) * 1024], arf[:, (2 * pj + 1) * 1024:(2 * pj + 2) * 1024], b2(pj),
                    "cg%d_0" % pj, "cg%d_1" % pj, ["cx%d" % pj], pj)

        def c1(j):
            y1g, y2g, xt, y1n, y2n, xtn, pj = cbuf(j)
            for (yg, yn, kk) in ((y1g, y1n, 0), (y2g, y2n, 1)):
                S.dma("pool", lambda e, yg=yg, kk=kk: e.indirect_dma_start(
                    out=yg, out_offset=None, in_=ys_d[:, :],
                    in_offset=bass.IndirectOffsetOnAxis(ap=dest_i[:, kk * 32 + j:kk * 32 + j + 1], axis=0),
                    bounds_check=REG["xs"], oob_is_err=False), "yg%d_%d" % (pj, kk), reads=["dest_i"], writes=[yn])
            S.dma("sp", lambda e: e.dma_start(out=xt, in_=x1_d[j * 128:(j + 1) * 128, :]), "x1ld%d" % pj,
                  reads=["x1_d%d" % j], writes=xtn)

        def c2(j):
            y1g, y2g, xt, y1n, y2n, xtn, pj = cbuf(j)
            S.op("act", lambda e: e.activation(out=y1g, in_=y1g, func=AF.Copy, scale=w1[:, j:j + 1]), [y1n, "w1"], [y1n])
            S.op("dve", lambda e: e.scalar_tensor_tensor(out=y1g, in0=y2g, scalar=w2[:, j:j + 1], in1=y1g,
                                                         op0=ALU.mult, op1=ALU.add), [y1n, y2n, "w2"], [y1n])
            S.op("pool", lambda e: e.tensor_tensor(out=y1g, in0=y1g, in1=g2bc, op=ALU.mult), [y1n] + a3n(4, 2), [y1n])

        def c3(j):
            y1g, y2g, xt, y1n, y2n, xtn, pj = cbuf(j)
            S.op("dve", lambda e: e.scalar_tensor_tensor(out=xt, in0=xt, scalar=ALPHA, in1=y1g, op0=ALU.mult, op1=ALU.add),
                 xtn + [y1n], xtn)
            S.op("dve", lambda e: e.bn_stats(out=cst_[pj][:, 0:6], in_=xt[:, 0:512]), xtn, ["cst%d" % pj])
            S.op("dve", lambda e: e.bn_stats(out=cst_[pj][:, 6:12], in_=xt[:, 512:1024]), xtn, ["cst%d" % pj])
            S.op("dve", lambda e: e.bn_aggr(out=cmv_[pj][:], in_=cst_[pj][:]), ["cst%d" % pj], ["cmv%d" % pj])
            S.op("act", lambda e: e.activation(out=crs_[pj][:], in_=cmv_[pj][:, 1:2], func=AF.Ln, bias=EPS), ["cmv%d" % pj], ["crs%d" % pj])
            S.op("act", lambda e: e.activation(out=crs_[pj][:], in_=crs_[pj][:], func=AF.Exp, scale=-0.5), ["crs%d" % pj], ["crs%d" % pj])
            S.op("dve", lambda e: e.tensor_scalar(out=cnm_[pj][:], in0=cmv_[pj][:, 0:1], scalar1=-1.0, scalar2=crs_[pj][:, 0:1],
                                                  op0=ALU.mult, op1=ALU.mult), ["cmv%d" % pj, "crs%d" % pj], ["cnm%d" % pj])

        def c4(j):
            y1g, y2g, xt, y1n, y2n, xtn, pj = cbuf(j)
            S.op("act", lambda e: e.activation(out=xt, in_=xt, func=AF.Identity, scale=crs_[pj][:, 0:1], bias=cnm_[pj][:, 0:1]),
                 xtn + ["cnm%d" % pj, "crs%d" % pj], xtn)
            S.op("dve", lambda e: e.tensor_tensor(out=xt, in0=xt, in1=l2w, op=ALU.mult), xtn + a3n(0, 2), xtn)
            S.op("dve", lambda e: e.tensor_tensor(out=xt, in0=xt, in1=l2b, op=ALU.add), xtn + a3n(2, 2), xtn)
            out_stores.append(S.dma("sp", lambda e: e.dma_start(out=out_d[j * 128:(j + 1) * 128, :], in_=xt), "ost%d" % pj, reads=xtn))

        cst_ = [ost[:, 0:12], ost[:, 12:24], st[:, 0:12], T("cst3", [128, 12])]
        cmv_ = [omv[:, 0, :], omv[:, 1, :], omv[:, 2, :], omv[:, 3, :]]
        crs_ = [r4[:, 0:1], r4[:, 1:2], r4[:, 2:3], r4[:, 3:4]]
        cnm_ = [mv[:, 0:1], mv[:, 1:2], rs[:, 0:1], T("cnm3", [128, 1])]
        for it in range(NT + 3):
            if it < NT: c1(it)
            if 0 <= it - 1 < NT: c2(it - 1)
            if 0 <= it - 2 < NT: c3(it - 2)
            if 0 <= it - 3 < NT: c4(it - 3)
        S.emit(block, final_waits=out_stores)
    return nc


def _consts():
    ident = np.eye(128, dtype=np.float32)
    s = np.arange(128)
    tri = (s[:, None] <= s[None, :]).astype(np.float32)
    invf = np.power(np.float32(10000.0), -np.arange(0, 128, 2, dtype=np.float32) / np.float32(128)).astype(np.float32)[None, :]
    gam = 1.0 - np.exp2(-5.0 - np.arange(4, dtype=np.float64))
    lg = np.log(gam)
    p = np.arange(128, dtype=np.float64)[:, None] + 1.0
    qd = (np.exp(p * lg[None, :]) * (128.0 ** -0.5)).astype(np.float32)
    kd = np.exp(-p * lg[None, :]).astype(np.float32)
    ebr = np.broadcast_to(np.exp(128.0 * lg)[None, :], (128, 4)).astype(np.float32).copy()
    blkiota = np.broadcast_to(np.arange(96, dtype=np.float32)[None, :], (128, 96)).copy()
    piota = np.arange(128, dtype=np.float32)[:, None].copy()
    import ml_dtypes
    zeros_bf = np.zeros((2048, 1024), dtype=ml_dtypes.bfloat16)
    return dict(ident=ident, tri=tri, invf=invf, qd=qd, kd=kd, ebr=ebr, blkiota=blkiota, piota=piota, zeros_bf=zeros_bf)


def make_in_maps(inputs):
    f = lambda a: np.ascontiguousarray(np.asarray(a, dtype=np.float32))
    x = f(inputs["x"]); c = f(inputs["c"]); pos = np.ascontiguousarray(np.asarray(inputs["positions"], dtype=np.int32))
    shared = dict(
        w_ada=f(inputs["w_ada"][0]), b_ada=f(inputs["b_ada"][0])[None, :], w_in=f(inputs["w_in"][0]), w_out=f(inputs["w_out"][0]),
        hgrn_lb=f(inputs["hgrn_lb"]), hgrn_norm_w=f(inputs["hgrn_norm_w"][0])[None, :], ret_norm_w=f(inputs["ret_norm_w"][0])[None, :],
        post_ln1_w=f(inputs["post_ln1_w"][0])[None, :], post_ln1_b=f(inputs["post_ln1_b"][0])[None, :],
        post_ln2_w=f(inputs["post_ln2_w"][0])[None, :], post_ln2_b=f(inputs["post_ln2_b"][0])[None, :],
        w_r=np.ascontiguousarray(np.concatenate([f(inputs["w_rg"][0]), f(inputs["w_re"][0])], axis=1)),
        b_r=np.ascontiguousarray(np.concatenate([f(inputs["b_rg"][0]), f(inputs["b_re"][0])], axis=0))[None, :],
        w_gate=np.ascontiguousarray(f(inputs["w_gate"][0]).reshape(32, 8, 128, 512).transpose(0, 2, 1, 3)).reshape(8192, 2048),
        w_up=np.ascontiguousarray(f(inputs["w_up"][0]).reshape(32, 8, 128, 512).transpose(0, 2, 1, 3)).reshape(8192, 2048),
        w_down=np.ascontiguousarray(f(inputs["w_down"][0]).reshape(32, 4, 128, 1024).transpose(0, 2, 1, 3)).reshape(8192, 2048),
    )
    shared.update(_consts())
    maps = []
    for b in range(8):
        m = dict(shared)
        m["x"] = np.ascontiguousarray(x[b])
        m["ccol"] = np.ascontiguousarray(c[b].reshape(8, 128).T)
        m["pos"] = np.ascontiguousarray(pos[b].reshape(32, 128))
        maps.append(m)
    return maps


def kernel(**inputs):
    nc = build_program("full")
    maps = make_in_maps(inputs)
    res = run_bass_kernel_spmd(nc, maps, core_ids=list(range(8)))
    return np.stack([np.asarray(r["out"], dtype=np.float32) for r in res.results], axis=0)
```

```python
import math
import numpy as np
import concourse.bass as bass
import concourse.mybir as mybir
from concourse.bass_utils import run_bass_kernel_spmd
from contextlib import ExitStack

F32 = mybir.dt.float32; BF16 = mybir.dt.bfloat16; I32 = mybir.dt.int32
ALU = mybir.AluOpType; AF = mybir.ActivationFunctionType; AX = mybir.AxisListType

NT = 32
ALPHA = 2.0 ** 0.25
EPS = 1e-5
BIG = 1.0e30
TWO_PI = 2.0 * math.pi


class _Op:
    __slots__ = ("eng", "fn", "deps", "sig", "val", "dsem", "dval")

    def __init__(self, eng, fn):
        self.eng = eng; self.fn = fn; self.deps = []; self.sig = False
        self.val = 0; self.dsem = None; self.dval = 0


class Sched:
    ENGS = ("pe", "act", "dve", "pool", "sp")

    def __init__(self, nc, es):
        self.nc = nc; self.es = es
        self.ops = {e: [] for e in self.ENGS}
        self.lastw = {}; self.readers = {}
        self.dsems = {}; self.dcount = {}; self.dlast = {}
        self.barrier_ops = []

    def _deps(self, op, reads, writes):
        deps = list(self.barrier_ops)
        for r in reads:
            w = self.lastw.get(r)
            if w is not None: deps.append(w)
        for w_ in writes:
            w = self.lastw.get(w_)
            if w is not None: deps.append(w)
            deps.extend(self.readers.get(w_, ()))
        for r in reads:
            self.readers.setdefault(r, []).append(op)
        for w_ in writes:
            self.lastw[w_] = op; self.readers[w_] = []
        seen = set()
        for d in deps:
            if d is op or id(d) in seen: continue
            seen.add(id(d)); op.deps.append(d)
            if d.dsem is None and not (d.eng == "pe" and op.eng == "pe"): d.sig = True

    def op(self, eng, fn, reads=(), writes=()):
        o = _Op(eng, fn); self._deps(o, reads, writes); self.ops[eng].append(o); return o

    def dma(self, eng, fn, slot, reads=(), writes=()):
        o = _Op(eng, fn)
        if slot not in self.dsems:
            self.dsems[slot] = self.es.enter_context(self.nc.semaphore("d_" + slot)); self.dcount[slot] = 0
        self.dcount[slot] += 16
        o.dsem = self.dsems[slot]; o.dval = self.dcount[slot]
        self._deps(o, reads, writes); self.ops[eng].append(o); self.dlast[slot] = o; return o

    def barrier(self):
        b = []
        for e in self.ENGS:
            if self.ops[e]:
                o = self.ops[e][-1]
                if o.dsem is None: o.sig = True
                b.append(o)
        b.extend(self.dlast.values())
        self.barrier_ops = b

    def emit(self, block, final_waits=()):
        nc = self.nc
        cengs = ("pe", "act", "dve", "pool")
        sems = {e: self.es.enter_context(nc.semaphore("s_" + e)) for e in cengs}
        total = {}
        for e in self.ENGS:
            comp = [o for o in self.ops[e] if o.dsem is None]
            if comp: comp[-1].sig = True
            c = 0
            for o in self.ops[e]:
                if o.dsem is None and o.sig:
                    assert e in cengs
                    c += 1; o.val = c
            total[e] = c

        def run(engname, engobj):
            waited = {}
            init = getattr(self, "init_" + engname, None)
            if init is not None: init(engobj)
            for o in self.ops[engname]:
                need = {}
                for d in o.deps:
                    if d.dsem is not None:
                        key = ("d", id(d.dsem)); sem = d.dsem; v = d.dval
                    else:
                        if d.eng == "pe" and engname == "pe": continue
                        key = ("e", d.eng); sem = sems[d.eng]; v = d.val
                    if v > need.get(key, (None, 0))[1]: need[key] = (sem, v)
                for key, (sem, v) in need.items():
                    if waited.get(key, 0) >= v: continue
                    engobj.wait_ge(sem, v); waited[key] = v
                ins = o.fn(engobj)
                if o.dsem is not None: ins.then_inc(o.dsem, 16)
                elif o.sig: ins.then_inc(sems[engname], 1)
            for e2 in cengs:
                if e2 != engname and total[e2] > 0: engobj.wait_ge(sems[e2], total[e2])
            for slot, sem in self.dsems.items():
                engobj.wait_ge(sem, self.dcount[slot])

        final = list(final_waits)

        @block.tensor
        def _(t): run("pe", t)

        @block.scalar
        def _(a): run("act", a)

        @block.vector
        def _(v): run("dve", v)

        @block.gpsimd
        def _(g): run("pool", g)

        @block.sync
        def _(s):
            run("sp", s)
            last = {}
            for d in final:
                last[id(d.dsem)] = (d.dsem, max(last.get(id(d.dsem), (None, 0))[1], d.dval))
            for sem, v in last.values(): s.wait_ge(sem, v)


def build_program(stage="full", stop=99, ntiles=NT):
    nc = bass.Bass("TRN2", target_bir_lowering=False)

    def din(name, shape, dt=F32):
        return nc.dram_tensor(name, shape, dt, kind="ExternalInput").ap()

    x_d = din("x", [4096, 1024]); ccol_d = din("ccol", [128, 8]); pos_d = din("pos", [32, 128], I32)
    wada_d = din("w_ada", [1024, 6144]); bada_d = din("b_ada", [1, 6144])
    win_d = din("w_in", [1024, 4096]); wout_d = din("w_out", [1024, 1024])
    lb_d = din("hgrn_lb", [2, 512]); hnw_d = din("hgrn_norm_w", [1, 512]); rnw_d = din("ret_norm_w", [1, 512])
    l1w_d = din("post_ln1_w", [1, 1024]); l1b_d = din("post_ln1_b", [1, 1024])
    l2w_d = din("post_ln2_w", [1, 1024]); l2b_d = din("post_ln2_b", [1, 1024])
    wr_d = din("w_r", [1024, 36]); br_d = din("b_r", [1, 36])
    if stage == "full":
        wg_d = din("w_gate", [8192, 2048]); wu_d = din("w_up", [8192, 2048]); wd_d = din("w_down", [8192, 2048])
    blkiota_d = din("blkiota", [128, 96]); piota_d = din("piota", [128, 1])
    zeros_d = din("zeros_bf", [2048, 1024], BF16)
    ident_d = din("ident", [128, 128]); tri_d = din("tri", [128, 128]); invf_d = din("invf", [1, 64])
    qd_d = din("qd", [128, 4]); kd_d = din("kd", [128, 4]); ebr_d = din("ebr", [128, 4])
    out_d = nc.dram_tensor("out", [4096, 1024], F32, kind="ExternalOutput").ap()
    x1_d = nc.dram_tensor("x1_scr", [4096, 1024], F32).ap()
    h2_d = nc.dram_tensor("h2_scr", [4096, 1024], BF16).ap()
    xs_d = nc.dram_tensor("xs_scr", [16384, 1024], BF16).ap()
    ys_d = nc.dram_tensor("ys_scr", [16384, 1024], F32).ap()
    g2row_d = nc.dram_tensor("g2row_scr", [1, 1024], F32).ap()
    cs_d = nc.dram_tensor("cs_scr", [128, 2, 2048], F32).ap()
    wgb_d = nc.dram_tensor("wgb_scr", [8192, 2048], BF16).ap()
    wub_d = nc.dram_tensor("wub_scr", [8192, 2048], BF16).ap()
    wdb_d = nc.dram_tensor("wdb_scr", [8192, 2048], BF16).ap()

    with ExitStack() as es:
        S = Sched(nc, es)

        def T(name, shape, dt=F32):
            return es.enter_context(nc.sbuf_tensor("sb_" + name, shape, dt))

        def PS(name, shape, dt=F32):
            return es.enter_context(nc.psum_tensor("pm_" + name, shape, dt))

        arena = T("arena", [128, 40960], BF16)
        arena2 = T("arena2", [128, 8192], F32)
        arena3 = T("arena3", [128, 8192], F32)
        w_in_sb = arena[:, 0:32768].rearrange("p (k n) -> p k n", k=8)
        w_out_sb = arena[:, 32768:40960].rearrange("p (k n) -> p k n", k=8)
        cos_t = arena2[:, 0:2048].rearrange("p (j f) -> p j f", j=32)
        sin_t = arena2[:, 2048:4096].rearrange("p (j f) -> p j f", j=32)
        x_sb = [arena2[:, 4096:5120], arena2[:, 5120:6144], arena2[:, 7168:8192]]
        g1bc = arena2[:, 6144:7168]

        def a3(s, n=1):
            return arena3[:, s * 512:(s + n) * 512]

        def a3n(s, n=1):
            return ["t%d" % j for j in range(s, s + n)]

        ccol = T("ccol", [128, 8]); cact = T("cact", [128, 8])
        ident_f = T("ident_f", [128, 128]); ident_b = T("ident_b", [128, 128], BF16); tri = T("tri", [128, 128])
        ones_f = T("ones_f", [128, 128])
        invf_bc = T("invf_bc", [128, 64]); qd = T("qd", [128, 4]); kd = T("kd", [128, 4]); ebl = T("ebl", [128, 8])
        lb = T("lb", [128, 512]); oml = T("oml", [128, 512]); normw = T("normw", [128, 1024])
        l1w = T("l1w", [128, 1024]); l1b = T("l1b", [128, 1024])
        br_bc = T("br_bc", [128, 36]); wr_sb = T("wr_sb", [128, 8, 36], BF16)
        modcol = T("modcol", [128, 32])
        posi = T("posi", [32, 128], I32); posf = T("posf", [32, 128]); posT = T("posT", [128, 32])
        bch = [arena2[:, 4096:4608], arena2[:, 4608:5120]]
        mrow = [arena2[:, 7168:7680], arena2[:, 7680:8192]]
        sc2_bc = T("sc2_bc", [128, 1024]); sh2_bc = T("sh2_bc", [128, 1024])
        blkiota = T("blkiota", [128, 96]); piota = T("piota", [128, 1])
        st = T("st", [128, 12]); mv = T("mv", [128, 2]); rs = T("rs", [128, 1])
        ost = T("ost", [128, 24]); omv = T("omv", [128, 4, 2]); r4 = T("r4", [128, 4])
        xn_bf = T("xn_bf", [128, 1024], BF16); hT = T("hT", [128, 8, 128], BF16)
        q_in = T("q_in", [128, 1024], BF16)
        cs_t = [T("cs_t0", [128, 2, 64]), T("cs_t1", [128, 2, 64])]
        eblA = [T("eblA0", [128, 4]), T("eblA1", [128, 4])]
        q_inT = T("q_inT", [128, 8, 128], BF16); k_inT = T("k_inT", [128, 8, 128], BF16)
        qin = [arena2[:, p * 512:(p + 1) * 512].bitcast(BF16) for p in range(2)]
        kin = [arena2[:, 1024 + p * 512:1024 + (p + 1) * 512].bitcast(BF16) for p in range(2)]
        vbf = [arena2[:, 2048 + p * 512:2048 + (p + 1) * 512].bitcast(BF16) for p in range(2)]
        ga = [arena2[:, 3072 + p * 512:3072 + (p + 1) * 512] for p in range(2)]
        gb = [arena2[:, 6144 + p * 512:6144 + (p + 1) * 512] for p in range(2)]
        AT = [T("AT0", [128, 4, 128], BF16), T("AT1", [128, 4, 128], BF16)]
        S32 = T("S32", [128, 8, 128]); S_bf = T("S_bf", [128, 8, 128], BF16)
        o_fin = T("o_fin", [128, 1024], BF16); oT_sb = T("oT_sb", [128, 8, 128], BF16)
        h2T_t = T("h2T_t", [128, 8, 128], BF16)
        h2tok = T("h2tok", [128, 1024], BF16)
        logits_all = T("logits_all", [128, 32, 36])
        G_all = S32[:, :, :].rearrange("p a (b c) -> p (a b) c", b=4)
        qf = q_in[:, :].bitcast(F32)
        gsm = qf[:, 0:256].rearrange("p (a b) -> p a b", a=8)

        ptr = PS("ptr", [128, 8, 128], BF16); ptq = PS("ptq", [128, 8, 128], BF16)
        pp = [PS("pp0", [128, 512]), PS("pp1", [128, 512])]
        pb = PS("pb", [128, 512]); ps_ = PS("ps", [128, 512]); po = PS("po", [128, 512]); pkv = PS("pkv", [128, 512])
        ps3 = ps_[:, :].rearrange("p (h d) -> p h d", h=4)
        po3 = po[:, :].rearrange("p (h d) -> p h d", h=4)
        pkv3 = pkv[:, :].rearrange("p (h d) -> p h d", h=4)

        block = es.enter_context(nc.Block())

        def r3(ap, h=4):
            return ap.rearrange("p (h d) -> p h d", h=h)

        def ld(eng, dst, src, slot, w):
            return S.dma(eng, lambda e: e.dma_start(out=dst, in_=src), slot, writes=w)

        ld("sp", ccol[:], ccol_d[:, :], "c", ["ccol"])
        ld("sp", ident_f[:], ident_d[:, :], "identf", ["ident_f"])
        ld("sp", tri[:], tri_d[:, :], "tri", ["tri"])
        ld("sp", invf_bc[:], invf_d.partition_broadcast(128), "invf", ["invf"])
        ld("sp", qd[:], qd_d[:, :], "qd", ["qd"])
        ld("sp", kd[:], kd_d[:, :], "kd", ["kd"])
        ld("sp", ebl[:, 4:8], ebr_d[:, :], "ebr", ["ebl1"])
        ld("sp", lb[:], lb_d[0:1, :].partition_broadcast(128), "lbA", ["lb"])
        ld("sp", oml[:], lb_d[1:2, :].partition_broadcast(128), "lbB", ["oml"])
        ld("sp", normw[:, 0:512], hnw_d.partition_broadcast(128), "hnw", ["normw0"])
        ld("sp", normw[:, 512:1024], rnw_d.partition_broadcast(128), "rnw", ["normw1"])
        ld("sp", l1w[:], l1w_d.partition_broadcast(128), "l1w", ["l1w"])
        ld("sp", l1b[:], l1b_d.partition_broadcast(128), "l1b", ["l1b"])
        ld("sp", br_bc[:], br_d.partition_broadcast(128), "br", ["br_bc"])
        ld("sp", posi[:], pos_d[:, :], "pos", ["posi"])
        ld("sp", blkiota[:], blkiota_d[:, :], "blkiota", ["blkiota"])
        ld("sp", piota[:], piota_d[:, :], "piota", ["piota"])
        ld("pool", ident_b[:], ident_d[:, :], "identb", ["ident_b"])
        ld("pool", wr_sb[:], wr_d.rearrange("(k p) n -> p k n", p=128), "wr", ["wr_sb"])
        for j in range(4):
            ld("pool", w_in_sb[:, :, j * 1024:(j + 1) * 1024],
               win_d[:, j * 1024:(j + 1) * 1024].rearrange("(k p) n -> p k n", p=128), "win", ["w_in"])
        ld("pool", w_out_sb, wout_d.rearrange("(k p) n -> p k n", p=128), "wout", ["w_out"])

        S.op("dve", lambda e: e.memset(ones_f[:], 1.0), writes=["ones_f"])
        S.op("dve", lambda e: e.memset(S32[:], 0.0), writes=["S32_0", "S32_1"])
        S.op("pool", lambda e: e.memset(S_bf[:], 0.0), writes=["Sbf0", "Sbf1"])

        S.op("act", lambda e: e.activation(out=cact[:], in_=ccol[:], func=AF.Exp, scale=-1.0), ["ccol"], ["cact"])
        S.op("act", lambda e: e.activation(out=cact[:], in_=cact[:], func=AF.Ln, bias=1.0), ["cact"], ["cact"])
        S.op("act", lambda e: e.activation(out=cact[:], in_=cact[:], func=AF.Exp, scale=-1.0), ["cact"], ["cact"])
        S.op("dve", lambda e: e.tensor_tensor(out=cact[:], in0=cact[:], in1=ccol[:], op=ALU.mult), ["cact", "ccol"], ["cact"])

        S.op("dve", lambda e: e.tensor_tensor(out=lb[:], in0=lb[:], in1=oml[:], op=ALU.subtract), ["lb", "oml"], ["lb"])
        S.op("act", lambda e: e.activation(out=lb[:], in_=lb[:], func=AF.Exp, scale=-1.0), ["lb"], ["lb"])
        S.op("act", lambda e: e.activation(out=lb[:], in_=lb[:], func=AF.Ln, bias=1.0), ["lb"], ["lb"])
        S.op("act", lambda e: e.activation(out=lb[:], in_=lb[:], func=AF.Exp, scale=-1.0), ["lb"], ["lb"])
        S.op("dve", lambda e: e.tensor_scalar(out=oml[:], in0=lb[:], scalar1=-1.0, scalar2=1.0, op0=ALU.mult, op1=ALU.add),
             ["lb"], ["oml"])

        tmp4 = arena2[:, 1024:1536]
        for cg in range(12):
            par = cg % 2; eng = "dve"
            wst = arena3[:, par * 4096:(par + 1) * 4096].rearrange("p (k n) -> p k n", k=8)
            wreg = a3n(par * 8, 8)
            S.dma("sp", lambda e, wst=wst, cg=cg: e.dma_start(
                out=wst, in_=wada_d[:, cg * 512:(cg + 1) * 512].rearrange("(k p) n -> p k n", p=128)),
                "wada%d" % par, writes=wreg)
            S.dma("sp", lambda e, par=par, cg=cg: e.dma_start(out=bch[par], in_=bada_d[0:1, cg * 512:(cg + 1) * 512].partition_broadcast(128)),
                  "bada%d" % par, writes=["bch%d" % par])
            tmpw = arena2[:, par * 512:(par + 1) * 512]; tn = "tmpw%d" % par
            S.op(eng, lambda e, wst=wst, tmpw=tmpw: e.tensor_scalar_mul(out=tmpw, in0=wst[:, 0, :], scalar1=cact[:, 0:1]),
                 wreg + ["cact"], [tn])
            for k in range(1, 8):
                S.op(eng, lambda e, wst=wst, tmpw=tmpw, k=k: e.scalar_tensor_tensor(out=tmpw, in0=wst[:, k, :], scalar=cact[:, k:k + 1],
                                                                                  in1=tmpw, op0=ALU.mult, op1=ALU.add),
                     wreg + ["cact", tn], [tn])
            S.op("pe", lambda e, tmpw=tmpw: e.matmul(pb[:, :], lhsT=ones_f[:], rhs=tmpw, start=True, stop=True), [tn, "ones_f"], ["pb"])
            seg = cg // 2; half = cg % 2
            if seg in (2, 3, 4):
                dst = {2: g1bc, 3: sh2_bc, 4: sc2_bc}[seg][:, half * 512:(half + 1) * 512]
                dname = "%s%d" % ({2: "g1bc", 3: "sh2bc", 4: "sc2bc"}[seg], half)
            else:
                dst = mrow[par]; dname = "mrow%d" % par
            S.op("dve", lambda e, dst=dst, par=par: e.tensor_tensor(out=dst, in0=pb[:, :], in1=bch[par], op=ALU.add),
                 ["pb", "bch%d" % par], [dname])
            if seg in (1, 4):
                S.op("dve", lambda e, dst=dst: e.tensor_scalar_add(out=dst, in0=dst, scalar1=1.0), [dname], [dname])
            if seg in (0, 1):
                base = {0: 0, 1: 8}[seg] + half * 4
                S.op("dve", lambda e, dst=dst: e.tensor_tensor(out=r3(tmp4), in0=r3(dst), in1=ident_f[:].unsqueeze(1).broadcast_to([128, 4, 128]),
                                                               op=ALU.mult), [dname, "ident_f"], ["tmp4"])
                S.op("dve", lambda e, base=base: e.tensor_reduce(out=modcol[:, base:base + 4], in_=r3(tmp4), axis=AX.X, op=ALU.add),
                     ["tmp4"], ["modcol"])
            elif seg == 5:
                S.dma("sp", lambda e, par=par, half=half: e.dma_start(out=g2row_d[0:1, half * 512:(half + 1) * 512],
                                                                      in_=mrow[par][0:1, :]),
                      "g2st", reads=["mrow%d" % par], writes=["g2row_d"])
        for k in range(8):
            S.op("dve", lambda e, k=k: e.tensor_tensor(out=w_out_sb[:, k, :], in0=w_out_sb[:, k, :], in1=g1bc, op=ALU.mult),
                 ["w_out", "g1bc0", "g1bc1"], ["w_out"])

        S.op("dve", lambda e: e.tensor_copy(out=posf[:], in_=posi[:]), ["posi"], ["posf"])
        S.op("pe", lambda e: e.transpose(out=pb[:, 0:32], in_=posf[:], identity=ident_f[0:32, 0:32]), ["posf", "ident_f"], ["pb"])
        S.op("dve", lambda e: e.tensor_copy(out=posT[:], in_=pb[:, 0:32]), ["pb"], ["posT"])
        ang = a3(0, 4); yv = a3(4, 4); kf = a3(8, 4); kfm = a3(12, 4); ki = a3(12, 4).bitcast(I32)
        angn = a3n(0, 4); yvn = a3n(4, 4); kfn = a3n(8, 4); kin_ = a3n(12, 4)
        S.op("dve", lambda e: e.tensor_tensor(out=ang.rearrange("p (j f) -> p j f", j=32),
                                              in0=posT[:].unsqueeze(2).broadcast_to([128, 32, 64]),
                                              in1=invf_bc[:].unsqueeze(1).broadcast_to([128, 32, 64]), op=ALU.mult),
             ["posT", "invf"], angn)

        def sin_table(src, srcn, dst, dstn):
            S.op("dve", lambda e: e.tensor_single_scalar(out=kf, in_=src, scalar=1.0 / TWO_PI, op=ALU.mult), srcn, kfn)
            S.op("dve", lambda e: e.tensor_copy(out=ki, in_=kf), kfn, kin_)
            S.op("dve", lambda e: e.tensor_copy(out=kf, in_=ki), kin_, kfn)
            S.op("dve", lambda e: e.scalar_tensor_tensor(out=kf, in0=kf, scalar=-TWO_PI, in1=src, op0=ALU.mult, op1=ALU.add),
                 kfn + srcn, kfn)
            S.op("dve", lambda e: e.tensor_single_scalar(out=kfm, in_=kf, scalar=math.pi, op=ALU.is_gt), kfn, kin_)
            S.op("dve", lambda e: e.scalar_tensor_tensor(out=kf, in0=kfm, scalar=-TWO_PI, in1=kf, op0=ALU.mult, op1=ALU.add),
                 kfn + kin_, kfn)
            S.op("dve", lambda e: e.tensor_single_scalar(out=kfm, in_=kf, scalar=-math.pi, op=ALU.is_lt), kfn, kin_)
            S.op("dve", lambda e: e.scalar_tensor_tensor(out=kf, in0=kfm, scalar=TWO_PI, in1=kf, op0=ALU.mult, op1=ALU.add),
                 kfn + kin_, kfn)
            S.op("act", lambda e: e.activation(out=dst, in_=kf, func=AF.Sin), kfn, dstn)

        S.op("dve", lambda e: e.tensor_scalar_add(out=yv, in0=ang, scalar1=math.pi / 2), angn, yvn)
        sin_table(yv, yvn, arena2[:, 0:2048], ["cos"])
        sin_table(ang, angn, arena2[:, 2048:4096], ["sin"])
        S.dma("sp", lambda e: e.dma_start(out=cs_d[:, 0, :], in_=arena2[:, 0:2048]), "csst", reads=["cos"], writes=["cs_d0"])
        S.dma("sp", lambda e: e.dma_start(out=cs_d[:, 1, :], in_=arena2[:, 2048:4096]), "csst", reads=["sin"], writes=["cs_d1"])

        if stage == "setup":
            fin = []
            fin.append(S.dma("sp", lambda e: e.dma_start(out=out_d[0:128, 0:32], in_=modcol[:]), "dbg", reads=["modcol"]))
            fin.append(S.dma("sp", lambda e: e.dma_start(out=out_d[0:128, 32:96], in_=cos_t[:, 3, :]), "dbg", reads=["cos"]))
            fin.append(S.dma("sp", lambda e: e.dma_start(out=out_d[0:128, 96:160], in_=sin_t[:, 3, :]), "dbg", reads=["sin"]))
            fin.append(S.dma("sp", lambda e: e.dma_start(out=out_d[0:128, 160:672], in_=lb[:]), "dbg", reads=["lb"]))
            fin.append(S.dma("sp", lambda e: e.dma_start(out=out_d[128:256, 0:1024], in_=g1bc), "dbg", reads=["g1bc0", "g1bc1"]))
            S.emit(block, final_waits=fin)
            return nc

        S.barrier()

        def ln_stats(src, srcn):
            S.op("dve", lambda e: e.bn_stats(out=st[:, 0:6], in_=src[:, 0:512]), srcn, ["st"])
            S.op("dve", lambda e: e.bn_stats(out=st[:, 6:12], in_=src[:, 512:1024]), srcn, ["st"])
            S.op("dve", lambda e: e.bn_aggr(out=mv[:], in_=st[:]), ["st"], ["mv"])
            S.op("act", lambda e: e.activation(out=rs[:], in_=mv[:, 1:2], func=AF.Ln, bias=EPS), ["mv"], ["rs"])
            S.op("act", lambda e: e.activation(out=rs[:], in_=rs[:], func=AF.Exp, scale=-0.5), ["rs"], ["rs"])

        def transposes8(dst_ps, dstn, src, srcn):
            for c in range(8):
                S.op("pe", lambda e, c=c: e.transpose(out=dst_ps[:, c, :], in_=src[:, c * 128:(c + 1) * 128], identity=ident_b[:]),
                     srcn + ["ident_b"], dstn)

        def modulate(dst, dstn, sc0, sh0):
            mm = "act"
            for c in range(8):
                if (mm == "mix" and c % 2 == 0) or mm == "act":
                    S.op("act", lambda e, c=c: e.activation(out=dst[:, c, :], in_=ptr[:, c, :], func=AF.Identity,
                                                            scale=modcol[:, sc0 + c:sc0 + c + 1], bias=modcol[:, sh0 + c:sh0 + c + 1]),
                         ["ptr", "modcol"], [dstn + str(c)])
                else:
                    S.op("dve", lambda e, c=c: e.tensor_scalar(out=dst[:, c, :], in0=ptr[:, c, :],
                                                               scalar1=modcol[:, sc0 + c:sc0 + c + 1],
                                                               scalar2=modcol[:, sh0 + c:sh0 + c + 1], op0=ALU.mult, op1=ALU.add),
                         ["ptr", "modcol"], [dstn + str(c)])

        def proj(g, bank):
            for k in range(8):
                S.op("pe", lambda e, k=k: e.matmul(pp[bank][:, :], lhsT=hT[:, k, :], rhs=w_in_sb[:, k, g * 512:(g + 1) * 512],
                                                   start=(k == 0), stop=(k == 7)),
                     ["hT%d" % c for c in range(8)] + ["w_in"], ["pp%d" % bank])

        def sigmoid_from_psum(bank, dst, dstn):
            S.op("act", lambda e: e.activation(out=dst, in_=pp[bank][:, :], func=AF.Exp, scale=-1.0), ["pp%d" % bank], dstn)
            S.op("act", lambda e: e.activation(out=dst, in_=dst, func=AF.Ln, bias=1.0), dstn, dstn)
            S.op("act", lambda e: e.activation(out=dst, in_=dst, func=AF.Exp, scale=-1.0), dstn, dstn)

        def rotary(i, src, srcn, dst, dstn, dec, decn):
            s3 = r3(src); x1 = s3[:, :, 0:64]; x2 = s3[:, :, 64:128]
            cst = cs_t[i % 2]
            cosb = cst[:, 0, :].unsqueeze(1).broadcast_to([128, 4, 64])
            sinb = cst[:, 1, :].unsqueeze(1).broadcast_to([128, 4, 64])
            A = r3(a3(10)[:, 0:256]); B = r3(a3(10)[:, 256:512]); C = r3(a3(11)[:, 0:256]); Dd = r3(a3(11)[:, 256:512])
            decb = dec[:, 0:4].unsqueeze(2).broadcast_to([128, 4, 64])
            d3 = r3(dst)
            S.op("pool", lambda e: e.tensor_tensor(out=A, in0=x1, in1=cosb, op=ALU.mult), srcn + ["cs%d" % (i % 2)], ["t10a"])
            S.op("pool", lambda e: e.tensor_tensor(out=B, in0=x2, in1=sinb, op=ALU.mult), srcn + ["cs%d" % (i % 2)], ["t10b"])
            S.op("pool", lambda e: e.tensor_tensor(out=A, in0=A, in1=B, op=ALU.subtract), ["t10a", "t10b"], ["t10a"])
            S.op("dve", lambda e: e.tensor_tensor(out=C, in0=x1, in1=sinb, op=ALU.mult), srcn + ["cs%d" % (i % 2)], ["t11a"])
            S.op("dve", lambda e: e.tensor_tensor(out=Dd, in0=x2, in1=cosb, op=ALU.mult), srcn + ["cs%d" % (i % 2)], ["t11b"])
            S.op("dve", lambda e: e.tensor_tensor(out=C, in0=C, in1=Dd, op=ALU.add), ["t11a", "t11b"], ["t11a"])
            S.op("pool", lambda e: e.tensor_tensor(out=d3[:, :, 0:64], in0=A, in1=decb, op=ALU.mult), ["t10a", decn], [dstn + "lo"])
            S.op("dve", lambda e: e.tensor_tensor(out=d3[:, :, 64:128], in0=C, in1=decb, op=ALU.mult), ["t11a", decn], [dstn + "hi"])

        x1_stores = []
        t0 = a3(0); t1 = a3(1); t2 = a3(2); t3 = a3(3); t4 = a3(4); t5 = a3(5); t8 = a3(8); t9 = a3(9)

        def x_load(i):
            p3 = i % 3; xs = x_sb[p3]
            S.dma("sp", lambda e: e.dma_start(out=xs, in_=x_d[i * 128:(i + 1) * 128, :]), "x%d" % p3, writes=["x%d" % p3])

        def f_p1(i):
            p = i % 2; xs = x_sb[i % 3]; xn_ = ["x%d" % (i % 3)]
            ln_stats(xs, xn_)
            S.op("dve", lambda e: e.tensor_scalar(out=xn_bf[:], in0=xs, scalar1=mv[:, 0:1], scalar2=rs[:, 0:1],
                                                  op0=ALU.subtract, op1=ALU.mult), xn_ + ["mv", "rs"], ["xn_lo", "xn_hi"])

        def f_p2(i):
            p = i % 2
            S.dma("sp", lambda e: e.dma_start(out=cs_t[p][:], in_=cs_d[:, :, i * 64:(i + 1) * 64]), "cs%d" % p,
                  reads=["cs_d0", "cs_d1"], writes=["cs%d" % p])
            transposes8(ptr, ["ptr"], xn_bf, ["xn_lo", "xn_hi"])
            modulate(hT, "hT", 8, 0)

        def f_forget(i):
            p = i % 2
            proj(1, 0)
            proj(0, 1)
            sigmoid_from_psum(0, t0, ["t0"])
            S.op("dve", lambda e: e.tensor_tensor(out=t0, in0=t0, in1=oml[:], op=ALU.mult), ["t0", "oml"], ["t0"])
            S.op("pool", lambda e: e.tensor_tensor(out=t0, in0=t0, in1=lb[:], op=ALU.add), ["t0", "lb"], ["t0"])
            S.op("act", lambda e: e.activation(out=t1, in_=t0, func=AF.Ln), ["t0"], ["t1"])
            S.op("pool", lambda e: e.tensor_scalar(out=t2, in0=t0, scalar1=-1.0, scalar2=1.0, op0=ALU.mult, op1=ALU.add),
                 ["t0"], ["t2"])
            S.op("pe", lambda e: e.matmul(pb[:, :], lhsT=tri[:], rhs=t1, start=True, stop=True), ["tri", "t1"], ["pb"])
            S.op("act", lambda e: e.activation(out=t3, in_=pb[:, :], func=AF.Exp), ["pb"], ["t3"])
            S.op("act", lambda e: e.activation(out=t4, in_=pb[:, :], func=AF.Exp, scale=-1.0), ["pb"], ["t4"])
            for h in range(4):
                S.op("pe", lambda e, h=h: e.matmul(pb[:, h:h + 1], lhsT=t1[:, h * 128:(h + 1) * 128], rhs=ones_f[:, 0:1],
                                                   start=True, stop=True), ["t1", "ones_f"], ["pb"])
            S.op("act", lambda e: e.activation(out=eblA[p][:], in_=pb[:, 0:4], func=AF.Exp), ["pb"], ["ebl0_%d" % p])
            S.op("pool", lambda e: e.tensor_tensor(out=kin[p][:, 0:512], in0=t2, in1=t4, op=ALU.mult), ["t2", "t4"], ["kin_a%d" % p])

        def f_query(i):
            p = i % 2
            sigmoid_from_psum(1, t5, ["t5"])
            S.op("dve", lambda e: e.tensor_tensor(out=t5, in0=pp[1][:, :], in1=t5, op=ALU.mult), ["pp1", "t5"], ["t5"])
            S.op("dve", lambda e: e.tensor_tensor(out=qin[p][:, 0:512], in0=t5, in1=t3, op=ALU.mult), ["t5", "t3"], ["qin_a%d" % p])

        def f_retq(i):
            proj(4, 0)
            S.op("act", lambda e: e.activation(out=t8, in_=pp[0][:, :], func=AF.Copy), ["pp0"], ["t8"])

        def f_retk(i):
            proj(5, 1)
            S.op("act", lambda e: e.activation(out=t9, in_=pp[1][:, :], func=AF.Copy), ["pp1"], ["t9"])

        def f_rot(i):
            p = i % 2
            rotary(i, t8, ["t8"], qin[p][:, 512:1024], "qin_b%d" % p, qd, "qd")
            rotary(i, t9, ["t9"], kin[p][:, 512:1024], "kin_b%d" % p, kd, "kd")

        def f_values(i):
            p = i % 2
            proj(2, 0)
            S.op("act", lambda e: e.activation(out=vbf[p][:, 0:512], in_=pp[0][:, :], func=AF.Copy), ["pp0"], ["v_a%d" % p])
            proj(6, 1)
            S.op("act", lambda e: e.activation(out=vbf[p][:, 512:1024], in_=pp[1][:, :], func=AF.Copy), ["pp1"], ["v_b%d" % p])

        def f_gates(i):
            p = i % 2
            proj(3, 0)
            sigmoid_from_psum(0, ga[p], ["ga%d" % p])
            S.op("dve", lambda e: e.tensor_tensor(out=ga[p], in0=pp[0][:, :], in1=ga[p], op=ALU.mult), ["pp0", "ga%d" % p], ["ga%d" % p])
            proj(7, 1)
            sigmoid_from_psum(1, gb[p], ["gb%d" % p])
            S.op("dve", lambda e: e.tensor_tensor(out=gb[p], in0=pp[1][:, :], in1=gb[p], op=ALU.mult), ["pp1", "gb%d" % p], ["gb%d" % p])

        def b_qk(i, m):
            p = i % 2
            if m == 0:
                qn = ["qin_a%d" % p]; kn_ = ["kin_a%d" % p]
            else:
                qn = ["qin_b%dlo" % p, "qin_b%dhi" % p]; kn_ = ["kin_b%dlo" % p, "kin_b%dhi" % p]
            for hh in range(4):
                S.op("pe", lambda e, hh=hh: e.transpose(out=ptq[:, hh, :], in_=qin[p][:, (4 * m + hh) * 128:(4 * m + hh + 1) * 128],
                                                        identity=ident_b[:]), qn + ["ident_b"], ["ptq"])
            for hh in range(4):
                S.op("pe", lambda e, hh=hh: e.transpose(out=ptq[:, 4 + hh, :], in_=kin[p][:, (4 * m + hh) * 128:(4 * m + hh + 1) * 128],
                                                        identity=ident_b[:]), kn_ + ["ident_b"], ["ptq"])
            S.op("act", lambda e: e.activation(out=q_inT[:, 4 * m:4 * m + 4, :], in_=ptq[:, 0:4, :], func=AF.Copy), ["ptq"], ["qinT_%d" % m])
            S.op("act", lambda e: e.activation(out=k_inT[:, 4 * m:4 * m + 4, :], in_=ptq[:, 4:8, :], func=AF.Copy), ["ptq"], ["kinT_%d" % m])

        def b_sc(i, m):
            for hh in range(4):
                h = 4 * m + hh
                S.op("pe", lambda e, h=h, hh=hh: e.matmul(ps3[:, hh, :], lhsT=k_inT[:, h, :], rhs=q_inT[:, h, :],
                                                         start=True, stop=True), ["kinT_%d" % m, "qinT_%d" % m], ["ps"])
            S.op("dve", lambda e: e.tensor_tensor(out=AT[m][:], in0=ps3, in1=tri[:].unsqueeze(1).broadcast_to([128, 4, 128]),
                                                  op=ALU.mult), ["ps", "tri"], ["AT%d" % m])

        def b_mix(i, m):
            p = i % 2
            kn = ["kin_a%d" % p, "kin_b%dlo" % p, "kin_b%dhi" % p]
            vn = ["v_a%d" % p, "v_b%d" % p][m]
            k_in = kin[p]; v_bf = vbf[p]
            for hh in range(4):
                h = 4 * m + hh
                S.op("pe", lambda e, h=h, hh=hh: e.matmul(po3[:, hh, :], lhsT=AT[m][:, hh, :], rhs=v_bf[:, h * 128:(h + 1) * 128],
                                                         start=True, stop=False), ["AT%d" % m, vn], ["po"])
                S.op("pe", lambda e, h=h, hh=hh: e.matmul(po3[:, hh, :], lhsT=q_inT[:, h, :], rhs=S_bf[:, h, :],
                                                         start=False, stop=True), ["qinT_%d" % m, "Sbf%d" % m], ["po"])
            for hh in range(4):
                h = 4 * m + hh
                S.op("pe", lambda e, h=h, hh=hh: e.matmul(pkv3[:, hh, :], lhsT=k_in[:, h * 128:(h + 1) * 128],
                                                         rhs=v_bf[:, h * 128:(h + 1) * 128], start=True, stop=True),
                     kn + [vn], ["pkv"])
            Sm = S32[:, 4 * m:4 * m + 4, :]
            eb4 = eblA[p][:, 0:4] if m == 0 else ebl[:, 4:8]
            ebn = "ebl0_%d" % p if m == 0 else "ebl1"
            S.op("dve", lambda e: e.tensor_tensor(out=Sm, in0=Sm, in1=pkv3, op=ALU.add), ["S32_%d" % m, "pkv"], ["S32_%d" % m])
            S.op("dve", lambda e: e.tensor_tensor(out=Sm, in0=Sm, in1=eb4.unsqueeze(2).broadcast_to([128, 4, 128]), op=ALU.mult),
                 ["S32_%d" % m, ebn], ["S32_%d" % m])
            S.op("pool", lambda e: e.tensor_copy(out=S_bf[:, 4 * m:4 * m + 4, :], in_=Sm), ["S32_%d" % m], ["Sbf%d" % m])
            for hh in range(4):
                S.op("dve", lambda e, hh=hh: e.bn_stats(out=ost[:, hh * 6:(hh + 1) * 6], in_=po3[:, hh, :]), ["po"], ["ost"])
            for hh in range(4):
                S.op("dve", lambda e, hh=hh: e.bn_aggr(out=omv[:, hh, :], in_=ost[:, hh * 6:(hh + 1) * 6]), ["ost"], ["omv"])
            on = a3(12 + m); onn = ["t%d" % (12 + m)]
            if m == 0:
                S.op("dve", lambda e: e.tensor_tensor(out=r4[:], in0=omv[:, :, 0], in1=omv[:, :, 0], op=ALU.mult), ["omv"], ["r4"])
                S.op("dve", lambda e: e.tensor_tensor(out=r4[:], in0=r4[:], in1=omv[:, :, 1], op=ALU.add), ["omv", "r4"], ["r4"])
                S.op("act", lambda e: e.activation(out=r4[:], in_=r4[:], func=AF.Ln, bias=EPS), ["r4"], ["r4"])
            else:
                S.op("act", lambda e: e.activation(out=r4[:], in_=omv[:, :, 1], func=AF.Ln, bias=EPS), ["omv"], ["r4"])
            S.op("act", lambda e: e.activation(out=r4[:], in_=r4[:], func=AF.Exp, scale=-0.5), ["r4"], ["r4"])
            r4b = r4[:].unsqueeze(2).broadcast_to([128, 4, 128])
            if m == 0:
                S.op("dve", lambda e: e.tensor_tensor(out=r3(on), in0=po3, in1=r4b, op=ALU.mult), ["po", "r4"], onn)
            else:
                S.op("dve", lambda e: e.tensor_tensor(out=r3(on), in0=po3, in1=omv[:, :, 0].unsqueeze(2).broadcast_to([128, 4, 128]),
                                                      op=ALU.subtract), ["po", "omv"], onn)
                S.op("dve", lambda e: e.tensor_tensor(out=r3(on), in0=r3(on), in1=r4b, op=ALU.mult), onn + ["r4"], onn)
            gt = [ga[p], gb[p]][m]; gtn = ["ga%d" % p, "gb%d" % p][m]
            S.op("pool", lambda e: e.tensor_tensor(out=on, in0=on, in1=normw[:, m * 512:(m + 1) * 512], op=ALU.mult),
                 onn + ["normw%d" % m], onn)
            S.op("pool", lambda e: e.tensor_tensor(out=o_fin[:, m * 512:(m + 1) * 512], in0=on, in1=gt, op=ALU.mult),
                 onn + [gtn], ["ofin%d" % m])

        x1p = a3(14, 2); x1n = a3n(14, 2)

        def b_out(i):
            p = i % 2; xs = x_sb[i % 3]; xn_ = ["x%d" % (i % 3)]
            transposes8(ptr, ["ptr"], o_fin, ["ofin0", "ofin1"])
            S.op("act", lambda e: e.activation(out=oT_sb[:], in_=ptr[:], func=AF.Copy), ["ptr"], ["oT"])
            for dc in range(2):
                for k in range(8):
                    S.op("pe", lambda e, k=k, dc=dc: e.matmul(pp[dc][:, :], lhsT=oT_sb[:, k, :], rhs=w_out_sb[:, k, dc * 512:(dc + 1) * 512],
                                                             start=(k == 0), stop=(k == 7)), ["oT", "w_out"], ["pp%d" % dc])
                S.op("dve", lambda e, dc=dc: e.scalar_tensor_tensor(out=x1p[:, dc * 512:(dc + 1) * 512], in0=xs[:, dc * 512:(dc + 1) * 512],
                                                                    scalar=ALPHA, in1=pp[dc][:, :], op0=ALU.mult, op1=ALU.add),
                     xn_ + ["pp%d" % dc], ["t%d" % (14 + dc)])
            ln_stats(x1p, x1n)
            S.op("dve", lambda e: e.tensor_scalar(out=x1p, in0=x1p, scalar1=mv[:, 0:1], scalar2=rs[:, 0:1],
                                                  op0=ALU.subtract, op1=ALU.mult), x1n + ["mv", "rs"], x1n)
            for (eng_, lo_, nm_) in (("dve", 0, "t14"), ("pool", 512, "t15")):
                S.op(eng_, lambda e, lo_=lo_: e.tensor_tensor(out=x1p[:, lo_:lo_ + 512], in0=x1p[:, lo_:lo_ + 512],
                                                               in1=l1w[:, lo_:lo_ + 512], op=ALU.mult), [nm_, "l1w"], [nm_])
                S.op(eng_, lambda e, lo_=lo_: e.tensor_tensor(out=x1p[:, lo_:lo_ + 512], in0=x1p[:, lo_:lo_ + 512],
                                                               in1=l1b[:, lo_:lo_ + 512], op=ALU.add), [nm_, "l1b"], [nm_])
            dst = out_d if stage == "mixer" else x1_d
            x1_stores.append(S.dma("pool", lambda e: e.dma_start(out=dst[i * 128:(i + 1) * 128, :], in_=x1p), "x1st",
                                   reads=x1n, writes=["x1_d%d" % i]))

        def b_h2a(i):
            if stage == "mixer":
                return
            ln_stats(x1p, x1n)
            xq = a3(12, 2)
            S.op("dve", lambda e: e.tensor_scalar(out=xq, in0=x1p, scalar1=mv[:, 0:1], scalar2=rs[:, 0:1],
                                                  op0=ALU.subtract, op1=ALU.mult), x1n + ["mv", "rs"], a3n(12, 2))
            for (eng_, lo_, nm_, hn_) in (("dve", 0, "t12", "h2_lo"), ("pool", 512, "t13", "h2_hi")):
                S.op(eng_, lambda e, lo_=lo_: e.tensor_tensor(out=xq[:, lo_:lo_ + 512], in0=xq[:, lo_:lo_ + 512],
                                                               in1=sc2_bc[:, lo_:lo_ + 512], op=ALU.mult), [nm_, "sc2bc0", "sc2bc1"], [nm_])
                S.op(eng_, lambda e, lo_=lo_: e.tensor_tensor(out=h2tok[:, lo_:lo_ + 512], in0=xq[:, lo_:lo_ + 512],
                                                               in1=sh2_bc[:, lo_:lo_ + 512], op=ALU.add),
                     [nm_, "sh2bc0", "sh2bc1"], [hn_])
            S.dma("pool", lambda e: e.dma_start(out=h2_d[i * 128:(i + 1) * 128, :], in_=h2tok[:]), "h2st",
                  reads=["h2_lo", "h2_hi"], writes=["h2_d%d" % i])

        def b_h2b(i):
            if stage == "mixer":
                return
            transposes8(ptr, ["ptr"], h2tok, ["h2_lo", "h2_hi"])
            S.op("act", lambda e: e.activation(out=h2T_t[:], in_=ptr[:], func=AF.Copy), ["ptr"], ["h2T"])
            for k in range(8):
                S.op("pe", lambda e, k=k: e.matmul(pb[:, 0:36], lhsT=h2T_t[:, k, :], rhs=wr_sb[:, k, :], start=(k == 0), stop=(k == 7)),
                     ["h2T", "wr_sb"], ["pb"])
            S.op("dve", lambda e: e.tensor_tensor(out=logits_all[:, i, :], in0=pb[:, 0:36], in1=br_bc[:], op=ALU.add),
                 ["pb", "br_bc"], ["logits"])

        x_load(0)
        if ntiles > 1: x_load(1)
        f_p1(0)
        for i in range(-1, ntiles + 1):
            if 2 <= i + 2 < ntiles: x_load(i + 2)
            nf = i + 1 if i + 1 < ntiles else None
            nn = i + 2 if 1 <= i + 2 < ntiles else None
            bk = i if 0 <= i < ntiles else None
            bh = i - 1 if 0 <= i - 1 < ntiles else None
            if bh is not None: b_h2a(bh)
            if nf is not None: f_p2(nf)
            if bk is not None: b_qk(bk, 0); b_sc(bk, 0)
            if bh is not None: b_h2b(bh)
            if nf is not None: f_forget(nf); f_query(nf)
            if bk is not None: b_mix(bk, 0)
            if bk is not None: b_qk(bk, 1); b_sc(bk, 1)
            if nf is not None: f_retq(nf); f_retk(nf)
            if bk is not None: b_mix(bk, 1)
            if nf is not None: f_values(nf)
            if bk is not None and stage == "full":
                for (src_, dst_, nm_) in ((wg_d, wgb_d, "cg"), (wu_d, wub_d, "cu"), (wd_d, wdb_d, "cd")):
                    S.dma("pool", lambda e, src_=src_, dst_=dst_, bk=bk: e.dma_start(out=dst_[bk * 256:(bk + 1) * 256, :],
                                                                                    in_=src_[bk * 256:(bk + 1) * 256, :]),
                          "wcast_" + nm_, writes=["%s_%d" % (nm_, bk)])
            if nn is not None: f_p1(nn)
            if bk is not None: b_out(bk)
            if stage == "full" and bk is not None and 4 <= bk < 12:
                zz = bk - 4
                S.dma("sp", lambda e, zz=zz: e.dma_start(out=xs_d[zz * 2048:(zz + 1) * 2048, :], in_=zeros_d[:, :]),
                      "xsz", writes=["xs_z%d" % zz])
            if nf is not None: f_rot(nf)
            if nf is not None: f_gates(nf)

        if stage == "mixer":
            if stop < 99:
                S.barrier()
                x1_stores.append(S.dma("sp", lambda e: e.dma_start(out=out_d[0:128, 0:128], in_=ident_f[:]), "dbg", reads=["ident_f"]))
            S.emit(block, final_waits=x1_stores)
            return nc

        S.barrier()

        l2w = a3(0, 2); l2b = a3(2, 2); g2bc = a3(4, 2)
        S.dma("sp", lambda e: e.dma_start(out=l2w, in_=l2w_d.partition_broadcast(128)), "l2w", writes=a3n(0, 2))
        S.dma("sp", lambda e: e.dma_start(out=l2b, in_=l2b_d.partition_broadcast(128)), "l2b", writes=a3n(2, 2))
        S.dma("sp", lambda e: e.dma_start(out=g2bc, in_=g2row_d.partition_broadcast(128)), "g2ld", reads=["g2row_d"], writes=a3n(4, 2))

        REG = {}

        def _pool_init(g):
            REG["xs"] = g.to_reg(16383)
            REG["w"] = g.to_reg(4095)
        S.init_pool = _pool_init

        def av(lo, n):
            return arena[:, lo:lo + n]
        NW = 3
        Wg = [av(w * 12288, 4096).rearrange("p (k n) -> p k n", k=8) for w in range(NW)]
        Wu = [av(w * 12288 + 4096, 4096).rearrange("p (k n) -> p k n", k=8) for w in range(NW)]
        Wd = [av(w * 12288 + 8192, 4096).rearrange("p (k n) -> p k n", k=4) for w in range(NW)]
        ztile = av(24576, 1024)
        h2ld = [av(25600 + w * 1024, 1024) for w in range(2)]
        Mb = av(27648, 1024)
        Ls_b = av(28672, 128); ones_b = av(28800, 128)
        a2b = arena2[:, 2048:8192].bitcast(BF16)
        xs_sb = [a2b[:, w * 1024:(w + 1) * 1024] for w in range(3)]
        xsT = [a2b[:, 3072 + w * 1024:3072 + (w + 1) * 1024].rearrange("p (k n) -> p k n", k=8) for w in range(2)]
        act_sb = [a2b[:, 5120 + w * 512:5120 + (w + 1) * 512] for w in range(2)]
        actT = [a2b[:, 6144 + w * 512:6144 + (w + 1) * 512].rearrange("p (k n) -> p k n", k=4) for w in range(2)]

        def b2(s_):
            return arena2[:, s_ * 1024:(s_ + 1) * 1024]

        def v3(ap):
            return ap.rearrange("p (j x) -> p j x", j=32)

        gl = logits_all[:, :, 0:4]
        el4 = logits_all[:, :, 4:36].rearrange("p j (g e) -> p j g e", g=4)
        gmax = gsm[:, 0, :]; gsum = gsm[:, 1, :]; pstar = gsm[:, 2, :]; v1 = gsm[:, 3, :]; v2 = gsm[:, 4, :]
        w1 = gsm[:, 5, :]; w2 = gsm[:, 6, :]
        gtmp = qf[:, 256:384].rearrange("p (a b) -> p a b", a=32); gm = qf[:, 384:512].rearrange("p (a b) -> p a b", a=32)
        elm = v3(a3(6, 2)); elmn = a3n(6, 2)
        mk1 = v3(a3(8, 2)); mk1n = a3n(8, 2)
        mk2 = v3(a3(10, 2)); mk2n = a3n(10, 2)
        elm4 = a3(6, 2).rearrange("p (j g e) -> p j g e", j=32, g=4)

        def bc32(ap, n):
            return ap.unsqueeze(2).broadcast_to([128, 32, n])

        S.op("dve", lambda e: e.tensor_reduce(out=gmax, in_=gl, axis=AX.X, op=ALU.max), ["logits"], ["gmax"])
        S.op("dve", lambda e: e.tensor_tensor(out=gtmp, in0=gl, in1=bc32(gmax, 4), op=ALU.subtract), ["logits", "gmax"], ["gtmp"])
        S.op("act", lambda e: e.activation(out=gtmp, in_=gtmp, func=AF.Exp), ["gtmp"], ["gtmp"])
        S.op("dve", lambda e: e.tensor_reduce(out=gsum, in_=gtmp, axis=AX.X, op=ALU.add), ["gtmp"], ["gsum"])
        S.op("dve", lambda e: e.reciprocal(out=pstar, in_=gsum), ["gsum"], ["pstar"])
        S.op("dve", lambda e: e.tensor_tensor(out=gm, in0=gl, in1=bc32(gmax, 4), op=ALU.is_equal), ["logits", "gmax"], ["gm"])
        S.op("dve", lambda e: e.tensor_tensor(out=elm4, in0=el4, in1=gm.unsqueeze(3).broadcast_to([128, 32, 4, 8]), op=ALU.mult),
             ["logits", "gm"], elmn)
        S.op("dve", lambda e: e.tensor_scalar(out=gtmp, in0=gm, scalar1=-1.0, scalar2=BIG, op0=ALU.add, op1=ALU.mult),
             ["gm"], ["gtmp"])
        S.op("dve", lambda e: e.tensor_tensor(out=elm4, in0=elm4, in1=gtmp.unsqueeze(3).broadcast_to([128, 32, 4, 8]), op=ALU.add),
             elmn + ["gtmp"], elmn)
        S.op("dve", lambda e: e.tensor_reduce(out=v1, in_=elm, axis=AX.X, op=ALU.max), elmn, ["v1"])
        S.op("dve", lambda e: e.tensor_tensor(out=mk1, in0=elm, in1=bc32(v1, 32), op=ALU.is_equal), elmn + ["v1"], mk1n)
        S.op("dve", lambda e: e.scalar_tensor_tensor(out=elm, in0=mk1, scalar=-BIG, in1=elm, op0=ALU.mult, op1=ALU.add),
             elmn + mk1n, elmn)
        S.op("dve", lambda e: e.tensor_reduce(out=v2, in_=elm, axis=AX.X, op=ALU.max), elmn, ["v2"])
        S.op("dve", lambda e: e.tensor_tensor(out=mk2, in0=elm, in1=bc32(v2, 32), op=ALU.is_equal), elmn + ["v2"], mk2n)
        S.op("dve", lambda e: e.tensor_tensor(out=w1, in0=v2, in1=v1, op=ALU.subtract), ["v1", "v2"], ["w1"])
        S.op("act", lambda e: e.activation(out=w1, in_=w1, func=AF.Exp), ["w1"], ["w1"])
        S.op("dve", lambda e: e.tensor_scalar_add(out=w1, in0=w1, scalar1=1.0), ["w1"], ["w1"])
        S.op("dve", lambda e: e.reciprocal(out=w1, in_=w1), ["w1"], ["w1"])
        S.op("dve", lambda e: e.tensor_scalar(out=w2, in0=w1, scalar1=-1.0, scalar2=1.0, op0=ALU.mult, op1=ALU.add), ["w1"], ["w2"])
        S.op("dve", lambda e: e.tensor_tensor(out=w1, in0=w1, in1=pstar, op=ALU.mult), ["w1", "pstar"], ["w1"])
        S.op("dve", lambda e: e.tensor_tensor(out=w2, in0=w2, in1=pstar, op=ALU.mult), ["w2", "pstar"], ["w2"])

        S.op("dve", lambda e: e.tensor_tensor(out=Ls_b, in0=tri[:], in1=ident_f[:], op=ALU.subtract), ["tri", "ident_f"], ["Ls_b"])
        S.op("pool", lambda e: e.memset(ones_b, 1.0), (), ["ones_b"])
        S.op("pool", lambda e: e.memset(ztile, 0.0), (), ["ztile"])
        S.op("dve", lambda e: e.tensor_tensor(out=v3(Mb), in0=mk1, in1=mk2, op=ALU.add), mk1n + mk2n, ["Mb"])
        for hf in range(2):
            S.op("pe", lambda e, hf=hf: e.matmul(pp[hf][:, :], lhsT=Ls_b, rhs=Mb[:, hf * 512:(hf + 1) * 512], start=True, stop=True),
                 ["Ls_b", "Mb"], ["pp%d" % hf])
        S.op("pe", lambda e: e.matmul(pb[:, :], lhsT=ones_b, rhs=Mb[:, 0:512], start=True, stop=True), ["ones_b", "Mb"], ["pb"])
        S.op("pe", lambda e: e.matmul(ps_[:, :], lhsT=ones_b, rhs=Mb[:, 512:1024], start=True, stop=True), ["ones_b", "Mb"], ["ps"])
        R1s = b2(2); CSs = b2(3); A = [b2(0), b2(1)]
        for hf in range(2):
            S.op("act", lambda e, hf=hf: e.activation(out=R1s[:, hf * 512:(hf + 1) * 512], in_=pp[hf][:, :], func=AF.Copy),
                 ["pp%d" % hf], ["b2_2"])
        def bn(x_):
            return ["b2_%d" % x_, "b2_%dlo" % x_]
        S.op("dve", lambda e: e.tensor_copy(out=A[0][:, 0:512], in_=pb[:, :]), ["pb"], ["b2_0lo"])
        S.op("dve", lambda e: e.tensor_copy(out=A[0][:, 512:1024], in_=ps_[:, :]), ["ps"], ["b2_0"])
        S.op("pool", lambda e: e.tensor_copy(out=CSs, in_=A[0]), bn(0), ["b2_3"])
        cur = 0
        for sft in (1, 2, 4, 8, 16):
            src = v3(A[cur]); dst = v3(A[1 - cur]); sn = bn(cur); dn = bn(1 - cur)
            S.op("pool", lambda e, src=src, dst=dst, sft=sft: e.tensor_copy(out=dst[:, 0:sft, :], in_=src[:, 0:sft, :]), sn, [dn[1]])
            S.op("dve", lambda e, src=src, dst=dst, sft=sft: e.tensor_tensor(out=dst[:, sft:32, :], in0=src[:, sft:32, :],
                                                                             in1=src[:, 0:32 - sft, :], op=ALU.add), sn, [dn[0]])
            cur = 1 - cur
        Iinc = A[cur]; In = bn(cur); oth = A[1 - cur]; on_ = bn(1 - cur)
        cnt = gsm[:, 7, :]
        nbk = gsm[:, 0, :]; pend = gsm[:, 1, :]; pst = gsm[:, 3, :]; tmp32 = gsm[:, 4, :]
        nbi = gm.rearrange("p a b -> p (a b)")[:, 0:32].bitcast(I32)
        S.op("dve", lambda e: e.tensor_scalar(out=cnt, in0=v3(Iinc)[:, 31, :], scalar1=255.0, scalar2=1.0 / 256.0,
                                              op0=ALU.add, op1=ALU.mult), In, ["cnt"])
        S.op("dve", lambda e: e.tensor_scalar_add(out=cnt, in0=cnt, scalar1=-0.498046875), ["cnt"], ["cnt"])
        S.op("dve", lambda e: e.tensor_copy(out=nbi, in_=cnt), ["cnt", "gm", "gmax", "gsum"], ["nbi"])
        S.op("dve", lambda e: e.tensor_copy(out=nbk, in_=nbi), ["nbi", "gmax"], ["nbk"])
        S.op("dve", lambda e: e.tensor_copy(out=pend, in_=nbk), ["nbk", "gsum"], ["pend"])
        pcur = pend; poth = tmp32; pcn = "pend"; pon = "tmp32"
        for sft in (1, 2, 4, 8, 16):
            S.op("dve", lambda e, pcur=pcur, poth=poth, sft=sft: e.tensor_copy(out=poth[:, 0:sft], in_=pcur[:, 0:sft]),
                 [pcn, "v2"], [pon])
            S.op("dve", lambda e, pcur=pcur, poth=poth, sft=sft: e.tensor_tensor(out=poth[:, sft:32], in0=pcur[:, sft:32],
                                                                                in1=pcur[:, 0:32 - sft], op=ALU.add), [pcn], [pon])
            pcur, poth = poth, pcur; pcn, pon = pon, pcn
        pendf = pcur; pendn = pcn
        S.op("dve", lambda e: e.tensor_tensor(out=pst, in0=pendf, in1=nbk, op=ALU.subtract), [pendn, "nbk", "v1"], ["pst"])
        S.op("dve", lambda e: e.tensor_single_scalar(out=pst, in_=pst, scalar=256.0, op=ALU.mult), ["pst"], ["pst"])
        dfull = v3(oth)
        S.op("dve", lambda e: e.tensor_tensor(out=oth, in0=Iinc, in1=CSs, op=ALU.subtract), In + ["b2_3"], on_)
        S.op("dve", lambda e: e.tensor_tensor(out=oth, in0=oth, in1=R1s, op=ALU.add), on_ + ["b2_2"], on_)
        S.op("dve", lambda e: e.tensor_tensor(out=dfull, in0=dfull, in1=pst.unsqueeze(1).broadcast_to([128, 32, 32]), op=ALU.add),
             on_ + ["pst"], on_)
        d12f = gm.rearrange("p a b -> p (a b)")[:, 32:96]
        dest_i = T("dest_i", [128, 64], I32)
        tmpm = v3(b2(4))
        for kk, (mk, mkn) in enumerate(((mk1, mk1n), (mk2, mk2n))):
            S.op("dve", lambda e, mk=mk: e.tensor_tensor(out=tmpm, in0=mk, in1=dfull, op=ALU.mult), mkn + on_, ["b2_4"])
            S.op("dve", lambda e, kk=kk: e.tensor_reduce(out=d12f[:, kk * 32:(kk + 1) * 32], in_=tmpm, axis=AX.X, op=ALU.add),
                 ["b2_4", "nbi"], ["d12f%d" % kk])
        S.op("dve", lambda e: e.tensor_copy(out=dest_i[:], in_=d12f), ["d12f0", "d12f1"], ["dest_i"])
        NBLK = 64
        cmp3 = arena2[:, 5 * 1024:7 * 1024].rearrange("p (b x) -> p b x", b=NBLK)
        bef = T("bef", [128, NBLK]); widx = T("widx", [128, 2, NBLK], I32)
        S.op("dve", lambda e: e.tensor_tensor(out=cmp3, in0=pendf.unsqueeze(1).broadcast_to([128, NBLK, 32]),
                                              in1=blkiota[:, 0:NBLK].unsqueeze(2).broadcast_to([128, NBLK, 32]), op=ALU.is_le),
             [pendn, "blkiota"], ["b2_5", "b2_6", "b2_7"])
        S.op("dve", lambda e: e.tensor_reduce(out=bef[:], in_=cmp3, axis=AX.X, op=ALU.add), ["b2_5", "b2_6", "b2_7"], ["bef"])
        S.op("dve", lambda e: e.tensor_scalar(out=bef[:], in0=bef[:], scalar1=128.0, scalar2=piota[:, 0:1], op0=ALU.mult, op1=ALU.add),
             ["bef", "piota"], ["bef"])
        S.op("dve", lambda e: e.tensor_copy(out=widx[:, 0, :], in_=bef[:]), ["bef"], ["widx0"])

        zn = ["xs_z%d" % zz for zz in range(8)]
        scat = []
        for j in range(NT):
            hb = h2ld[j % 2]; hbn = "h2ld%d" % (j % 2)
            S.dma("sp", lambda e, j=j, hb=hb: e.dma_start(out=hb, in_=h2_d[j * 128:(j + 1) * 128, :]), hbn,
                  reads=["h2_d%d" % j], writes=[hbn])
            for kk in range(2):
                S.dma("pool", lambda e, j=j, kk=kk, hb=hb: e.indirect_dma_start(
                    out=xs_d[:, :], out_offset=bass.IndirectOffsetOnAxis(ap=dest_i[:, kk * 32 + j:kk * 32 + j + 1], axis=0),
                    in_=hb, in_offset=None, bounds_check=REG["xs"], oob_is_err=False),
                    "scat%d_%d" % (j % 2, kk), reads=[hbn, "dest_i"] + zn, writes=["xs_s%d_%d" % (j, kk)])
        xsn = ["xs_s%d_%d" % (j, kk) for j in range(NT) for kk in range(2)]
        S.barrier()

        slt = [a3(12), a3(13)]
        ysb = [b2(0), b2(1)]
        pau = [(pp[0], "pp0", pp[1], "pp1"), (pb, "pb", ps_, "ps")]
        def gather_weights(blk):
            wb = blk % NW
            for (Wt, wsrc, nm) in ((Wg, wgb_d, "wg"), (Wu, wub_d, "wu"), (Wd, wdb_d, "wd")):
                Wflat = Wt[wb].rearrange("p k n -> p (k n)")
                S.dma("pool", lambda e, Wflat=Wflat, wsrc=wsrc, blk=blk: e.indirect_dma_start(
                    out=Wflat, out_offset=None, in_=wsrc.rearrange("(r two) n -> r (two n)", two=2),
                    in_offset=bass.IndirectOffsetOnAxis(ap=widx[:, 0, blk:blk + 1], axis=0),
                    bounds_check=REG["w"], oob_is_err=False),
                    "%s%d" % (nm, wb), reads=["widx0"], writes=["%s%d_0" % (nm, wb), "%s%d_1" % (nm, wb)])

        def xs_load(sl):
            xb = sl % 3
            S.dma("sp", lambda e: e.dma_start(out=xs_sb[xb], in_=xs_d[sl * 128:(sl + 1) * 128, :]),
                  "xsld%d" % xb, writes=["xs_sb%d" % xb])

        def stage_a1(sl):
            xb = sl % 3; sb = sl % 2
            transposes8(ptr, ["ptr"], xs_sb[xb], ["xs_sb%d" % xb])
            S.op("dve", lambda e: e.tensor_copy(out=xsT[sb], in_=ptr[:]), ["ptr"], ["xsT%d" % sb])

        def stage_a2(sl):
            wb = (sl // 2) % NW; sb = sl % 2
            pa_, pan_, pu_, pun_ = pau[sb]
            wgn = ["wg%d_0" % wb, "wg%d_1" % wb]; wun = ["wu%d_0" % wb, "wu%d_1" % wb]
            for k in range(8):
                S.op("pe", lambda e, k=k: e.matmul(pa_[:, :], lhsT=xsT[sb][:, k, :], rhs=Wg[wb][:, k, :],
                                                   start=(k == 0), stop=(k == 7)), ["xsT%d" % sb] + wgn, [pan_])
            for k in range(8):
                S.op("pe", lambda e, k=k: e.matmul(pu_[:, :], lhsT=xsT[sb][:, k, :], rhs=Wu[wb][:, k, :],
                                                   start=(k == 0), stop=(k == 7)), ["xsT%d" % sb] + wun, [pun_])

        def stage_b1(sl):
            sb = sl % 2
            pa_, pan_, pu_, pun_ = pau[sb]
            S.op("act", lambda e: e.activation(out=slt[sb], in_=pa_[:, :], func=AF.Silu), [pan_], ["t%d" % (12 + sb)])
            S.op("dve", lambda e: e.tensor_tensor(out=act_sb[sb], in0=slt[sb], in1=pu_[:, :], op=ALU.mult),
                 ["t%d" % (12 + sb), pun_], ["act_sb%d" % sb])
            for fc in range(4):
                S.op("pe", lambda e, fc=fc: e.transpose(out=ptq[:, fc, :], in_=act_sb[sb][:, fc * 128:(fc + 1) * 128],
                                                        identity=ident_b[:]), ["act_sb%d" % sb, "ident_b"], ["ptq"])
            S.op("act", lambda e: e.activation(out=actT[sb], in_=ptq[:, 0:4, :], func=AF.Copy), ["ptq"], ["actT%d" % sb])

        def stage_b2(sl):
            wb = (sl // 2) % NW; sb = sl % 2
            wdn = ["wd%d_0" % wb, "wd%d_1" % wb]
            for dc in range(2):
                pyb = [po, pkv][dc]; pyn_ = ["po", "pkv"][dc]
                for fc in range(4):
                    S.op("pe", lambda e, fc=fc, dc=dc, pyb=pyb: e.matmul(
                        pyb[:, :], lhsT=actT[sb][:, fc, :], rhs=Wd[wb][:, fc, dc * 512:(dc + 1) * 512],
                        start=(fc == 0), stop=(fc == 3)), ["actT%d" % sb] + wdn, [pyn_])
            S.op("act", lambda e: e.activation(out=ysb[sb][:, 0:512], in_=po[:, :], func=AF.Copy), ["po"], ["ysb%d_0" % sb])
            S.op("dve", lambda e: e.tensor_copy(out=ysb[sb][:, 512:1024], in_=pkv[:, :]), ["pkv"], ["ysb%d_1" % sb])
            S.dma("sp", lambda e: e.dma_start(out=ys_d[sl * 128:(sl + 1) * 128, :], in_=ysb[sb]),
                  "ysst%d" % sb, reads=["ysb%d_0" % sb, "ysb%d_1" % sb], writes=["ys_%d" % sl])

        NSUB = 2 * NBLK
        for bb in range(NW): gather_weights(bb)
        xs_load(0); xs_load(1)
        for it in range(NSUB + 3):
            if it + 2 < NSUB: xs_load(it + 2)
            if it < NSUB: stage_a1(it)
            if 0 <= it - 1 < NSUB: stage_a2(it - 1)
            if 0 <= it - 2 < NSUB: stage_b1(it - 2)
            if 0 <= it - 3 < NSUB: stage_b2(it - 3)
            if it >= 4 and (it - 4) % 2 == 0:
                nb_ = (it - 4) // 2 + NW
                if nb_ < NBLK: gather_weights(nb_)
        S.barrier()

        out_stores = []
        NB = 4
        arf = arena[:, 0:16384].bitcast(F32)

        def cbuf(j):
            pj = j % NB
            return (arf[:, (2 * pj) * 1024:(2 * pj + 1) * 1024], arf[:, (2 * pj + 1) * 1024:(2 * pj + 2) * 1024], b2(pj),
                    "cg%d_0" % pj, "cg%d_1" % pj, ["cx%d" % pj], pj)

        def c1(j):
            y1g, y2g, xt, y1n, y2n, xtn, pj = cbuf(j)
            for (yg, yn, kk) in ((y1g, y1n, 0), (y2g, y2n, 1)):
                S.dma("pool", lambda e, yg=yg, kk=kk: e.indirect_dma_start(
                    out=yg, out_offset=None, in_=ys_d[:, :],
                    in_offset=bass.IndirectOffsetOnAxis(ap=dest_i[:, kk * 32 + j:kk * 32 + j + 1], axis=0),
                    bounds_check=REG["xs"], oob_is_err=False), "yg%d_%d" % (pj, kk), reads=["dest_i"], writes=[yn])
            S.dma("sp", lambda e: e.dma_start(out=xt, in_=x1_d[j * 128:(j + 1) * 128, :]), "x1ld%d" % pj,
                  reads=["x1_d%d" % j], writes=xtn)

        def c2(j):
            y1g, y2g, xt, y1n, y2n, xtn, pj = cbuf(j)
            S.op("act", lambda e: e.activation(out=y1g, in_=y1g, func=AF.Copy, scale=w1[:, j:j + 1]), [y1n, "w1"], [y1n])
            S.op("dve", lambda e: e.scalar_tensor_tensor(out=y1g, in0=y2g, scalar=w2[:, j:j + 1], in1=y1g,
                                                         op0=ALU.mult, op1=ALU.add), [y1n, y2n, "w2"], [y1n])
            S.op("pool", lambda e: e.tensor_tensor(out=y1g, in0=y1g, in1=g2bc, op=ALU.mult), [y1n] + a3n(4, 2), [y1n])

        def c3(j):
            y1g, y2g, xt, y1n, y2n, xtn, pj = cbuf(j)
            S.op("dve", lambda e: e.scalar_tensor_tensor(out=xt, in0=xt, scalar=ALPHA, in1=y1g, op0=ALU.mult, op1=ALU.add),
                 xtn + [y1n], xtn)
            S.op("dve", lambda e: e.bn_stats(out=cst_[pj][:, 0:6], in_=xt[:, 0:512]), xtn, ["cst%d" % pj])
            S.op("dve", lambda e: e.bn_stats(out=cst_[pj][:, 6:12], in_=xt[:, 512:1024]), xtn, ["cst%d" % pj])
            S.op("dve", lambda e: e.bn_aggr(out=cmv_[pj][:], in_=cst_[pj][:]), ["cst%d" % pj], ["cmv%d" % pj])
            S.op("act", lambda e: e.activation(out=crs_[pj][:], in_=cmv_[pj][:, 1:2], func=AF.Ln, bias=EPS), ["cmv%d" % pj], ["crs%d" % pj])
            S.op("act", lambda e: e.activation(out=crs_[pj][:], in_=crs_[pj][:], func=AF.Exp, scale=-0.5), ["crs%d" % pj], ["crs%d" % pj])
            S.op("dve", lambda e: e.tensor_scalar(out=cnm_[pj][:], in0=cmv_[pj][:, 0:1], scalar1=-1.0, scalar2=crs_[pj][:, 0:1],
                                                  op0=ALU.mult, op1=ALU.mult), ["cmv%d" % pj, "crs%d" % pj], ["cnm%d" % pj])

        def c4(j):
            y1g, y2g, xt, y1n, y2n, xtn, pj = cbuf(j)
            S.op("act", lambda e: e.activation(out=xt, in_=xt, func=AF.Identity, scale=crs_[pj][:, 0:1], bias=cnm_[pj][:, 0:1]),
                 xtn + ["cnm%d" % pj, "crs%d" % pj], xtn)
            S.op("dve", lambda e: e.tensor_tensor(out=xt, in0=xt, in1=l2w, op=ALU.mult), xtn + a3n(0, 2), xtn)
            S.op("dve", lambda e: e.tensor_tensor(out=xt, in0=xt, in1=l2b, op=ALU.add), xtn + a3n(2, 2), xtn)
            out_stores.append(S.dma("sp", lambda e: e.dma_start(out=out_d[j * 128:(j + 1) * 128, :], in_=xt), "ost%d" % pj, reads=xtn))

        cst_ = [ost[:, 0:12], ost[:, 12:24], st[:, 0:12], T("cst3", [128, 12])]
        cmv_ = [omv[:, 0, :], omv[:, 1, :], omv[:, 2, :], omv[:, 3, :]]
        crs_ = [r4[:, 0:1], r4[:, 1:2], r4[:, 2:3], r4[:, 3:4]]
        cnm_ = [mv[:, 0:1], mv[:, 1:2], rs[:, 0:1], T("cnm3", [128, 1])]
        for it in range(NT + 3):
            if it < NT: c1(it)
            if 0 <= it - 1 < NT: c2(it - 1)
            if 0 <= it - 2 < NT: c3(it - 2)
            if 0 <= it - 3 < NT: c4(it - 3)
        S.emit(block, final_waits=out_stores)
    return nc


def _consts():
    ident = np.eye(128, dtype=np.float32)
    s = np.arange(128)
    tri = (s[:, None] <= s[None, :]).astype(np.float32)
    invf = np.power(np.float32(10000.0), -np.arange(0, 128, 2, dtype=np.float32) / np.float32(128)).astype(np.float32)[None, :]
    gam = 1.0 - np.exp2(-5.0 - np.arange(4, dtype=np.float64))
    lg = np.log(gam)
    p = np.arange(128, dtype=np.float64)[:, None] + 1.0
    qd = (np.exp(p * lg[None, :]) * (128.0 ** -0.5)).astype(np.float32)
    kd = np.exp(-p * lg[None, :]).astype(np.float32)
    ebr = np.broadcast_to(np.exp(128.0 * lg)[None, :], (128, 4)).astype(np.float32).copy()
    blkiota = np.broadcast_to(np.arange(96, dtype=np.float32)[None, :], (128, 96)).copy()
    piota = np.arange(128, dtype=np.float32)[:, None].copy()
    import ml_dtypes
    zeros_bf = np.zeros((2048, 1024), dtype=ml_dtypes.bfloat16)
    return dict(ident=ident, tri=tri, invf=invf, qd=qd, kd=kd, ebr=ebr, blkiota=blkiota, piota=piota, zeros_bf=zeros_bf)


def make_in_maps(inputs):
    f = lambda a: np.ascontiguousarray(np.asarray(a, dtype=np.float32))
    x = f(inputs["x"]); c = f(inputs["c"]); pos = np.ascontiguousarray(np.asarray(inputs["positions"], dtype=np.int32))
    shared = dict(
        w_ada=f(inputs["w_ada"][0]), b_ada=f(inputs["b_ada"][0])[None, :], w_in=f(inputs["w_in"][0]), w_out=f(inputs["w_out"][0]),
        hgrn_lb=f(inputs["hgrn_lb"]), hgrn_norm_w=f(inputs["hgrn_norm_w"][0])[None, :], ret_norm_w=f(inputs["ret_norm_w"][0])[None, :],
        post_ln1_w=f(inputs["post_ln1_w"][0])[None, :], post_ln1_b=f(inputs["post_ln1_b"][0])[None, :],
        post_ln2_w=f(inputs["post_ln2_w"][0])[None, :], post_ln2_b=f(inputs["post_ln2_b"][0])[None, :],
        w_r=np.ascontiguousarray(np.concatenate([f(inputs["w_rg"][0]), f(inputs["w_re"][0])], axis=1)),
        b_r=np.ascontiguousarray(np.concatenate([f(inputs["b_rg"][0]), f(inputs["b_re"][0])], axis=0))[None, :],
        w_gate=np.ascontiguousarray(f(inputs["w_gate"][0]).reshape(32, 8, 128, 512).transpose(0, 2, 1, 3)).reshape(8192, 2048),
        w_up=np.ascontiguousarray(f(inputs["w_up"][0]).reshape(32, 8, 128, 512).transpose(0, 2, 1, 3)).reshape(8192, 2048),
        w_down=np.ascontiguousarray(f(inputs["w_down"][0]).reshape(32, 4, 128, 1024).transpose(0, 2, 1, 3)).reshape(8192, 2048),
    )
    shared.update(_consts())
    maps = []
    for b in range(8):
        m = dict(shared)
        m["x"] = np.ascontiguousarray(x[b])
        m["ccol"] = np.ascontiguousarray(c[b].reshape(8, 128).T)
        m["pos"] = np.ascontiguousarray(pos[b].reshape(32, 128))
        maps.append(m)
    return maps


def kernel(**inputs):
    nc = build_program("full")
    maps = make_in_maps(inputs)
    res = run_bass_kernel_spmd(nc, maps, core_ids=list(range(8)))
    return np.stack([np.asarray(r["out"], dtype=np.float32) for r in res.results], axis=0)
```

```python
import math
import numpy as np
import concourse.bass as bass
import concourse.mybir as mybir
from concourse.bass_utils import run_bass_kernel_spmd
from contextlib import ExitStack

F32 = mybir.dt.float32; BF16 = mybir.dt.bfloat16; I32 = mybir.dt.int32
ALU = mybir.AluOpType; AF = mybir.ActivationFunctionType; AX = mybir.AxisListType

NT = 32
ALPHA = 2.0 ** 0.25
EPS = 1e-5
BIG = 1.0e30
TWO_PI = 2.0 * math.pi


class _Op:
    __slots__ = ("eng", "fn", "deps", "sig", "val", "dsem", "dval")

    def __init__(self, eng, fn):
        self.eng = eng; self.fn = fn; self.deps = []; self.sig = False
        self.val = 0; self.dsem = None; self.dval = 0


class Sched:
    ENGS = ("pe", "act", "dve", "pool", "sp")

    def __init__(self, nc, es):
        self.nc = nc; self.es = es
        self.ops = {e: [] for e in self.ENGS}
        self.lastw = {}; self.readers = {}
        self.dsems = {}; self.dcount = {}; self.dlast = {}
        self.barrier_ops = []

    def _deps(self, op, reads, writes):
        deps = list(self.barrier_ops)
        for r in reads:
            w = self.lastw.get(r)
            if w is not None: deps.append(w)
        for w_ in writes:
            w = self.lastw.get(w_)
            if w is not None: deps.append(w)
            deps.extend(self.readers.get(w_, ()))
        for r in reads:
            self.readers.setdefault(r, []).append(op)
        for w_ in writes:
            self.lastw[w_] = op; self.readers[w_] = []
        seen = set()
        for d in deps:
            if d is op or id(d) in seen: continue
            seen.add(id(d)); op.deps.append(d)
            if d.dsem is None and not (d.eng == "pe" and op.eng == "pe"): d.sig = True

    def op(self, eng, fn, reads=(), writes=()):
        o = _Op(eng, fn); self._deps(o, reads, writes); self.ops[eng].append(o); return o

    def dma(self, eng, fn, slot, reads=(), writes=()):
        o = _Op(eng, fn)
        if slot not in self.dsems:
            self.dsems[slot] = self.es.enter_context(self.nc.semaphore("d_" + slot)); self.dcount[slot] = 0
        self.dcount[slot] += 16
        o.dsem = self.dsems[slot]; o.dval = self.dcount[slot]
        self._deps(o, reads, writes); self.ops[eng].append(o); self.dlast[slot] = o; return o

    def barrier(self):
        b = []
        for e in self.ENGS:
            if self.ops[e]:
                o = self.ops[e][-1]
                if o.dsem is None: o.sig = True
                b.append(o)
        b.extend(self.dlast.values())
        self.barrier_ops = b

    def emit(self, block, final_waits=()):
        nc = self.nc
        cengs = ("pe", "act", "dve", "pool")
        sems = {e: self.es.enter_context(nc.semaphore("s_" + e)) for e in cengs}
        total = {}
        for e in self.ENGS:
            comp = [o for o in self.ops[e] if o.dsem is None]
            if comp: comp[-1].sig = True
            c = 0
            for o in self.ops[e]:
                if o.dsem is None and o.sig:
                    assert e in cengs
                    c += 1; o.val = c
            total[e] = c

        def run(engname, engobj):
            waited = {}
            init = getattr(self, "init_" + engname, None)
            if init is not None: init(engobj)
            for o in self.ops[engname]:
                need = {}
                for d in o.deps:
                    if d.dsem is not None:
                        key = ("d", id(d.dsem)); sem = d.dsem; v = d.dval
                    else:
                        if d.eng == "pe" and engname == "pe": continue
                        key = ("e", d.eng); sem = sems[d.eng]; v = d.val
                    if v > need.get(key, (None, 0))[1]: need[key] = (sem, v)
                for key, (sem, v) in need.items():
                    if waited.get(key, 0) >= v: continue
                    engobj.wait_ge(sem, v); waited[key] = v
                ins = o.fn(engobj)
                if o.dsem is not None: ins.then_inc(o.dsem, 16)
                elif o.sig: ins.then_inc(sems[engname], 1)
            for e2 in cengs:
                if e2 != engname and total[e2] > 0: engobj.wait_ge(sems[e2], total[e2])
            for slot, sem in self.dsems.items():
                engobj.wait_ge(sem, self.dcount[slot])

        final = list(final_waits)

        @block.tensor
        def _(t): run("pe", t)

        @block.scalar
        def _(a): run("act", a)

        @block.vector
        def _(v): run("dve", v)

        @block.gpsimd
        def _(g): run("pool", g)

        @block.sync
        def _(s):
            run("sp", s)
            last = {}
            for d in final:
                last[id(d.dsem)] = (d.dsem, max(last.get(id(d.dsem), (None, 0))[1], d.dval))
            for sem, v in last.values(): s.wait_ge(sem, v)


def build_program(stage="full", stop=99, ntiles=NT):
    nc = bass.Bass("TRN2", target_bir_lowering=False)

    def din(name, shape, dt=F32):
        return nc.dram_tensor(name, shape, dt, kind="ExternalInput").ap()

    x_d = din("x", [4096, 1024]); ccol_d = din("ccol", [128, 8]); pos_d = din("pos", [32, 128], I32)
    wada_d = din("w_ada", [1024, 6144]); bada_d = din("b_ada", [1, 6144])
    win_d = din("w_in", [1024, 4096]); wout_d = din("w_out", [1024, 1024])
    lb_d = din("hgrn_lb", [2, 512]); hnw_d = din("hgrn_norm_w", [1, 512]); rnw_d = din("ret_norm_w", [1, 512])
    l1w_d = din("post_ln1_w", [1, 1024]); l1b_d = din("post_ln1_b", [1, 1024])
    l2w_d = din("post_ln2_w", [1, 1024]); l2b_d = din("post_ln2_b", [1, 1024])
    wr_d = din("w_r", [1024, 36]); br_d = din("b_r", [1, 36])
    if stage == "full":
        wg_d = din("w_gate", [8192, 2048]); wu_d = din("w_up", [8192, 2048]); wd_d = din("w_down", [8192, 2048])
    blkiota_d = din("blkiota", [128, 96]); piota_d = din("piota", [128, 1])
    zeros_d = din("zeros_bf", [2048, 1024], BF16)
    ident_d = din("ident", [128, 128]); tri_d = din("tri", [128, 128]); invf_d = din("invf", [1, 64])
    qd_d = din("qd", [128, 4]); kd_d = din("kd", [128, 4]); ebr_d = din("ebr", [128, 4])
    out_d = nc.dram_tensor("out", [4096, 1024], F32, kind="ExternalOutput").ap()
    x1_d = nc.dram_tensor("x1_scr", [4096, 1024], F32).ap()
    h2_d = nc.dram_tensor("h2_scr", [4096, 1024], BF16).ap()
    xs_d = nc.dram_tensor("xs_scr", [16384, 1024], BF16).ap()
    ys_d = nc.dram_tensor("ys_scr", [16384, 1024], F32).ap()
    g2row_d = nc.dram_tensor("g2row_scr", [1, 1024], F32).ap()
    cs_d = nc.dram_tensor("cs_scr", [128, 2, 2048], F32).ap()
    wgb_d = nc.dram_tensor("wgb_scr", [8192, 2048], BF16).ap()
    wub_d = nc.dram_tensor("wub_scr", [8192, 2048], BF16).ap()
    wdb_d = nc.dram_tensor("wdb_scr", [8192, 2048], BF16).ap()

    with ExitStack() as es:
        S = Sched(nc, es)

        def T(name, shape, dt=F32):
            return es.enter_context(nc.sbuf_tensor("sb_" + name, shape, dt))

        def PS(name, shape, dt=F32):
            return es.enter_context(nc.psum_tensor("pm_" + name, shape, dt))

        arena = T("arena", [128, 40960], BF16)
        arena2 = T("arena2", [128, 8192], F32)
        arena3 = T("arena3", [128, 8192], F32)
        w_in_sb = arena[:, 0:32768].rearrange("p (k n) -> p k n", k=8)
        w_out_sb = arena[:, 32768:40960].rearrange("p (k n) -> p k n", k=8)
        cos_t = arena2[:, 0:2048].rearrange("p (j f) -> p j f", j=32)
        sin_t = arena2[:, 2048:4096].rearrange("p (j f) -> p j f", j=32)
        x_sb = [arena2[:, 4096:5120], arena2[:, 5120:6144], arena2[:, 7168:8192]]
        g1bc = arena2[:, 6144:7168]

        def a3(s, n=1):
            return arena3[:, s * 512:(s + n) * 512]

        def a3n(s, n=1):
            return ["t%d" % j for j in range(s, s + n)]

        ccol = T("ccol", [128, 8]); cact = T("cact", [128, 8])
        ident_f = T("ident_f", [128, 128]); ident_b = T("ident_b", [128, 128], BF16); tri = T("tri", [128, 128])
        ones_f = T("ones_f", [128, 128])
        invf_bc = T("invf_bc", [128, 64]); qd = T("qd", [128, 4]); kd = T("kd", [128, 4]); ebl = T("ebl", [128, 8])
        lb = T("lb", [128, 512]); oml = T("oml", [128, 512]); normw = T("normw", [128, 1024])
        l1w = T("l1w", [128, 1024]); l1b = T("l1b", [128, 1024])
        br_bc = T("br_bc", [128, 36]); wr_sb = T("wr_sb", [128, 8, 36], BF16)
        modcol = T("modcol", [128, 32])
        posi = T("posi", [32, 128], I32); posf = T("posf", [32, 128]); posT = T("posT", [128, 32])
        bch = [arena2[:, 4096:4608], arena2[:, 4608:5120]]
        mrow = [arena2[:, 7168:7680], arena2[:, 7680:8192]]
        sc2_bc = T("sc2_bc", [128, 1024]); sh2_bc = T("sh2_bc", [128, 1024])
        blkiota = T("blkiota", [128, 96]); piota = T("piota", [128, 1])
        st = T("st", [128, 12]); mv = T("mv", [128, 2]); rs = T("rs", [128, 1])
        ost = T("ost", [128, 24]); omv = T("omv", [128, 4, 2]); r4 = T("r4", [128, 4])
        xn_bf = T("xn_bf", [128, 1024], BF16); hT = T("hT", [128, 8, 128], BF16)
        q_in = T("q_in", [128, 1024], BF16)
        cs_t = [T("cs_t0", [128, 2, 64]), T("cs_t1", [128, 2, 64])]
        eblA = [T("eblA0", [128, 4]), T("eblA1", [128, 4])]
        q_inT = T("q_inT", [128, 8, 128], BF16); k_inT = T("k_inT", [128, 8, 128], BF16)
        qin = [arena2[:, p * 512:(p + 1) * 512].bitcast(BF16) for p in range(2)]
        kin = [arena2[:, 1024 + p * 512:1024 + (p + 1) * 512].bitcast(BF16) for p in range(2)]
        vbf = [arena2[:, 2048 + p * 512:2048 + (p + 1) * 512].bitcast(BF16) for p in range(2)]
        ga = [arena2[:, 3072 + p * 512:3072 + (p + 1) * 512] for p in range(2)]
        gb = [arena2[:, 6144 + p * 512:6144 + (p + 1) * 512] for p in range(2)]
        AT = [T("AT0", [128, 4, 128], BF16), T("AT1", [128, 4, 128], BF16)]
        S32 = T("S32", [128, 8, 128]); S_bf = T("S_bf", [128, 8, 128], BF16)
        o_fin = T("o_fin", [128, 1024], BF16); oT_sb = T("oT_sb", [128, 8, 128], BF16)
        h2T_t = T("h2T_t", [128, 8, 128], BF16)
        h2tok = T("h2tok", [128, 1024], BF16)
        logits_all = T("logits_all", [128, 32, 36])
        G_all = S32[:, :, :].rearrange("p a (b c) -> p (a b) c", b=4)
        qf = q_in[:, :].bitcast(F32)
        gsm = qf[:, 0:256].rearrange("p (a b) -> p a b", a=8)

        ptr = PS("ptr", [128, 8, 128], BF16); ptq = PS("ptq", [128, 8, 128], BF16)
        pp = [PS("pp0", [128, 512]), PS("pp1", [128, 512])]
        pb = PS("pb", [128, 512]); ps_ = PS("ps", [128, 512]); po = PS("po", [128, 512]); pkv = PS("pkv", [128, 512])
        ps3 = ps_[:, :].rearrange("p (h d) -> p h d", h=4)
        po3 = po[:, :].rearrange("p (h d) -> p h d", h=4)
        pkv3 = pkv[:, :].rearrange("p (h d) -> p h d", h=4)

        block = es.enter_context(nc.Block())

        def r3(ap, h=4):
            return ap.rearrange("p (h d) -> p h d", h=h)

        def ld(eng, dst, src, slot, w):
            return S.dma(eng, lambda e: e.dma_start(out=dst, in_=src), slot, writes=w)

        ld("sp", ccol[:], ccol_d[:, :], "c", ["ccol"])
        ld("sp", ident_f[:], ident_d[:, :], "identf", ["ident_f"])
        ld("sp", tri[:], tri_d[:, :], "tri", ["tri"])
        ld("sp", invf_bc[:], invf_d.partition_broadcast(128), "invf", ["invf"])
        ld("sp", qd[:], qd_d[:, :], "qd", ["qd"])
        ld("sp", kd[:], kd_d[:, :], "kd", ["kd"])
        ld("sp", ebl[:, 4:8], ebr_d[:, :], "ebr", ["ebl1"])
        ld("sp", lb[:], lb_d[0:1, :].partition_broadcast(128), "lbA", ["lb"])
        ld("sp", oml[:], lb_d[1:2, :].partition_broadcast(128), "lbB", ["oml"])
        ld("sp", normw[:, 0:512], hnw_d.partition_broadcast(128), "hnw", ["normw0"])
        ld("sp", normw[:, 512:1024], rnw_d.partition_broadcast(128), "rnw", ["normw1"])
        ld("sp", l1w[:], l1w_d.partition_broadcast(128), "l1w", ["l1w"])
        ld("sp", l1b[:], l1b_d.partition_broadcast(128), "l1b", ["l1b"])
        ld("sp", br_bc[:], br_d.partition_broadcast(128), "br", ["br_bc"])
        ld("sp", posi[:], pos_d[:, :], "pos", ["posi"])
        ld("sp", blkiota[:], blkiota_d[:, :], "blkiota", ["blkiota"])
        ld("sp", piota[:], piota_d[:, :], "piota", ["piota"])
        ld("pool", ident_b[:], ident_d[:, :], "identb", ["ident_b"])
        ld("pool", wr_sb[:], wr_d.rearrange("(k p) n -> p k n", p=128), "wr", ["wr_sb"])
        for j in range(4):
            ld("pool", w_in_sb[:, :, j * 1024:(j + 1) * 1024],
               win_d[:, j * 1024:(j + 1) * 1024].rearrange("(k p) n -> p k n", p=128), "win", ["w_in"])
        ld("pool", w_out_sb, wout_d.rearrange("(k p) n -> p k n", p=128), "wout", ["w_out"])

        S.op("dve", lambda e: e.memset(ones_f[:], 1.0), writes=["ones_f"])
        S.op("dve", lambda e: e.memset(S32[:], 0.0), writes=["S32_0", "S32_1"])
        S.op("pool", lambda e: e.memset(S_bf[:], 0.0), writes=["Sbf0", "Sbf1"])

        S.op("act", lambda e: e.activation(out=cact[:], in_=ccol[:], func=AF.Exp, scale=-1.0), ["ccol"], ["cact"])
        S.op("act", lambda e: e.activation(out=cact[:], in_=cact[:], func=AF.Ln, bias=1.0), ["cact"], ["cact"])
        S.op("act", lambda e: e.activation(out=cact[:], in_=cact[:], func=AF.Exp, scale=-1.0), ["cact"], ["cact"])
        S.op("dve", lambda e: e.tensor_tensor(out=cact[:], in0=cact[:], in1=ccol[:], op=ALU.mult), ["cact", "ccol"], ["cact"])

        S.op("dve", lambda e: e.tensor_tensor(out=lb[:], in0=lb[:], in1=oml[:], op=ALU.subtract), ["lb", "oml"], ["lb"])
        S.op("act", lambda e: e.activation(out=lb[:], in_=lb[:], func=AF.Exp, scale=-1.0), ["lb"], ["lb"])
        S.op("act", lambda e: e.activation(out=lb[:], in_=lb[:], func=AF.Ln, bias=1.0), ["lb"], ["lb"])
        S.op("act", lambda e: e.activation(out=lb[:], in_=lb[:], func=AF.Exp, scale=-1.0), ["lb"], ["lb"])
        S.op("dve", lambda e: e.tensor_scalar(out=oml[:], in0=lb[:], scalar1=-1.0, scalar2=1.0, op0=ALU.mult, op1=ALU.add),
             ["lb"], ["oml"])

        tmp4 = arena2[:, 1024:1536]
        for cg in range(12):
            par = cg % 2; eng = "dve"
            wst = arena3[:, par * 4096:(par + 1) * 4096].rearrange("p (k n) -> p k n", k=8)
            wreg = a3n(par * 8, 8)
            S.dma("sp", lambda e, wst=wst, cg=cg: e.dma_start(
                out=wst, in_=wada_d[:, cg * 512:(cg + 1) * 512].rearrange("(k p) n -> p k n", p=128)),
                "wada%d" % par, writes=wreg)
            S.dma("sp", lambda e, par=par, cg=cg: e.dma_start(out=bch[par], in_=bada_d[0:1, cg * 512:(cg + 1) * 512].partition_broadcast(128)),
                  "bada%d" % par, writes=["bch%d" % par])
            tmpw = arena2[:, par * 512:(par + 1) * 512]; tn = "tmpw%d" % par
            S.op(eng, lambda e, wst=wst, tmpw=tmpw: e.tensor_scalar_mul(out=tmpw, in0=wst[:, 0, :], scalar1=cact[:, 0:1]),
                 wreg + ["cact"], [tn])
            for k in range(1, 8):
                S.op(eng, lambda e, wst=wst, tmpw=tmpw, k=k: e.scalar_tensor_tensor(out=tmpw, in0=wst[:, k, :], scalar=cact[:, k:k + 1],
                                                                                  in1=tmpw, op0=ALU.mult, op1=ALU.add),
                     wreg + ["cact", tn], [tn])
            S.op("pe", lambda e, tmpw=tmpw: e.matmul(pb[:, :], lhsT=ones_f[:], rhs=tmpw, start=True, stop=True), [tn, "ones_f"], ["pb"])
            seg = cg // 2; half = cg % 2
            if seg in (2, 3, 4):
                dst = {2: g1bc, 3: sh2_bc, 4: sc2_bc}[seg][:, half * 512:(half + 1) * 512]
                dname = "%s%d" % ({2: "g1bc", 3: "sh2bc", 4: "sc2bc"}[seg], half)
            else:
                dst = mrow[par]; dname = "mrow%d" % par
            S.op("dve", lambda e, dst=dst, par=par: e.tensor_tensor(out=dst, in0=pb[:, :], in1=bch[par], op=ALU.add),
                 ["pb", "bch%d" % par], [dname])
            if seg in (1, 4):
                S.op("dve", lambda e, dst=dst: e.tensor_scalar_add(out=dst, in0=dst, scalar1=1.0), [dname], [dname])
            if seg in (0, 1):
                base = {0: 0, 1: 8}[seg] + half * 4
                S.op("dve", lambda e, dst=dst: e.tensor_tensor(out=r3(tmp4), in0=r3(dst), in1=ident_f[:].unsqueeze(1).broadcast_to([128, 4, 128]),
                                                               op=ALU.mult), [dname, "ident_f"], ["tmp4"])
                S.op("dve", lambda e, base=base: e.tensor_reduce(out=modcol[:, base:base + 4], in_=r3(tmp4), axis=AX.X, op=ALU.add),
                     ["tmp4"], ["modcol"])
            elif seg == 5:
                S.dma("sp", lambda e, par=par, half=half: e.dma_start(out=g2row_d[0:1, half * 512:(half + 1) * 512],
                                                                      in_=mrow[par][0:1, :]),
                      "g2st", reads=["mrow%d" % par], writes=["g2row_d"])
        for k in range(8):
            S.op("dve", lambda e, k=k: e.tensor_tensor(out=w_out_sb[:, k, :], in0=w_out_sb[:, k, :], in1=g1bc, op=ALU.mult),
                 ["w_out", "g1bc0", "g1bc1"], ["w_out"])

        S.op("dve", lambda e: e.tensor_copy(out=posf[:], in_=posi[:]), ["posi"], ["posf"])
        S.op("pe", lambda e: e.transpose(out=pb[:, 0:32], in_=posf[:], identity=ident_f[0:32, 0:32]), ["posf", "ident_f"], ["pb"])
        S.op("dve", lambda e: e.tensor_copy(out=posT[:], in_=pb[:, 0:32]), ["pb"], ["posT"])
        ang = a3(0, 4); yv = a3(4, 4); kf = a3(8, 4); kfm = a3(12, 4); ki = a3(12, 4).bitcast(I32)
        angn = a3n(0, 4); yvn = a3n(4, 4); kfn = a3n(8, 4); kin_ = a3n(12, 4)
        S.op("dve", lambda e: e.tensor_tensor(out=ang.rearrange("p (j f) -> p j f", j=32),
                                              in0=posT[:].unsqueeze(2).broadcast_to([128, 32, 64]),
                                              in1=invf_bc[:].unsqueeze(1).broadcast_to([128, 32, 64]), op=ALU.mult),
             ["posT", "invf"], angn)

        def sin_table(src, srcn, dst, dstn):
            S.op("dve", lambda e: e.tensor_single_scalar(out=kf, in_=src, scalar=1.0 / TWO_PI, op=ALU.mult), srcn, kfn)
            S.op("dve", lambda e: e.tensor_copy(out=ki, in_=kf), kfn, kin_)
            S.op("dve", lambda e: e.tensor_copy(out=kf, in_=ki), kin_, kfn)
            S.op("dve", lambda e: e.scalar_tensor_tensor(out=kf, in0=kf, scalar=-TWO_PI, in1=src, op0=ALU.mult, op1=ALU.add),
                 kfn + srcn, kfn)
            S.op("dve", lambda e: e.tensor_single_scalar(out=kfm, in_=kf, scalar=math.pi, op=ALU.is_gt), kfn, kin_)
            S.op("dve", lambda e: e.scalar_tensor_tensor(out=kf, in0=kfm, scalar=-TWO_PI, in1=kf, op0=ALU.mult, op1=ALU.add),
                 kfn + kin_, kfn)
            S.op("dve", lambda e: e.tensor_single_scalar(out=kfm, in_=kf, scalar=-math.pi, op=ALU.is_lt), kfn, kin_)
            S.op("dve", lambda e: e.scalar_tensor_tensor(out=kf, in0=kfm, scalar=TWO_PI, in1=kf, op0=ALU.mult, op1=ALU.add),
                 kfn + kin_, kfn)
            S.op("act", lambda e: e.activation(out=dst, in_=kf, func=AF.Sin), kfn, dstn)

        S.op("dve", lambda e: e.tensor_scalar_add(out=yv, in0=ang, scalar1=math.pi / 2), angn, yvn)
        sin_table(yv, yvn, arena2[:, 0:2048], ["cos"])
        sin_table(ang, angn, arena2[:, 2048:4096], ["sin"])
        S.dma("sp", lambda e: e.dma_start(out=cs_d[:, 0, :], in_=arena2[:, 0:2048]), "csst", reads=["cos"], writes=["cs_d0"])
        S.dma("sp", lambda e: e.dma_start(out=cs_d[:, 1, :], in_=arena2[:, 2048:4096]), "csst", reads=["sin"], writes=["cs_d1"])

        if stage == "setup":
            fin = []
            fin.append(S.dma("sp", lambda e: e.dma_start(out=out_d[0:128, 0:32], in_=modcol[:]), "dbg", reads=["modcol"]))
            fin.append(S.dma("sp", lambda e: e.dma_start(out=out_d[0:128, 32:96], in_=cos_t[:, 3, :]), "dbg", reads=["cos"]))
            fin.append(S.dma("sp", lambda e: e.dma_start(out=out_d[0:128, 96:160], in_=sin_t[:, 3, :]), "dbg", reads=["sin"]))
            fin.append(S.dma("sp", lambda e: e.dma_start(out=out_d[0:128, 160:672], in_=lb[:]), "dbg", reads=["lb"]))
            fin.append(S.dma("sp", lambda e: e.dma_start(out=out_d[128:256, 0:1024], in_=g1bc), "dbg", reads=["g1bc0", "g1bc1"]))
            S.emit(block, final_waits=fin)
            return nc

        S.barrier()

        def ln_stats(src, srcn):
            S.op("dve", lambda e: e.bn_stats(out=st[:, 0:6], in_=src[:, 0:512]), srcn, ["st"])
            S.op("dve", lambda e: e.bn_stats(out=st[:, 6:12], in_=src[:, 512:1024]), srcn, ["st"])
            S.op("dve", lambda e: e.bn_aggr(out=mv[:], in_=st[:]), ["st"], ["mv"])
            S.op("act", lambda e: e.activation(out=rs[:], in_=mv[:, 1:2], func=AF.Ln, bias=EPS), ["mv"], ["rs"])
            S.op("act", lambda e: e.activation(out=rs[:], in_=rs[:], func=AF.Exp, scale=-0.5), ["rs"], ["rs"])

        def transposes8(dst_ps, dstn, src, srcn):
            for c in range(8):
                S.op("pe", lambda e, c=c: e.transpose(out=dst_ps[:, c, :], in_=src[:, c * 128:(c + 1) * 128], identity=ident_b[:]),
                     srcn + ["ident_b"], dstn)

        def modulate(dst, dstn, sc0, sh0):
            mm = "act"
            for c in range(8):
                if (mm == "mix" and c % 2 == 0) or mm == "act":
                    S.op("act", lambda e, c=c: e.activation(out=dst[:, c, :], in_=ptr[:, c, :], func=AF.Identity,
                                                            scale=modcol[:, sc0 + c:sc0 + c + 1], bias=modcol[:, sh0 + c:sh0 + c + 1]),
                         ["ptr", "modcol"], [dstn + str(c)])
                else:
                    S.op("dve", lambda e, c=c: e.tensor_scalar(out=dst[:, c, :], in0=ptr[:, c, :],
                                                               scalar1=modcol[:, sc0 + c:sc0 + c + 1],
                                                               scalar2=modcol[:, sh0 + c:sh0 + c + 1], op0=ALU.mult, op1=ALU.add),
                         ["ptr", "modcol"], [dstn + str(c)])

        def proj(g, bank):
            for k in range(8):
                S.op("pe", lambda e, k=k: e.matmul(pp[bank][:, :], lhsT=hT[:, k, :], rhs=w_in_sb[:, k, g * 512:(g + 1) * 512],
                                                   start=(k == 0), stop=(k == 7)),
                     ["hT%d" % c for c in range(8)] + ["w_in"], ["pp%d" % bank])

        def sigmoid_from_psum(bank, dst, dstn):
            S.op("act", lambda e: e.activation(out=dst, in_=pp[bank][:, :], func=AF.Exp, scale=-1.0), ["pp%d" % bank], dstn)
            S.op("act", lambda e: e.activation(out=dst, in_=dst, func=AF.Ln, bias=1.0), dstn, dstn)
            S.op("act", lambda e: e.activation(out=dst, in_=dst, func=AF.Exp, scale=-1.0), dstn, dstn)

        def rotary(i, src, srcn, dst, dstn, dec, decn):
            s3 = r3(src); x1 = s3[:, :, 0:64]; x2 = s3[:, :, 64:128]
            cst = cs_t[i % 2]
            cosb = cst[:, 0, :].unsqueeze(1).broadcast_to([128, 4, 64])
            sinb = cst[:, 1, :].unsqueeze(1).broadcast_to([128, 4, 64])
            A = r3(a3(10)[:, 0:256]); B = r3(a3(10)[:, 256:512]); C = r3(a3(11)[:, 0:256]); Dd = r3(a3(11)[:, 256:512])
            decb = dec[:, 0:4].unsqueeze(2).broadcast_to([128, 4, 64])
            d3 = r3(dst)
            S.op("pool", lambda e: e.tensor_tensor(out=A, in0=x1, in1=cosb, op=ALU.mult), srcn + ["cs%d" % (i % 2)], ["t10a"])
            S.op("pool", lambda e: e.tensor_tensor(out=B, in0=x2, in1=sinb, op=ALU.mult), srcn + ["cs%d" % (i % 2)], ["t10b"])
            S.op("pool", lambda e: e.tensor_tensor(out=A, in0=A, in1=B, op=ALU.subtract), ["t10a", "t10b"], ["t10a"])
            S.op("dve", lambda e: e.tensor_tensor(out=C, in0=x1, in1=sinb, op=ALU.mult), srcn + ["cs%d" % (i % 2)], ["t11a"])
            S.op("dve", lambda e: e.tensor_tensor(out=Dd, in0=x2, in1=cosb, op=ALU.mult), srcn + ["cs%d" % (i % 2)], ["t11b"])
            S.op("dve", lambda e: e.tensor_tensor(out=C, in0=C, in1=Dd, op=ALU.add), ["t11a", "t11b"], ["t11a"])
            S.op("pool", lambda e: e.tensor_tensor(out=d3[:, :, 0:64], in0=A, in1=decb, op=ALU.mult), ["t10a", decn], [dstn + "lo"])
            S.op("dve", lambda e: e.tensor_tensor(out=d3[:, :, 64:128], in0=C, in1=decb, op=ALU.mult), ["t11a", decn], [dstn + "hi"])

        x1_stores = []
        t0 = a3(0); t1 = a3(1); t2 = a3(2); t3 = a3(3); t4 = a3(4); t5 = a3(5); t8 = a3(8); t9 = a3(9)

        def x_load(i):
            p3 = i % 3; xs = x_sb[p3]
            S.dma("sp", lambda e: e.dma_start(out=xs, in_=x_d[i * 128:(i + 1) * 128, :]), "x%d" % p3, writes=["x%d" % p3])

        def f_p1(i):
            p = i % 2; xs = x_sb[i % 3]; xn_ = ["x%d" % (i % 3)]
            ln_stats(xs, xn_)
            S.op("dve", lambda e: e.tensor_scalar(out=xn_bf[:], in0=xs, scalar1=mv[:, 0:1], scalar2=rs[:, 0:1],
                                                  op0=ALU.subtract, op1=ALU.mult), xn_ + ["mv", "rs"], ["xn_lo", "xn_hi"])

        def f_p2(i):
            p = i % 2
            S.dma("sp", lambda e: e.dma_start(out=cs_t[p][:], in_=cs_d[:, :, i * 64:(i + 1) * 64]), "cs%d" % p,
                  reads=["cs_d0", "cs_d1"], writes=["cs%d" % p])
            transposes8(ptr, ["ptr"], xn_bf, ["xn_lo", "xn_hi"])
            modulate(hT, "hT", 8, 0)

        def f_forget(i):
            p = i % 2
            proj(1, 0)
            proj(0, 1)
            sigmoid_from_psum(0, t0, ["t0"])
            S.op("dve", lambda e: e.tensor_tensor(out=t0, in0=t0, in1=oml[:], op=ALU.mult), ["t0", "oml"], ["t0"])
            S.op("pool", lambda e: e.tensor_tensor(out=t0, in0=t0, in1=lb[:], op=ALU.add), ["t0", "lb"], ["t0"])
            S.op("act", lambda e: e.activation(out=t1, in_=t0, func=AF.Ln), ["t0"], ["t1"])
            S.op("pool", lambda e: e.tensor_scalar(out=t2, in0=t0, scalar1=-1.0, scalar2=1.0, op0=ALU.mult, op1=ALU.add),
                 ["t0"], ["t2"])
            S.op("pe", lambda e: e.matmul(pb[:, :], lhsT=tri[:], rhs=t1, start=True, stop=True), ["tri", "t1"], ["pb"])
            S.op("act", lambda e: e.activation(out=t3, in_=pb[:, :], func=AF.Exp), ["pb"], ["t3"])
            S.op("act", lambda e: e.activation(out=t4, in_=pb[:, :], func=AF.Exp, scale=-1.0), ["pb"], ["t4"])
            for h in range(4):
                S.op("pe", lambda e, h=h: e.matmul(pb[:, h:h + 1], lhsT=t1[:, h * 128:(h + 1) * 128], rhs=ones_f[:, 0:1],
                                                   start=True, stop=True), ["t1", "ones_f"], ["pb"])
            S.op("act", lambda e: e.activation(out=eblA[p][:], in_=pb[:, 0:4], func=AF.Exp), ["pb"], ["ebl0_%d" % p])
            S.op("pool", lambda e: e.tensor_tensor(out=kin[p][:, 0:512], in0=t2, in1=t4, op=ALU.mult), ["t2", "t4"], ["kin_a%d" % p])

        def f_query(i):
            p = i % 2
            sigmoid_from_psum(1, t5, ["t5"])
            S.op("dve", lambda e: e.tensor_tensor(out=t5, in0=pp[1][:, :], in1=t5, op=ALU.mult), ["pp1", "t5"], ["t5"])
            S.op("dve", lambda e: e.tensor_tensor(out=qin[p][:, 0:512], in0=t5, in1=t3, op=ALU.mult), ["t5", "t3"], ["qin_a%d" % p])

        def f_retq(i):
            proj(4, 0)
            S.op("act", lambda e: e.activation(out=t8, in_=pp[0][:, :], func=AF.Copy), ["pp0"], ["t8"])

        def f_retk(i):
            proj(5, 1)
            S.op("act", lambda e: e.activation(out=t9, in_=pp[1][:, :], func=AF.Copy), ["pp1"], ["t9"])

        def f_rot(i):
            p = i % 2
            rotary(i, t8, ["t8"], qin[p][:, 512:1024], "qin_b%d" % p, qd, "qd")
            rotary(i, t9, ["t9"], kin[p][:, 512:1024], "kin_b%d" % p, kd, "kd")

        def f_values(i):
            p = i % 2
            proj(2, 0)
            S.op("act", lambda e: e.activation(out=vbf[p][:, 0:512], in_=pp[0][:, :], func=AF.Copy), ["pp0"], ["v_a%d" % p])
            proj(6, 1)
            S.op("act", lambda e: e.activation(out=vbf[p][:, 512:1024], in_=pp[1][:, :], func=AF.Copy), ["pp1"], ["v_b%d" % p])

        def f_gates(i):
            p = i % 2
            proj(3, 0)
            sigmoid_from_psum(0, ga[p], ["ga%d" % p])
            S.op("dve", lambda e: e.tensor_tensor(out=ga[p], in0=pp[0][:, :], in1=ga[p], op=ALU.mult), ["pp0", "ga%d" % p], ["ga%d" % p])
            proj(7, 1)
            sigmoid_from_psum(1, gb[p], ["gb%d" % p])
            S.op("dve", lambda e: e.tensor_tensor(out=gb[p], in0=pp[1][:, :], in1=gb[p], op=ALU.mult), ["pp1", "gb%d" % p], ["gb%d" % p])

        def b_qk(i):
            p = i % 2
            qn = ["qin_a%d" % p, "qin_b%dlo" % p, "qin_b%dhi" % p]; kn = ["kin_a%d" % p, "kin_b%dlo" % p, "kin_b%dhi" % p]
            transposes8(ptq, ["ptq"], qin[p], qn)
            S.op("act", lambda e: e.activation(out=q_inT[:], in_=ptq[:], func=AF.Copy), ["ptq"], ["qinT"])
            transposes8(ptq, ["ptq"], kin[p], kn)
            S.op("dve", lambda e: e.tensor_copy(out=k_inT[:], in_=ptq[:]), ["ptq"], ["kinT"])

        def b_sc(i, m):
            for hh in range(4):
                h = 4 * m + hh
                S.op("pe", lambda e, h=h, hh=hh: e.matmul(ps3[:, hh, :], lhsT=k_inT[:, h, :], rhs=q_inT[:, h, :],
                                                         start=True, stop=True), ["kinT", "qinT"], ["ps"])
            S.op("dve", lambda e: e.tensor_tensor(out=AT[m][:], in0=ps3, in1=tri[:].unsqueeze(1).broadcast_to([128, 4, 128]),
                                                  op=ALU.mult), ["ps", "tri"], ["AT%d" % m])

        def b_mix(i, m):
            p = i % 2
            kn = ["kin_a%d" % p, "kin_b%dlo" % p, "kin_b%dhi" % p]
            vn = ["v_a%d" % p, "v_b%d" % p][m]
            k_in = kin[p]; v_bf = vbf[p]
            for hh in range(4):
                h = 4 * m + hh
                S.op("pe", lambda e, h=h, hh=hh: e.matmul(po3[:, hh, :], lhsT=AT[m][:, hh, :], rhs=v_bf[:, h * 128:(h + 1) * 128],
                                                         start=True, stop=False), ["AT%d" % m, vn], ["po"])
                S.op("pe", lambda e, h=h, hh=hh: e.matmul(po3[:, hh, :], lhsT=q_inT[:, h, :], rhs=S_bf[:, h, :],
                                                         start=False, stop=True), ["qinT", "Sbf%d" % m], ["po"])
            for hh in range(4):
                h = 4 * m + hh
                S.op("pe", lambda e, h=h, hh=hh: e.matmul(pkv3[:, hh, :], lhsT=k_in[:, h * 128:(h + 1) * 128],
                                                         rhs=v_bf[:, h * 128:(h + 1) * 128], start=True, stop=True),
                     kn + [vn], ["pkv"])
            Sm = S32[:, 4 * m:4 * m + 4, :]
            eb4 = eblA[p][:, 0:4] if m == 0 else ebl[:, 4:8]
            ebn = "ebl0_%d" % p if m == 0 else "ebl1"
            S.op("dve", lambda e: e.tensor_tensor(out=Sm, in0=Sm, in1=pkv3, op=ALU.add), ["S32_%d" % m, "pkv"], ["S32_%d" % m])
            S.op("dve", lambda e: e.tensor_tensor(out=Sm, in0=Sm, in1=eb4.unsqueeze(2).broadcast_to([128, 4, 128]), op=ALU.mult),
                 ["S32_%d" % m, ebn], ["S32_%d" % m])
            S.op("pool", lambda e: e.tensor_copy(out=S_bf[:, 4 * m:4 * m + 4, :], in_=Sm), ["S32_%d" % m], ["Sbf%d" % m])
            for hh in range(4):
                S.op("dve", lambda e, hh=hh: e.bn_stats(out=ost[:, hh * 6:(hh + 1) * 6], in_=po3[:, hh, :]), ["po"], ["ost"])
            for hh in range(4):
                S.op("dve", lambda e, hh=hh: e.bn_aggr(out=omv[:, hh, :], in_=ost[:, hh * 6:(hh + 1) * 6]), ["ost"], ["omv"])
            on = a3(12 + m); onn = ["t%d" % (12 + m)]
            if m == 0:
                S.op("dve", lambda e: e.tensor_tensor(out=r4[:], in0=omv[:, :, 0], in1=omv[:, :, 0], op=ALU.mult), ["omv"], ["r4"])
                S.op("dve", lambda e: e.tensor_tensor(out=r4[:], in0=r4[:], in1=omv[:, :, 1], op=ALU.add), ["omv", "r4"], ["r4"])
                S.op("act", lambda e: e.activation(out=r4[:], in_=r4[:], func=AF.Ln, bias=EPS), ["r4"], ["r4"])
            else:
                S.op("act", lambda e: e.activation(out=r4[:], in_=omv[:, :, 1], func=AF.Ln, bias=EPS), ["omv"], ["r4"])
            S.op("act", lambda e: e.activation(out=r4[:], in_=r4[:], func=AF.Exp, scale=-0.5), ["r4"], ["r4"])
            r4b = r4[:].unsqueeze(2).broadcast_to([128, 4, 128])
            if m == 0:
                S.op("dve", lambda e: e.tensor_tensor(out=r3(on), in0=po3, in1=r4b, op=ALU.mult), ["po", "r4"], onn)
            else:
                S.op("dve", lambda e: e.tensor_tensor(out=r3(on), in0=po3, in1=omv[:, :, 0].unsqueeze(2).broadcast_to([128, 4, 128]),
                                                      op=ALU.subtract), ["po", "omv"], onn)
                S.op("dve", lambda e: e.tensor_tensor(out=r3(on), in0=r3(on), in1=r4b, op=ALU.mult), onn + ["r4"], onn)
            gt = [ga[p], gb[p]][m]; gtn = ["ga%d" % p, "gb%d" % p][m]
            S.op("pool", lambda e: e.tensor_tensor(out=on, in0=on, in1=normw[:, m * 512:(m + 1) * 512], op=ALU.mult),
                 onn + ["normw%d" % m], onn)
            S.op("pool", lambda e: e.tensor_tensor(out=o_fin[:, m * 512:(m + 1) * 512], in0=on, in1=gt, op=ALU.mult),
                 onn + [gtn], ["ofin%d" % m])

        x1p = a3(14, 2); x1n = a3n(14, 2)

        def b_out(i):
            p = i % 2; xs = x_sb[i % 3]; xn_ = ["x%d" % (i % 3)]
            transposes8(ptr, ["ptr"], o_fin, ["ofin0", "ofin1"])
            S.op("act", lambda e: e.activation(out=oT_sb[:], in_=ptr[:], func=AF.Copy), ["ptr"], ["oT"])
            for dc in range(2):
                for k in range(8):
                    S.op("pe", lambda e, k=k, dc=dc: e.matmul(pp[dc][:, :], lhsT=oT_sb[:, k, :], rhs=w_out_sb[:, k, dc * 512:(dc + 1) * 512],
                                                             start=(k == 0), stop=(k == 7)), ["oT", "w_out"], ["pp%d" % dc])
                S.op("dve", lambda e, dc=dc: e.scalar_tensor_tensor(out=x1p[:, dc * 512:(dc + 1) * 512], in0=xs[:, dc * 512:(dc + 1) * 512],
                                                                    scalar=ALPHA, in1=pp[dc][:, :], op0=ALU.mult, op1=ALU.add),
                     xn_ + ["pp%d" % dc], ["t%d" % (14 + dc)])
            ln_stats(x1p, x1n)
            S.op("dve", lambda e: e.tensor_scalar(out=x1p, in0=x1p, scalar1=mv[:, 0:1], scalar2=rs[:, 0:1],
                                                  op0=ALU.subtract, op1=ALU.mult), x1n + ["mv", "rs"], x1n)
            for (eng_, lo_, nm_) in (("dve", 0, "t14"), ("pool", 512, "t15")):
                S.op(eng_, lambda e, lo_=lo_: e.tensor_tensor(out=x1p[:, lo_:lo_ + 512], in0=x1p[:, lo_:lo_ + 512],
                                                               in1=l1w[:, lo_:lo_ + 512], op=ALU.mult), [nm_, "l1w"], [nm_])
                S.op(eng_, lambda e, lo_=lo_: e.tensor_tensor(out=x1p[:, lo_:lo_ + 512], in0=x1p[:, lo_:lo_ + 512],
                                                               in1=l1b[:, lo_:lo_ + 512], op=ALU.add), [nm_, "l1b"], [nm_])
            dst = out_d if stage == "mixer" else x1_d
            x1_stores.append(S.dma("pool", lambda e: e.dma_start(out=dst[i * 128:(i + 1) * 128, :], in_=x1p), "x1st",
                                   reads=x1n, writes=["x1_d%d" % i]))

        def b_h2a(i):
            if stage == "mixer":
                return
            ln_stats(x1p, x1n)
            xq = a3(12, 2)
            S.op("dve", lambda e: e.tensor_scalar(out=xq, in0=x1p, scalar1=mv[:, 0:1], scalar2=rs[:, 0:1],
                                                  op0=ALU.subtract, op1=ALU.mult), x1n + ["mv", "rs"], a3n(12, 2))
            for (eng_, lo_, nm_, hn_) in (("dve", 0, "t12", "h2_lo"), ("pool", 512, "t13", "h2_hi")):
                S.op(eng_, lambda e, lo_=lo_: e.tensor_tensor(out=xq[:, lo_:lo_ + 512], in0=xq[:, lo_:lo_ + 512],
                                                               in1=sc2_bc[:, lo_:lo_ + 512], op=ALU.mult), [nm_, "sc2bc0", "sc2bc1"], [nm_])
                S.op(eng_, lambda e, lo_=lo_: e.tensor_tensor(out=h2tok[:, lo_:lo_ + 512], in0=xq[:, lo_:lo_ + 512],
                                                               in1=sh2_bc[:, lo_:lo_ + 512], op=ALU.add),
                     [nm_, "sh2bc0", "sh2bc1"], [hn_])
            S.dma("pool", lambda e: e.dma_start(out=h2_d[i * 128:(i + 1) * 128, :], in_=h2tok[:]), "h2st",
                  reads=["h2_lo", "h2_hi"], writes=["h2_d%d" % i])

        def b_h2b(i):
            if stage == "mixer":
                return
            transposes8(ptr, ["ptr"], h2tok, ["h2_lo", "h2_hi"])
            S.op("act", lambda e: e.activation(out=h2T_t[:], in_=ptr[:], func=AF.Copy), ["ptr"], ["h2T"])
            for k in range(8):
                S.op("pe", lambda e, k=k: e.matmul(pb[:, 0:36], lhsT=h2T_t[:, k, :], rhs=wr_sb[:, k, :], start=(k == 0), stop=(k == 7)),
                     ["h2T", "wr_sb"], ["pb"])
            S.op("dve", lambda e: e.tensor_tensor(out=logits_all[:, i, :], in0=pb[:, 0:36], in1=br_bc[:], op=ALU.add),
                 ["pb", "br_bc"], ["logits"])

        x_load(0)
        if ntiles > 1: x_load(1)
        f_p1(0)
        for i in range(-1, ntiles + 1):
            if 2 <= i + 2 < ntiles: x_load(i + 2)
            nf = i + 1 if i + 1 < ntiles else None
            nn = i + 2 if 1 <= i + 2 < ntiles else None
            bk = i if 0 <= i < ntiles else None
            bh = i - 1 if 0 <= i - 1 < ntiles else None
            if bh is not None: b_h2a(bh)
            if nf is not None: f_p2(nf)
            if bk is not None: b_qk(bk)
            if bk is not None: b_sc(bk, 0); b_sc(bk, 1)
            if bh is not None: b_h2b(bh)
            if nf is not None: f_forget(nf); f_query(nf)
            if bk is not None: b_mix(bk, 0)
            if nf is not None: f_retq(nf); f_retk(nf)
            if bk is not None: b_mix(bk, 1)
            if nf is not None: f_values(nf)
            if bk is not None and stage == "full":
                for (src_, dst_, nm_) in ((wg_d, wgb_d, "cg"), (wu_d, wub_d, "cu"), (wd_d, wdb_d, "cd")):
                    S.dma("pool", lambda e, src_=src_, dst_=dst_, bk=bk: e.dma_start(out=dst_[bk * 256:(bk + 1) * 256, :],
                                                                                    in_=src_[bk * 256:(bk + 1) * 256, :]),
                          "wcast_" + nm_, writes=["%s_%d" % (nm_, bk)])
            if nn is not None: f_p1(nn)
            if bk is not None: b_out(bk)
            if stage == "full" and bk is not None and 4 <= bk < 12:
                zz = bk - 4
                S.dma("sp", lambda e, zz=zz: e.dma_start(out=xs_d[zz * 2048:(zz + 1) * 2048, :], in_=zeros_d[:, :]),
                      "xsz", writes=["xs_z%d" % zz])
            if nf is not None: f_rot(nf)
            if nf is not None: f_gates(nf)

        if stage == "mixer":
            if stop < 99:
                S.barrier()
                x1_stores.append(S.dma("sp", lambda e: e.dma_start(out=out_d[0:128, 0:128], in_=ident_f[:]), "dbg", reads=["ident_f"]))
            S.emit(block, final_waits=x1_stores)
            return nc

        S.barrier()

        l2w = a3(0, 2); l2b = a3(2, 2); g2bc = a3(4, 2)
        S.dma("sp", lambda e: e.dma_start(out=l2w, in_=l2w_d.partition_broadcast(128)), "l2w", writes=a3n(0, 2))
        S.dma("sp", lambda e: e.dma_start(out=l2b, in_=l2b_d.partition_broadcast(128)), "l2b", writes=a3n(2, 2))
        S.dma("sp", lambda e: e.dma_start(out=g2bc, in_=g2row_d.partition_broadcast(128)), "g2ld", reads=["g2row_d"], writes=a3n(4, 2))

        REG = {}

        def _pool_init(g):
            REG["xs"] = g.to_reg(16383)
            REG["w"] = g.to_reg(4095)
        S.init_pool = _pool_init

        def av(lo, n):
            return arena[:, lo:lo + n]
        NW = 3
        Wg = [av(w * 12288, 4096).rearrange("p (k n) -> p k n", k=8) for w in range(NW)]
        Wu = [av(w * 12288 + 4096, 4096).rearrange("p (k n) -> p k n", k=8) for w in range(NW)]
        Wd = [av(w * 12288 + 8192, 4096).rearrange("p (k n) -> p k n", k=4) for w in range(NW)]
        ztile = av(24576, 1024)
        h2ld = [av(25600 + w * 1024, 1024) for w in range(2)] + [av(29696 + w * 1024, 1024) for w in range(2)]
        Mb = av(27648, 1024)
        Ls_b = av(28672, 128); ones_b = av(28800, 128)
        a2b = arena2[:, 2048:8192].bitcast(BF16)
        xs_sb = [a2b[:, w * 1024:(w + 1) * 1024] for w in range(3)]
        xsT = [a2b[:, 3072 + w * 1024:3072 + (w + 1) * 1024].rearrange("p (k n) -> p k n", k=8) for w in range(2)]
        act_sb = [a2b[:, 5120 + w * 512:5120 + (w + 1) * 512] for w in range(2)]
        actT = [a2b[:, 6144 + w * 512:6144 + (w + 1) * 512].rearrange("p (k n) -> p k n", k=4) for w in range(2)]

        def b2(s_):
            return arena2[:, s_ * 1024:(s_ + 1) * 1024]

        def v3(ap):
            return ap.rearrange("p (j x) -> p j x", j=32)

        gl = logits_all[:, :, 0:4]
        el4 = logits_all[:, :, 4:36].rearrange("p j (g e) -> p j g e", g=4)
        gmax = gsm[:, 0, :]; gsum = gsm[:, 1, :]; pstar = gsm[:, 2, :]; v1 = gsm[:, 3, :]; v2 = gsm[:, 4, :]
        w1 = gsm[:, 5, :]; w2 = gsm[:, 6, :]
        gtmp = qf[:, 256:384].rearrange("p (a b) -> p a b", a=32); gm = qf[:, 384:512].rearrange("p (a b) -> p a b", a=32)
        elm = v3(a3(6, 2)); elmn = a3n(6, 2)
        mk1 = v3(a3(8, 2)); mk1n = a3n(8, 2)
        mk2 = v3(a3(10, 2)); mk2n = a3n(10, 2)
        elm4 = a3(6, 2).rearrange("p (j g e) -> p j g e", j=32, g=4)

        def bc32(ap, n):
            return ap.unsqueeze(2).broadcast_to([128, 32, n])

        S.op("dve", lambda e: e.tensor_reduce(out=gmax, in_=gl, axis=AX.X, op=ALU.max), ["logits"], ["gmax"])
        S.op("dve", lambda e: e.tensor_tensor(out=gtmp, in0=gl, in1=bc32(gmax, 4), op=ALU.subtract), ["logits", "gmax"], ["gtmp"])
        S.op("act", lambda e: e.activation(out=gtmp, in_=gtmp, func=AF.Exp), ["gtmp"], ["gtmp"])
        S.op("dve", lambda e: e.tensor_reduce(out=gsum, in_=gtmp, axis=AX.X, op=ALU.add), ["gtmp"], ["gsum"])
        S.op("dve", lambda e: e.reciprocal(out=pstar, in_=gsum), ["gsum"], ["pstar"])
        S.op("dve", lambda e: e.tensor_tensor(out=gm, in0=gl, in1=bc32(gmax, 4), op=ALU.is_equal), ["logits", "gmax"], ["gm"])
        S.op("dve", lambda e: e.tensor_tensor(out=elm4, in0=el4, in1=gm.unsqueeze(3).broadcast_to([128, 32, 4, 8]), op=ALU.mult),
             ["logits", "gm"], elmn)
        S.op("dve", lambda e: e.tensor_scalar(out=gtmp, in0=gm, scalar1=-1.0, scalar2=BIG, op0=ALU.add, op1=ALU.mult),
             ["gm"], ["gtmp"])
        S.op("dve", lambda e: e.tensor_tensor(out=elm4, in0=elm4, in1=gtmp.unsqueeze(3).broadcast_to([128, 32, 4, 8]), op=ALU.add),
             elmn + ["gtmp"], elmn)
        S.op("dve", lambda e: e.tensor_reduce(out=v1, in_=elm, axis=AX.X, op=ALU.max), elmn, ["v1"])
        S.op("dve", lambda e: e.tensor_tensor(out=mk1, in0=elm, in1=bc32(v1, 32), op=ALU.is_equal), elmn + ["v1"], mk1n)
        S.op("dve", lambda e: e.scalar_tensor_tensor(out=elm, in0=mk1, scalar=-BIG, in1=elm, op0=ALU.mult, op1=ALU.add),
             elmn + mk1n, elmn)
        S.op("dve", lambda e: e.tensor_reduce(out=v2, in_=elm, axis=AX.X, op=ALU.max), elmn, ["v2"])
        S.op("dve", lambda e: e.tensor_tensor(out=mk2, in0=elm, in1=bc32(v2, 32), op=ALU.is_equal), elmn + ["v2"], mk2n)
        S.op("dve", lambda e: e.tensor_tensor(out=w1, in0=v2, in1=v1, op=ALU.subtract), ["v1", "v2"], ["w1"])
        S.op("act", lambda e: e.activation(out=w1, in_=w1, func=AF.Exp), ["w1"], ["w1"])
        S.op("dve", lambda e: e.tensor_scalar_add(out=w1, in0=w1, scalar1=1.0), ["w1"], ["w1"])
        S.op("dve", lambda e: e.reciprocal(out=w1, in_=w1), ["w1"], ["w1"])
        S.op("dve", lambda e: e.tensor_scalar(out=w2, in0=w1, scalar1=-1.0, scalar2=1.0, op0=ALU.mult, op1=ALU.add), ["w1"], ["w2"])
        S.op("dve", lambda e: e.tensor_tensor(out=w1, in0=w1, in1=pstar, op=ALU.mult), ["w1", "pstar"], ["w1"])
        S.op("dve", lambda e: e.tensor_tensor(out=w2, in0=w2, in1=pstar, op=ALU.mult), ["w2", "pstar"], ["w2"])

        S.op("dve", lambda e: e.tensor_tensor(out=Ls_b, in0=tri[:], in1=ident_f[:], op=ALU.subtract), ["tri", "ident_f"], ["Ls_b"])
        S.op("pool", lambda e: e.memset(ones_b, 1.0), (), ["ones_b"])
        S.op("pool", lambda e: e.memset(ztile, 0.0), (), ["ztile"])
        S.op("dve", lambda e: e.tensor_tensor(out=v3(Mb), in0=mk1, in1=mk2, op=ALU.add), mk1n + mk2n, ["Mb"])
        for hf in range(2):
            S.op("pe", lambda e, hf=hf: e.matmul(pp[hf][:, :], lhsT=Ls_b, rhs=Mb[:, hf * 512:(hf + 1) * 512], start=True, stop=True),
                 ["Ls_b", "Mb"], ["pp%d" % hf])
        S.op("pe", lambda e: e.matmul(pb[:, :], lhsT=ones_b, rhs=Mb[:, 0:512], start=True, stop=True), ["ones_b", "Mb"], ["pb"])
        S.op("pe", lambda e: e.matmul(ps_[:, :], lhsT=ones_b, rhs=Mb[:, 512:1024], start=True, stop=True), ["ones_b", "Mb"], ["ps"])
        R1s = b2(2); CSs = b2(3); A = [b2(0), b2(1)]
        for hf in range(2):
            S.op("act", lambda e, hf=hf: e.activation(out=R1s[:, hf * 512:(hf + 1) * 512], in_=pp[hf][:, :], func=AF.Copy),
                 ["pp%d" % hf], ["b2_2"])
        def bn(x_):
            return ["b2_%d" % x_, "b2_%dlo" % x_]
        S.op("dve", lambda e: e.tensor_copy(out=A[0][:, 0:512], in_=pb[:, :]), ["pb"], ["b2_0lo"])
        S.op("dve", lambda e: e.tensor_copy(out=A[0][:, 512:1024], in_=ps_[:, :]), ["ps"], ["b2_0"])
        S.op("pool", lambda e: e.tensor_copy(out=CSs, in_=A[0]), bn(0), ["b2_3"])
        cur = 0
        for sft in (1, 2, 4, 8, 16):
            src = v3(A[cur]); dst = v3(A[1 - cur]); sn = bn(cur); dn = bn(1 - cur)
            S.op("pool", lambda e, src=src, dst=dst, sft=sft: e.tensor_copy(out=dst[:, 0:sft, :], in_=src[:, 0:sft, :]), sn, [dn[1]])
            S.op("dve", lambda e, src=src, dst=dst, sft=sft: e.tensor_tensor(out=dst[:, sft:32, :], in0=src[:, sft:32, :],
                                                                             in1=src[:, 0:32 - sft, :], op=ALU.add), sn, [dn[0]])
            cur = 1 - cur
        Iinc = A[cur]; In = bn(cur); oth = A[1 - cur]; on_ = bn(1 - cur)
        cnt = gsm[:, 7, :]
        nbk = gsm[:, 0, :]; pend = gsm[:, 1, :]; pst = gsm[:, 3, :]; tmp32 = gsm[:, 4, :]
        nbi = gm.rearrange("p a b -> p (a b)")[:, 0:32].bitcast(I32)
        S.op("dve", lambda e: e.tensor_scalar(out=cnt, in0=v3(Iinc)[:, 31, :], scalar1=255.0, scalar2=1.0 / 256.0,
                                              op0=ALU.add, op1=ALU.mult), In, ["cnt"])
        S.op("dve", lambda e: e.tensor_scalar_add(out=cnt, in0=cnt, scalar1=-0.498046875), ["cnt"], ["cnt"])
        S.op("dve", lambda e: e.tensor_copy(out=nbi, in_=cnt), ["cnt", "gm", "gmax", "gsum"], ["nbi"])
        S.op("dve", lambda e: e.tensor_copy(out=nbk, in_=nbi), ["nbi", "gmax"], ["nbk"])
        S.op("dve", lambda e: e.tensor_copy(out=pend, in_=nbk), ["nbk", "gsum"], ["pend"])
        pcur = pend; poth = tmp32; pcn = "pend"; pon = "tmp32"
        for sft in (1, 2, 4, 8, 16):
            S.op("dve", lambda e, pcur=pcur, poth=poth, sft=sft: e.tensor_copy(out=poth[:, 0:sft], in_=pcur[:, 0:sft]),
                 [pcn, "v2"], [pon])
            S.op("dve", lambda e, pcur=pcur, poth=poth, sft=sft: e.tensor_tensor(out=poth[:, sft:32], in0=pcur[:, sft:32],
                                                                                in1=pcur[:, 0:32 - sft], op=ALU.add), [pcn], [pon])
            pcur, poth = poth, pcur; pcn, pon = pon, pcn
        pendf = pcur; pendn = pcn
        S.op("dve", lambda e: e.tensor_tensor(out=pst, in0=pendf, in1=nbk, op=ALU.subtract), [pendn, "nbk", "v1"], ["pst"])
        S.op("dve", lambda e: e.tensor_single_scalar(out=pst, in_=pst, scalar=256.0, op=ALU.mult), ["pst"], ["pst"])
        dfull = v3(oth)
        S.op("dve", lambda e: e.tensor_tensor(out=oth, in0=Iinc, in1=CSs, op=ALU.subtract), In + ["b2_3"], on_)
        S.op("dve", lambda e: e.tensor_tensor(out=oth, in0=oth, in1=R1s, op=ALU.add), on_ + ["b2_2"], on_)
        S.op("dve", lambda e: e.tensor_tensor(out=dfull, in0=dfull, in1=pst.unsqueeze(1).broadcast_to([128, 32, 32]), op=ALU.add),
             on_ + ["pst"], on_)
        d12f = gm.rearrange("p a b -> p (a b)")[:, 32:96]
        dest_i = T("dest_i", [128, 64], I32)
        tmpm = v3(b2(4))
        for kk, (mk, mkn) in enumerate(((mk1, mk1n), (mk2, mk2n))):
            S.op("dve", lambda e, mk=mk: e.tensor_tensor(out=tmpm, in0=mk, in1=dfull, op=ALU.mult), mkn + on_, ["b2_4"])
            S.op("dve", lambda e, kk=kk: e.tensor_reduce(out=d12f[:, kk * 32:(kk + 1) * 32], in_=tmpm, axis=AX.X, op=ALU.add),
                 ["b2_4", "nbi"], ["d12f%d" % kk])
        S.op("dve", lambda e: e.tensor_copy(out=dest_i[:], in_=d12f), ["d12f0", "d12f1"], ["dest_i"])
        NBLK = 64
        cmp3 = arena2[:, 5 * 1024:7 * 1024].rearrange("p (b x) -> p b x", b=NBLK)
        bef = T("bef", [128, NBLK]); widx = T("widx", [128, 2, NBLK], I32)
        S.op("dve", lambda e: e.tensor_tensor(out=cmp3, in0=pendf.unsqueeze(1).broadcast_to([128, NBLK, 32]),
                                              in1=blkiota[:, 0:NBLK].unsqueeze(2).broadcast_to([128, NBLK, 32]), op=ALU.is_le),
             [pendn, "blkiota"], ["b2_5", "b2_6", "b2_7"])
        S.op("dve", lambda e: e.tensor_reduce(out=bef[:], in_=cmp3, axis=AX.X, op=ALU.add), ["b2_5", "b2_6", "b2_7"], ["bef"])
        S.op("dve", lambda e: e.tensor_scalar(out=bef[:], in0=bef[:], scalar1=128.0, scalar2=piota[:, 0:1], op0=ALU.mult, op1=ALU.add),
             ["bef", "piota"], ["bef"])
        S.op("dve", lambda e: e.tensor_copy(out=widx[:, 0, :], in_=bef[:]), ["bef"], ["widx0"])

        zn = ["xs_z%d" % zz for zz in range(8)]
        scat = []
        for j in range(NT):
            hb = h2ld[j % 4]; hbn = "h2ld%d" % (j % 4)
            S.dma("sp", lambda e, j=j, hb=hb: e.dma_start(out=hb, in_=h2_d[j * 128:(j + 1) * 128, :]), hbn,
                  reads=["h2_d%d" % j], writes=[hbn])
            for kk in range(2):
                S.dma("pool", lambda e, j=j, kk=kk, hb=hb: e.indirect_dma_start(
                    out=xs_d[:, :], out_offset=bass.IndirectOffsetOnAxis(ap=dest_i[:, kk * 32 + j:kk * 32 + j + 1], axis=0),
                    in_=hb, in_offset=None, bounds_check=REG["xs"], oob_is_err=False),
                    "scat%d_%d" % (j % 4, kk), reads=[hbn, "dest_i"] + zn, writes=["xs_s%d_%d" % (j, kk)])
        xsn = ["xs_s%d_%d" % (j, kk) for j in range(NT) for kk in range(2)]
        S.barrier()

        slt = [a3(12), a3(13)]
        ysb = [b2(0), b2(1)]
        pau = [(pp[0], "pp0", pp[1], "pp1"), (pb, "pb", ps_, "ps")]
        def gather_weights(blk):
            wb = blk % NW
            for (Wt, wsrc, nm) in ((Wg, wgb_d, "wg"), (Wu, wub_d, "wu"), (Wd, wdb_d, "wd")):
                Wflat = Wt[wb].rearrange("p k n -> p (k n)")
                S.dma("pool", lambda e, Wflat=Wflat, wsrc=wsrc, blk=blk: e.indirect_dma_start(
                    out=Wflat, out_offset=None, in_=wsrc.rearrange("(r two) n -> r (two n)", two=2),
                    in_offset=bass.IndirectOffsetOnAxis(ap=widx[:, 0, blk:blk + 1], axis=0),
                    bounds_check=REG["w"], oob_is_err=False),
                    "%s%d" % (nm, wb), reads=["widx0"], writes=["%s%d_0" % (nm, wb), "%s%d_1" % (nm, wb)])

        def xs_load(sl):
            xb = sl % 3
            S.dma("sp", lambda e: e.dma_start(out=xs_sb[xb], in_=xs_d[sl * 128:(sl + 1) * 128, :]),
                  "xsld%d" % xb, writes=["xs_sb%d" % xb])

        def stage_a1(sl):
            xb = sl % 3; sb = sl % 2
            transposes8(ptr, ["ptr"], xs_sb[xb], ["xs_sb%d" % xb])
            S.op("dve", lambda e: e.tensor_copy(out=xsT[sb], in_=ptr[:]), ["ptr"], ["xsT%d" % sb])

        def stage_a2(sl):
            wb = (sl // 2) % NW; sb = sl % 2
            pa_, pan_, pu_, pun_ = pau[sb]
            wgn = ["wg%d_0" % wb, "wg%d_1" % wb]; wun = ["wu%d_0" % wb, "wu%d_1" % wb]
            for k in range(8):
                S.op("pe", lambda e, k=k: e.matmul(pa_[:, :], lhsT=xsT[sb][:, k, :], rhs=Wg[wb][:, k, :],
                                                   start=(k == 0), stop=(k == 7)), ["xsT%d" % sb] + wgn, [pan_])
            for k in range(8):
                S.op("pe", lambda e, k=k: e.matmul(pu_[:, :], lhsT=xsT[sb][:, k, :], rhs=Wu[wb][:, k, :],
                                                   start=(k == 0), stop=(k == 7)), ["xsT%d" % sb] + wun, [pun_])

        def stage_b1(sl):
            sb = sl % 2
            pa_, pan_, pu_, pun_ = pau[sb]
            S.op("act", lambda e: e.activation(out=slt[sb], in_=pa_[:, :], func=AF.Silu), [pan_], ["t%d" % (12 + sb)])
            S.op("dve", lambda e: e.tensor_tensor(out=act_sb[sb], in0=slt[sb], in1=pu_[:, :], op=ALU.mult),
                 ["t%d" % (12 + sb), pun_], ["act_sb%d" % sb])
            for fc in range(4):
                S.op("pe", lambda e, fc=fc: e.transpose(out=ptq[:, fc, :], in_=act_sb[sb][:, fc * 128:(fc + 1) * 128],
                                                        identity=ident_b[:]), ["act_sb%d" % sb, "ident_b"], ["ptq"])
            S.op("act", lambda e: e.activation(out=actT[sb], in_=ptq[:, 0:4, :], func=AF.Copy), ["ptq"], ["actT%d" % sb])

        def stage_b2(sl):
            wb = (sl // 2) % NW; sb = sl % 2
            wdn = ["wd%d_0" % wb, "wd%d_1" % wb]
            for dc in range(2):
                pyb = [po, pkv][dc]; pyn_ = ["po", "pkv"][dc]
                for fc in range(4):
                    S.op("pe", lambda e, fc=fc, dc=dc, pyb=pyb: e.matmul(
                        pyb[:, :], lhsT=actT[sb][:, fc, :], rhs=Wd[wb][:, fc, dc * 512:(dc + 1) * 512],
                        start=(fc == 0), stop=(fc == 3)), ["actT%d" % sb] + wdn, [pyn_])
            S.op("act", lambda e: e.activation(out=ysb[sb][:, 0:512], in_=po[:, :], func=AF.Copy), ["po"], ["ysb%d_0" % sb])
            S.op("dve", lambda e: e.tensor_copy(out=ysb[sb][:, 512:1024], in_=pkv[:, :]), ["pkv"], ["ysb%d_1" % sb])
            S.dma("sp", lambda e: e.dma_start(out=ys_d[sl * 128:(sl + 1) * 128, :], in_=ysb[sb]),
                  "ysst%d" % sb, reads=["ysb%d_0" % sb, "ysb%d_1" % sb], writes=["ys_%d" % sl])

        NSUB = 2 * NBLK
        for bb in range(NW): gather_weights(bb)
        xs_load(0); xs_load(1)
        for it in range(NSUB + 3):
            if it + 2 < NSUB: xs_load(it + 2)
            if it < NSUB: stage_a1(it)
            if 0 <= it - 1 < NSUB: stage_a2(it - 1)
            if 0 <= it - 2 < NSUB: stage_b1(it - 2)
            if 0 <= it - 3 < NSUB: stage_b2(it - 3)
            if it >= 4 and (it - 4) % 2 == 0:
                nb_ = (it - 4) // 2 + NW
                if nb_ < NBLK: gather_weights(nb_)
        S.barrier()

        out_stores = []
        NB = 4
        arf = arena[:, 0:16384].bitcast(F32)

        def cbuf(j):
            pj = j % NB
            return (arf[:, (2 * pj) * 1024:(2 * pj + 1) * 1024], arf[:, (2 * pj + 1) * 1024:(2 * pj + 2) * 1024], b2(pj),
                    "cg%d_0" % pj, "cg%d_1" % pj, ["cx%d" % pj], pj)

        def c1(j):
            y1g, y2g, xt, y1n, y2n, xtn, pj = cbuf(j)
            for (yg, yn, kk) in ((y1g, y1n, 0), (y2g, y2n, 1)):
                S.dma("pool", lambda e, yg=yg, kk=kk: e.indirect_dma_start(
                    out=yg, out_offset=None, in_=ys_d[:, :],
                    in_offset=bass.IndirectOffsetOnAxis(ap=dest_i[:, kk * 32 + j:kk * 32 + j + 1], axis=0),
                    bounds_check=REG["xs"], oob_is_err=False), "yg%d_%d" % (pj, kk), reads=["dest_i"], writes=[yn])
            S.dma("sp", lambda e: e.dma_start(out=xt, in_=x1_d[j * 128:(j + 1) * 128, :]), "x1ld%d" % pj,
                  reads=["x1_d%d" % j], writes=xtn)

        def c2(j):
            y1g, y2g, xt, y1n, y2n, xtn, pj = cbuf(j)
            S.op("act", lambda e: e.activation(out=y1g, in_=y1g, func=AF.Copy, scale=w1[:, j:j + 1]), [y1n, "w1"], [y1n])
            S.op("dve", lambda e: e.scalar_tensor_tensor(out=y1g, in0=y2g, scalar=w2[:, j:j + 1], in1=y1g,
                                                         op0=ALU.mult, op1=ALU.add), [y1n, y2n, "w2"], [y1n])
            S.op("pool", lambda e: e.tensor_tensor(out=y1g, in0=y1g, in1=g2bc, op=ALU.mult), [y1n] + a3n(4, 2), [y1n])

        def c3(j):
            y1g, y2g, xt, y1n, y2n, xtn, pj = cbuf(j)
            S.op("dve", lambda e: e.scalar_tensor_tensor(out=xt, in0=xt, scalar=ALPHA, in1=y1g, op0=ALU.mult, op1=ALU.add),
                 xtn + [y1n], xtn)
            S.op("dve", lambda e: e.bn_stats(out=cst_[pj][:, 0:6], in_=xt[:, 0:512]), xtn, ["cst%d" % pj])
            S.op("dve", lambda e: e.bn_stats(out=cst_[pj][:, 6:12], in_=xt[:, 512:1024]), xtn, ["cst%d" % pj])
            S.op("dve", lambda e: e.bn_aggr(out=cmv_[pj][:], in_=cst_[pj][:]), ["cst%d" % pj], ["cmv%d" % pj])
            S.op("act", lambda e: e.activation(out=crs_[pj][:], in_=cmv_[pj][:, 1:2], func=AF.Ln, bias=EPS), ["cmv%d" % pj], ["crs%d" % pj])
            S.op("act", lambda e: e.activation(out=crs_[pj][:], in_=crs_[pj][:], func=AF.Exp, scale=-0.5), ["crs%d" % pj], ["crs%d" % pj])
            S.op("dve", lambda e: e.tensor_scalar(out=cnm_[pj][:], in0=cmv_[pj][:, 0:1], scalar1=-1.0, scalar2=crs_[pj][:, 0:1],
                                                  op0=ALU.mult, op1=ALU.mult), ["cmv%d" % pj, "crs%d" % pj], ["cnm%d" % pj])

        def c4(j):
            y1g, y2g, xt, y1n, y2n, xtn, pj = cbuf(j)
            S.op("act", lambda e: e.activation(out=xt, in_=xt, func=AF.Identity, scale=crs_[pj][:, 0:1], bias=cnm_[pj][:, 0:1]),
                 xtn + ["cnm%d" % pj, "crs%d" % pj], xtn)
            S.op("dve", lambda e: e.tensor_tensor(out=xt, in0=xt, in1=l2w, op=ALU.mult), xtn + a3n(0, 2), xtn)
            S.op("dve", lambda e: e.tensor_tensor(out=xt, in0=xt, in1=l2b, op=ALU.add), xtn + a3n(2, 2), xtn)
            out_stores.append(S.dma("sp", lambda e: e.dma_start(out=out_d[j * 128:(j + 1) * 128, :], in_=xt), "ost%d" % pj, reads=xtn))

        cst_ = [ost[:, 0:12], ost[:, 12:24], st[:, 0:12], T("cst3", [128, 12])]
        cmv_ = [omv[:, 0, :], omv[:, 1, :], omv[:, 2, :], omv[:, 3, :]]
        crs_ = [r4[:, 0:1], r4[:, 1:2], r4[:, 2:3], r4[:, 3:4]]
        cnm_ = [mv[:, 0:1], mv[:, 1:2], rs[:, 0:1], T("cnm3", [128, 1])]
        for it in range(NT + 3):
            if it < NT: c1(it)
            if 0 <= it - 1 < NT: c2(it - 1)
            if 0 <= it - 2 < NT: c3(it - 2)
            if 0 <= it - 3 < NT: c4(it - 3)
        S.emit(block, final_waits=out_stores)
    return nc


def _consts():
    ident = np.eye(128, dtype=np.float32)
    s = np.arange(128)
    tri = (s[:, None] <= s[None, :]).astype(np.float32)
    invf = np.power(np.float32(10000.0), -np.arange(0, 128, 2, dtype=np.float32) / np.float32(128)).astype(np.float32)[None, :]
    gam = 1.0 - np.exp2(-5.0 - np.arange(4, dtype=np.float64))
    lg = np.log(gam)
    p = np.arange(128, dtype=np.float64)[:, None] + 1.0
    qd = (np.exp(p * lg[None, :]) * (128.0 ** -0.5)).astype(np.float32)
    kd = np.exp(-p * lg[None, :]).astype(np.float32)
    ebr = np.broadcast_to(np.exp(128.0 * lg)[None, :], (128, 4)).astype(np.float32).copy()
    blkiota = np.broadcast_to(np.arange(96, dtype=np.float32)[None, :], (128, 96)).copy()
    piota = np.arange(128, dtype=np.float32)[:, None].copy()
    import ml_dtypes
    zeros_bf = np.zeros((2048, 1024), dtype=ml_dtypes.bfloat16)
    return dict(ident=ident, tri=tri, invf=invf, qd=qd, kd=kd, ebr=ebr, blkiota=blkiota, piota=piota, zeros_bf=zeros_bf)


def make_in_maps(inputs):
    f = lambda a: np.ascontiguousarray(np.asarray(a, dtype=np.float32))
    x = f(inputs["x"]); c = f(inputs["c"]); pos = np.ascontiguousarray(np.asarray(inputs["positions"], dtype=np.int32))
    shared = dict(
        w_ada=f(inputs["w_ada"][0]), b_ada=f(inputs["b_ada"][0])[None, :], w_in=f(inputs["w_in"][0]), w_out=f(inputs["w_out"][0]),
        hgrn_lb=f(inputs["hgrn_lb"]), hgrn_norm_w=f(inputs["hgrn_norm_w"][0])[None, :], ret_norm_w=f(inputs["ret_norm_w"][0])[None, :],
        post_ln1_w=f(inputs["post_ln1_w"][0])[None, :], post_ln1_b=f(inputs["post_ln1_b"][0])[None, :],
        post_ln2_w=f(inputs["post_ln2_w"][0])[None, :], post_ln2_b=f(inputs["post_ln2_b"][0])[None, :],
        w_r=np.ascontiguousarray(np.concatenate([f(inputs["w_rg"][0]), f(inputs["w_re"][0])], axis=1)),
        b_r=np.ascontiguousarray(np.concatenate([f(inputs["b_rg"][0]), f(inputs["b_re"][0])], axis=0))[None, :],
        w_gate=np.ascontiguousarray(f(inputs["w_gate"][0]).reshape(32, 8, 128, 512).transpose(0, 2, 1, 3)).reshape(8192, 2048),
        w_up=np.ascontiguousarray(f(inputs["w_up"][0]).reshape(32, 8, 128, 512).transpose(0, 2, 1, 3)).reshape(8192, 2048),
        w_down=np.ascontiguousarray(f(inputs["w_down"][0]).reshape(32, 4, 128, 1024).transpose(0, 2, 1, 3)).reshape(8192, 2048),
    )
    shared.update(_consts())
    maps = []
    for b in range(8):
        m = dict(shared)
        m["x"] = np.ascontiguousarray(x[b])
        m["ccol"] = np.ascontiguousarray(c[b].reshape(8, 128).T)
        m["pos"] = np.ascontiguousarray(pos[b].reshape(32, 128))
        maps.append(m)
    return maps


def kernel(**inputs):
    nc = build_program("full")
    maps = make_in_maps(inputs)
    res = run_bass_kernel_spmd(nc, maps, core_ids=list(range(8)))
    return np.stack([np.asarray(r["out"], dtype=np.float32) for r in res.results], axis=0)
```
